# Optimizing a Trainium2 kernel written in Bass

```python
import jax, jax.numpy as jnp
from jax import lax
import numpy as np

D_MODEL = 2048
BATCH = 4
SEQ = 8192
DEPTH = 4

D_MIX = D_MODEL
NORM_EPS = 1e-6
NEG_INF = -1e30
SGU_WIDTH = D_MIX // 4
SGU_GROUPS = 4
SGU_GROUP_DIM = SGU_WIDTH // SGU_GROUPS
SGU_CHUNK = 128
ATT_HEADS = 4
ATT_HEAD_DIM = 128
ATT_WIDTH = ATT_HEADS * ATT_HEAD_DIM
ROPE_DIM = ATT_HEAD_DIM // 4
ROPE_THETA = 500000.0
MOBA_BLOCK = 256
MOBA_TOPK = 3
MOBA_Q_BLOCK = 64
SSM_WIDTH = D_MIX - SGU_WIDTH - ATT_WIDTH
SSM_HEAD_DIM = 64
SSM_HEADS = SSM_WIDTH // SSM_HEAD_DIM
SSM_GROUPS = 2
SSM_STATE = 128
SSM_CONV = 4
SSM_CHUNK = 128
SSM_CONV_DIM = SSM_WIDTH + 2 * SSM_GROUPS * SSM_STATE
IN_SECTIONS = (SGU_WIDTH, SGU_WIDTH, ATT_WIDTH, ATT_WIDTH, ATT_WIDTH, SSM_WIDTH, SSM_CONV_DIM, SSM_HEADS)
D_IN_PROJ = sum(IN_SECTIONS)
MOE_GROUPS = 4
MOE_EXPERTS_PER_GROUP = 8
MOE_EXPERTS = MOE_GROUPS * MOE_EXPERTS_PER_GROUP
MOE_TOPK = 2
MOE_FF = D_MODEL // 4
MOE_BLOCK = 128

kernel_name = "hybrid_sgu_moba_ssd_hmoe_trunk"


def rms_norm(x, g):
    xf = x.astype(jnp.float32)
    y = xf * lax.rsqrt(jnp.mean(xf * xf, axis=-1, keepdims=True) + NORM_EPS)
    return (y * g.astype(jnp.float32)).astype(x.dtype)


def rotary_tables(seq):
    pos = jnp.arange(seq, dtype=jnp.float32)
    inv_freq = ROPE_THETA ** (-jnp.arange(0, ROPE_DIM, 2, dtype=jnp.float32) / ROPE_DIM)
    ang = pos[:, None] * inv_freq[None, :]
    return jnp.cos(ang), jnp.sin(ang)


def apply_partial_rotary(t, cos, sin):
    half = ROPE_DIM // 2
    cos = cos.astype(t.dtype)
    sin = sin.astype(t.dtype)
    t1 = t[..., :half]
    t2 = t[..., half:ROPE_DIM]
    return jnp.concatenate([t1 * cos - t2 * sin, t2 * cos + t1 * sin, t[..., ROPE_DIM:]], axis=-1)


def chunked_spatial_gating(u, v, ln_g, ln_b, w_s, b_s):
    bsz, seq, width = u.shape
    u = jax.nn.gelu(u)
    vf = jax.nn.gelu(v).astype(jnp.float32)
    mu = jnp.mean(vf, axis=-1, keepdims=True)
    var = jnp.mean(jnp.square(vf - mu), axis=-1, keepdims=True)
    v = ((vf - mu) * lax.rsqrt(var + NORM_EPS) * ln_g + ln_b).astype(u.dtype)
    nc = seq // SGU_CHUNK
    v = v.reshape(bsz, nc, SGU_CHUNK, SGU_GROUPS, SGU_GROUP_DIM)
    w = w_s * jnp.tril(jnp.ones((SGU_CHUNK, SGU_CHUNK), w_s.dtype))
    s = jnp.einsum("gts,bnsgc->bntgc", w, v) + b_s.T[None, None, :, :, None]
    return u * s.reshape(bsz, seq, width)


def moba_attention(q, k, v):
    bsz, nh, seq, hd = q.shape
    nb = -(-seq // MOBA_BLOCK)
    pad = nb * MOBA_BLOCK - seq
    kp = jnp.pad(k, ((0, 0), (0, 0), (0, pad), (0, 0)))
    vp = jnp.pad(v, ((0, 0), (0, 0), (0, pad), (0, 0)))
    kb = kp.reshape(bsz, nh, nb, MOBA_BLOCK, hd)
    vb = vp.reshape(bsz, nh, nb, MOBA_BLOCK, hd)
    k_mean = jnp.mean(kb.astype(jnp.float32), axis=3)
    topk = min(MOBA_TOPK, nb)
    n_sel = topk * MOBA_BLOCK
    n_steps = seq // MOBA_Q_BLOCK
    q_steps = q.reshape(bsz, nh, n_steps, MOBA_Q_BLOCK, hd).transpose(2, 0, 1, 3, 4)
    b_idx = jnp.arange(bsz)[:, None, None, None]
    h_idx = jnp.arange(nh)[None, :, None, None]
    blk_ids = jnp.arange(nb)
    scale = hd ** -0.5

    def step(args):
        i, qc = args
        q0 = i * MOBA_Q_BLOCK
        own = q0 // MOBA_BLOCK
        q_pos = q0 + jnp.arange(MOBA_Q_BLOCK)
        gate = jnp.einsum("bhqd,bhnd->bhqn", qc.astype(jnp.float32), k_mean)
        gate = jnp.where(blk_ids < own, gate, NEG_INF)
        _, sel = lax.top_k(gate, topk)
        sel_ok = sel < own
        k_sel = kb[b_idx, h_idx, sel]
        v_sel = vb[b_idx, h_idx, sel]
        s_sel = jnp.einsum("bhqd,bhqjsd->bhqjs", qc, k_sel).astype(jnp.float32) * scale
        s_sel = jnp.where(sel_ok[..., None], s_sel, NEG_INF).reshape(bsz, nh, MOBA_Q_BLOCK, n_sel)
        k_own = lax.dynamic_slice_in_dim(kp, own * MOBA_BLOCK, MOBA_BLOCK, axis=2)
        v_own = lax.dynamic_slice_in_dim(vp, own * MOBA_BLOCK, MOBA_BLOCK, axis=2)
        s_own = jnp.einsum("bhqd,bhsd->bhqs", qc, k_own).astype(jnp.float32) * scale
        k_pos = own * MOBA_BLOCK + jnp.arange(MOBA_BLOCK)
        s_own = jnp.where(k_pos[None, :] <= q_pos[:, None], s_own, NEG_INF)
        p = jax.nn.softmax(jnp.concatenate([s_sel, s_own], axis=-1), axis=-1)
        p_sel = p[..., :n_sel].reshape(bsz, nh, MOBA_Q_BLOCK, topk, MOBA_BLOCK).astype(v.dtype)
        p_own = p[..., n_sel:].astype(v.dtype)
        return (jnp.einsum("bhqjs,bhqjsd->bhqd", p_sel, v_sel)
                + jnp.einsum("bhqs,bhsd->bhqd", p_own, v_own))

    out = lax.map(step, (jnp.arange(n_steps), q_steps))
    return out.transpose(1, 2, 0, 3, 4).reshape(bsz, nh, seq, hd)


def causal_depthwise_conv(x, w):
    return lax.conv_general_dilated(
        x, w[:, None, :].astype(x.dtype), window_strides=(1,), padding=[(w.shape[0] - 1, 0)],
        dimension_numbers=("NWC", "WIO", "NWC"), feature_group_count=x.shape[-1])


def ssd_chunked_scan(x, dt, a, bmat, cmat):
    bsz, seq, nh, hp = x.shape
    ng, ns = bmat.shape[2], bmat.shape[3]
    hg = nh // ng
    L = SSM_CHUNK
    nc = seq // L

    def chunks(t):
        return jnp.moveaxis(t.reshape((bsz, nc, L) + t.shape[2:]), 1, 0)

    xc = chunks(x.reshape(bsz, seq, ng, hg, hp))
    dtc = chunks(dt.reshape(bsz, seq, ng, hg))
    bc = chunks(bmat)
    cc = chunks(cmat)
    a_g = a.reshape(ng, hg)
    causal = jnp.tril(jnp.ones((L, L), bool))[None, :, :, None, None]

    def step(state, inp):
        xk, dtk, bk, ck = inp
        acum = jnp.cumsum(dtk * a_g, axis=1)
        decay = jnp.exp(jnp.where(causal, acum[:, :, None] - acum[:, None, :], -jnp.inf))
        cb = jnp.einsum("blgn,bsgn->blsg", ck, bk)
        y = jnp.einsum("blsgh,bsghp->blghp", cb[..., None] * decay * dtk[:, None], xk)
        y = y + jnp.einsum("blgn,bghpn->blghp", ck, state) * jnp.exp(acum)[..., None]
        w_end = jnp.exp(acum[:, -1:] - acum) * dtk
        state = (state * jnp.exp(acum[:, -1])[..., None, None]
                 + jnp.einsum("bsgh,bsgn,bsghp->bghpn", w_end, bk, xk))
        return state, y

    state0 = jnp.zeros((bsz, ng, hg, hp, ns), jnp.float32)
    _, y = lax.scan(step, state0, (xc, dtc, bc, cc))
    return jnp.moveaxis(y, 0, 1).reshape(bsz, seq, nh, hp)


def mamba2_group(z, xbc, dt_raw, conv_w, conv_b, dt_bias, a_log, d_skip, ssm_norm):
    bsz, seq, _ = z.shape
    xbc = jax.nn.silu(causal_depthwise_conv(xbc, conv_w) + conv_b)
    gn = SSM_GROUPS * SSM_STATE
    xs = xbc[..., :SSM_WIDTH].astype(jnp.float32).reshape(bsz, seq, SSM_HEADS, SSM_HEAD_DIM)
    bm = xbc[..., SSM_WIDTH:SSM_WIDTH + gn].astype(jnp.float32).reshape(bsz, seq, SSM_GROUPS, SSM_STATE)
    cm = xbc[..., SSM_WIDTH + gn:].astype(jnp.float32).reshape(bsz, seq, SSM_GROUPS, SSM_STATE)
    dt = jax.nn.softplus(dt_raw.astype(jnp.float32) + dt_bias.astype(jnp.float32))
    a = -jnp.exp(a_log.astype(jnp.float32))
    y = ssd_chunked_scan(xs, dt, a, bm, cm)
    y = y + d_skip.astype(jnp.float32)[:, None] * xs
    y = y.reshape(bsz, seq, SSM_WIDTH) * jax.nn.silu(z.astype(jnp.float32))
    yg = y.reshape(bsz, seq, SSM_GROUPS, SSM_WIDTH // SSM_GROUPS)
    yg = yg * lax.rsqrt(jnp.mean(yg * yg, axis=-1, keepdims=True) + NORM_EPS)
    return (yg.reshape(bsz, seq, SSM_WIDTH) * ssm_norm.astype(jnp.float32)).astype(z.dtype)


def hybrid_mixer(h, cos, sin, w_in, w_out, sgu_ln_g, sgu_ln_b, sgu_w, sgu_b,
                 conv_w, conv_b, dt_bias, a_log, d_skip, ssm_norm):
    bsz, seq, _ = h.shape
    proj = h @ w_in
    splits = [int(s) for s in np.cumsum(IN_SECTIONS)[:-1]]
    u, v, q, k, va, z, xbc, dt_raw = jnp.split(proj, splits, axis=-1)
    y_a = chunked_spatial_gating(u, v, sgu_ln_g, sgu_ln_b, sgu_w, sgu_b)
    heads = lambda t: t.reshape(bsz, seq, ATT_HEADS, ATT_HEAD_DIM).transpose(0, 2, 1, 3)
    qh = apply_partial_rotary(heads(q), cos, sin)
    kh = apply_partial_rotary(heads(k), cos, sin)
    y_b = moba_attention(qh, kh, heads(va)).transpose(0, 2, 1, 3).reshape(bsz, seq, ATT_WIDTH)
    y_c = mamba2_group(z, xbc, dt_raw, conv_w, conv_b, dt_bias, a_log, d_skip, ssm_norm)
    y = jnp.concatenate([y_a, y_b.astype(h.dtype), y_c], axis=-1)
    return y @ w_out


def hierarchical_moe(h, w_coarse, b_coarse, w_fine, b_fine, w_gate, w_up, w_down):
    bsz, seq, d = h.shape
    n_tok = bsz * seq
    ht = h.reshape(n_tok, d)
    p_group = jax.nn.softmax((ht @ w_coarse + b_coarse).astype(jnp.float32), axis=-1)
    g_prob, g_idx = lax.top_k(p_group, 1)
    fine_all = jnp.einsum("td,gde->tge", ht, w_fine) + b_fine
    fine = jnp.take_along_axis(fine_all, g_idx[:, :, None], axis=1)[:, 0]
    p_exp = jax.nn.softmax(fine.astype(jnp.float32), axis=-1)
    e_prob, e_idx = lax.top_k(p_exp, MOE_TOPK)
    gate = g_prob * e_prob / jnp.sum(e_prob, axis=-1, keepdims=True)
    expert = g_idx * MOE_EXPERTS_PER_GROUP + e_idx
    n_asg = n_tok * MOE_TOPK
    flat_e = expert.reshape(n_asg).astype(jnp.int32)
    order = jnp.argsort(flat_e)
    s_e = flat_e[order]
    s_tok = (order // MOE_TOPK).astype(jnp.int32)
    s_gate = gate.reshape(n_asg)[order]
    counts = jnp.bincount(flat_e, length=MOE_EXPERTS)
    start = jnp.cumsum(counts) - counts
    pcounts = (counts + MOE_BLOCK - 1) // MOE_BLOCK * MOE_BLOCK
    pend = jnp.cumsum(pcounts)
    pstart = pend - pcounts
    dest = pstart[s_e] + jnp.arange(n_asg) - start[s_e]
    n_rows = -(-n_asg // MOE_BLOCK) * MOE_BLOCK + MOE_EXPERTS * MOE_BLOCK
    n_blocks = n_rows // MOE_BLOCK
    row_tok = jnp.full((n_rows,), n_tok, jnp.int32).at[dest].set(s_tok)
    row_gate = jnp.zeros((n_rows,), jnp.float32).at[dest].set(s_gate)
    blk_exp = jnp.minimum(jnp.searchsorted(pend, jnp.arange(n_blocks) * MOE_BLOCK, side="right"),
                          MOE_EXPERTS - 1)
    h_pad = jnp.concatenate([ht, jnp.zeros((1, d), ht.dtype)], axis=0)

    def expert_block(args):
        e, toks = args
        xb = h_pad[toks]
        hid = jax.nn.silu(xb @ w_gate[e]) * (xb @ w_up[e])
        return hid @ w_down[e]

    y_rows = lax.map(expert_block, (blk_exp, row_tok.reshape(n_blocks, MOE_BLOCK))).reshape(n_rows, d)
    out = jnp.zeros((n_tok + 1, d), h.dtype).at[row_tok].add(y_rows * row_gate[:, None].astype(h.dtype))
    return out[:n_tok].reshape(bsz, seq, d)


def setup_inputs(seed: int = 0) -> dict:
    key = jax.random.key(seed)
    ks = jax.random.split(key, 26)
    f32 = jnp.float32

    def nrm(k, shape, s):
        return jax.random.normal(k, shape, f32) * s

    L, D = DEPTH, D_MODEL
    dt0 = jnp.exp(jax.random.uniform(ks[14], (L, SSM_HEADS), f32) * (np.log(0.1) - np.log(0.001)) + np.log(0.001))
    dt0 = jnp.maximum(dt0, 1e-4)
    return {
        "x": nrm(ks[0], (BATCH, SEQ, D), 1.0),
        "c": nrm(ks[1], (BATCH, D), 1.0),
        "norm1": 1.0 + nrm(ks[2], (L, D), 0.02),
        "norm2": 1.0 + nrm(ks[3], (L, D), 0.02),
        "w_ada": nrm(ks[4], (L, D, 6 * D), 0.5 * D ** -0.5),
        "b_ada": nrm(ks[5], (L, 6 * D), 0.02),
        "w_in": nrm(ks[6], (L, D, D_IN_PROJ), D ** -0.5),
        "w_out": nrm(ks[7], (L, D_MIX, D), D_MIX ** -0.5),
        "sgu_ln_g": 1.0 + nrm(ks[8], (L, SGU_WIDTH), 0.02),
        "sgu_ln_b": nrm(ks[9], (L, SGU_WIDTH), 0.02),
        "sgu_w": nrm(ks[10], (L, SGU_GROUPS, SGU_CHUNK, SGU_CHUNK), SGU_CHUNK ** -0.5),
        "sgu_b": 1.0 + nrm(ks[11], (L, SGU_GROUPS, SGU_CHUNK), 0.02),
        "conv_w": nrm(ks[12], (L, SSM_CONV, SSM_CONV_DIM), SSM_CONV ** -0.5),
        "conv_b": nrm(ks[13], (L, SSM_CONV_DIM), 0.02),
        "dt_bias": dt0 + jnp.log(-jnp.expm1(-dt0)),
        "a_log": jnp.log(jax.random.uniform(ks[15], (L, SSM_HEADS), f32, 1.0, 16.0)),
        "d_skip": 1.0 + nrm(ks[16], (L, SSM_HEADS), 0.02),
        "ssm_norm": 1.0 + nrm(ks[17], (L, SSM_WIDTH), 0.02),
        "w_coarse": nrm(ks[18], (L, D, MOE_GROUPS), D ** -0.5),
        "b_coarse": nrm(ks[19], (L, MOE_GROUPS), 0.01),
        "w_fine": nrm(ks[20], (L, MOE_GROUPS, D, MOE_EXPERTS_PER_GROUP), D ** -0.5),
        "b_fine": nrm(ks[21], (L, MOE_GROUPS, MOE_EXPERTS_PER_GROUP), 0.01),
        "w_gate": nrm(ks[22], (L, MOE_EXPERTS, D, MOE_FF), D ** -0.5),
        "w_up": nrm(ks[23], (L, MOE_EXPERTS, D, MOE_FF), D ** -0.5),
        "w_down": nrm(ks[24], (L, MOE_EXPERTS, MOE_FF, D), MOE_FF ** -0.5),
        "final_norm": 1.0 + nrm(ks[25], (D,), 0.02),
    }


def reference(x, c, norm1, norm2, w_ada, b_ada, w_in, w_out, sgu_ln_g, sgu_ln_b, sgu_w, sgu_b,
              conv_w, conv_b, dt_bias, a_log, d_skip, ssm_norm, w_coarse, b_coarse, w_fine, b_fine,
              w_gate, w_up, w_down, final_norm):
    bsz, seq, _ = x.shape
    cos, sin = rotary_tables(seq)
    c_act = jax.nn.silu(c)
    for l in range(DEPTH):
        mod = (c_act @ w_ada[l] + b_ada[l]).reshape(bsz, 6, D_MODEL)
        sh1, sc1, g1, sh2, sc2, g2 = [mod[:, i, None, :] for i in range(6)]
        h = rms_norm(x, norm1[l]) * (1.0 + sc1) + sh1
        x = x + g1 * hybrid_mixer(h, cos, sin, w_in[l], w_out[l], sgu_ln_g[l], sgu_ln_b[l], sgu_w[l], sgu_b[l],
                                  conv_w[l], conv_b[l], dt_bias[l], a_log[l], d_skip[l], ssm_norm[l])
        h = rms_norm(x, norm2[l]) * (1.0 + sc2) + sh2
        x = x + g2 * hierarchical_moe(h, w_coarse[l], b_coarse[l], w_fine[l], b_fine[l],
                                      w_gate[l], w_up[l], w_down[l])
    return rms_norm(x, final_norm)
```

```python
import numpy as np
from contextlib import ExitStack
import concourse.bass as bass
import concourse.mybir as mybir

F32 = mybir.dt.float32
BF16 = mybir.dt.bfloat16
I32 = mybir.dt.int32
U32 = mybir.dt.uint32
AF = mybir.ActivationFunctionType
ALU = mybir.AluOpType
AX = mybir.AxisListType


class Buf:
    __slots__ = ("t", "name", "w", "rd", "dw_sem", "dw_cnt", "dr_sem", "dr_cnt", "dw_base", "dr_base")

    def __init__(self, t, name):
        self.t = t
        self.name = name
        self.w = None
        self.rd = {}
        self.dw_sem = None
        self.dw_cnt = 0
        self.dr_sem = None
        self.dr_cnt = 0
        self.dw_base = 0
        self.dr_base = 0

    def __getitem__(self, idx):
        return self.t[idx]


class DBuf:
    def __init__(self, t, name):
        self.t = t
        self.name = name
        self.pw = {}
        self.pr = {}

    def __getitem__(self, idx):
        return self.t[idx]


class K:
    ENG = ("pe", "dve", "act", "pool", "sp")

    def __init__(self, nc, same_engine_sync=True):
        self.nc = nc
        self.es = ExitStack()
        self.E = {"pe": nc.tensor, "dve": nc.vector, "act": nc.scalar, "pool": nc.gpsimd, "sp": nc.sync}
        self.sem = {e: self.es.enter_context(nc.semaphore("sem_" + e)) for e in self.ENG}
        self.cnt = {e: 0 for e in self.ENG}
        self.seen = {}
        self.same = same_engine_sync
        self.cur = self.es
        self.dma_all = {}
        self.dbufs = []
        self.sem_pool = []
        self.scope_bufs = [[]]
        self.uid = 0
        self.nsem = len(self.ENG)

    def sbuf(self, name, shape, dt):
        self.uid += 1
        name = name + "_" + str(self.uid)
        t = self.cur.enter_context(self.nc.sbuf_tensor(name, list(shape), dt))
        b = Buf(t, name)
        self.scope_bufs[-1].append(b)
        return b

    def psum(self, name, shape, dt=F32):
        self.uid += 1
        name = name + "_" + str(self.uid)
        t = self.cur.enter_context(self.nc.psum_tensor(name, list(shape), dt))
        return Buf(t, name)

    def barrier(self):
        for e in self.ENG:
            for e2 in self.ENG:
                if e2 != e and e2 != "sp" and self.cnt[e2]:
                    self._wait(e, self.sem[e2], self.cnt[e2], "E" + e2)
            for key, (sem, val) in self.dma_all.items():
                self._wait(e, sem, val, key)

    def scope(self):
        from contextlib import contextmanager

        @contextmanager
        def _s():
            prev = self.cur
            st = ExitStack()
            self.cur = st
            self.scope_bufs.append([])
            try:
                yield
            finally:
                self.barrier()
                for b in self.scope_bufs.pop():
                    if b.dw_sem is not None:
                        self.sem_pool.append((b.dw_sem, b.dw_base + 16 * b.dw_cnt))
                    if b.dr_sem is not None:
                        self.sem_pool.append((b.dr_sem, b.dr_base + 16 * b.dr_cnt))
                self.dma_all = {}
                for d in self.dbufs:
                    d.pw = {}
                    d.pr = {}
                self.seen = {kk: v for kk, v in self.seen.items() if kk[1].startswith("E")}
                self.cur = prev
                st.close()
        return _s()

    def dram(self, name, shape, dt, kind="Internal"):
        if kind == "Internal":
            self.uid += 1
            name = name + "_" + str(self.uid)
        t = self.nc.dram_tensor(name, list(shape), dt, kind=kind).ap()
        d = DBuf(t, name)
        self.dbufs.append(d)
        return d

    def view(self, buf_t, name):
        return Buf(buf_t, name)

    def _newsem(self, name):
        if self.sem_pool:
            return self.sem_pool.pop()
        self.nsem += 1
        return self.es.enter_context(self.nc.semaphore("s" + str(self.nsem))), 0

    def _wait(self, eng, sem, val, key):
        if val <= 0:
            return
        k = (eng, key)
        if self.seen.get(k, 0) >= val:
            return
        self.seen[k] = val
        self.E[eng].wait_ge(sem, val)

    def _wait_eng(self, eng, dep):
        if dep is None:
            return
        e2, c = dep
        if e2 == eng and (not self.same or eng in ("pe", "sp")):
            return
        self._wait(eng, self.sem[e2], c, "E" + e2)

    def _deps_read(self, eng, b):
        self._wait_eng(eng, b.w)
        if b.dw_cnt:
            self._wait(eng, b.dw_sem, b.dw_base + 16 * b.dw_cnt, "DW" + b.name)

    def _deps_write(self, eng, b):
        self._wait_eng(eng, b.w)
        for e2, c in b.rd.items():
            if e2 != eng:
                self._wait_eng(eng, (e2, c))
        if b.dw_cnt:
            self._wait(eng, b.dw_sem, b.dw_base + 16 * b.dw_cnt, "DW" + b.name)
        if b.dr_cnt:
            self._wait(eng, b.dr_sem, b.dr_base + 16 * b.dr_cnt, "DR" + b.name)

    def op(self, eng, fn, reads=(), writes=(), inc=True):
        for b in reads:
            self._deps_read(eng, b)
        for b in writes:
            self._deps_write(eng, b)
        ins = fn(self.E[eng])
        if inc:
            self.cnt[eng] += 1
            ins.then_inc(self.sem[eng], 1)
            c = self.cnt[eng]
        else:
            c = self.cnt[eng] + 1
        for b in reads:
            b.rd[eng] = c
        for b in writes:
            b.w = (eng, c)
            b.rd = {}
        return ins

    def dma(self, q, out_ap, in_ap, src=None, dst=None, **kw):
        s_sb = isinstance(src, Buf)
        d_sb = isinstance(dst, Buf)
        assert s_sb != d_sb, "exactly one side must be an SBUF Buf"
        if d_sb:
            self._deps_write(q, dst)
            if isinstance(src, DBuf):
                for sname, (sem, val) in src.pw.items():
                    self._wait(q, sem, val, sname)
        else:
            self._deps_read(q, src)
            if isinstance(dst, DBuf):
                for d in (dst.pw, dst.pr):
                    for sname, (sem, val) in d.items():
                        self._wait(q, sem, val, sname)
        ins = self.E[q].dma_start(out=out_ap, in_=in_ap, **kw)
        if d_sb:
            if dst.dw_sem is None:
                dst.dw_sem, dst.dw_base = self._newsem("dw_" + dst.name)
            dst.dw_cnt += 1
            ins.then_inc(dst.dw_sem, 16)
            self.dma_all["DW" + dst.name] = (dst.dw_sem, dst.dw_base + 16 * dst.dw_cnt)
            dst.rd = {}
            if isinstance(src, DBuf):
                src.pr["DW" + dst.name] = (dst.dw_sem, dst.dw_base + 16 * dst.dw_cnt)
        else:
            if src.dr_sem is None:
                src.dr_sem, src.dr_base = self._newsem("dr_" + src.name)
            src.dr_cnt += 1
            ins.then_inc(src.dr_sem, 16)
            self.dma_all["DR" + src.name] = (src.dr_sem, src.dr_base + 16 * src.dr_cnt)
            if isinstance(dst, DBuf):
                dst.pw["DR" + src.name] = (src.dr_sem, src.dr_base + 16 * src.dr_cnt)
                dst.pr = {}
        return ins

    def indirect(self, q, sb, dram, idxbuf, out_ap, in_ap, out_idx=None, in_idx=None, bound=None):
        g = self.E["pool"]
        self._deps_read("pool", idxbuf)
        if in_idx is not None:
            dst = sb
            self._deps_write("pool", dst)
            if isinstance(dram, DBuf):
                for sname, (sem, val) in dram.pw.items():
                    self._wait("pool", sem, val, sname)
            ins = g.indirect_dma_start(out=out_ap, out_offset=None, in_=in_ap, in_offset=bass.IndirectOffsetOnAxis(ap=in_idx, axis=0))
            if dst.dw_sem is None:
                dst.dw_sem, dst.dw_base = self._newsem("dw_" + dst.name)
            dst.dw_cnt += 1
            ins.then_inc(dst.dw_sem, 16)
            self.dma_all["DW" + dst.name] = (dst.dw_sem, dst.dw_base + 16 * dst.dw_cnt)
            dst.rd = {}
            if isinstance(dram, DBuf):
                dram.pr["DW" + dst.name] = (dst.dw_sem, dst.dw_base + 16 * dst.dw_cnt)
        else:
            srcb, dd = dram, sb
            self._deps_read("pool", srcb)
            for sname, (sem, val) in dd.pr.items():
                self._wait("pool", sem, val, sname)
            kw = {}
            if bound is not None:
                kw = dict(bounds_check=bound, oob_is_err=False)
            ins = g.indirect_dma_start(out=out_ap, out_offset=bass.IndirectOffsetOnAxis(ap=out_idx, axis=0), in_=in_ap, in_offset=None, **kw)
            if srcb.dr_sem is None:
                srcb.dr_sem, srcb.dr_base = self._newsem("dr_" + srcb.name)
            srcb.dr_cnt += 1
            ins.then_inc(srcb.dr_sem, 16)
            self.dma_all["DR" + srcb.name] = (srcb.dr_sem, srcb.dr_base + 16 * srcb.dr_cnt)
            dd.pw["DR" + srcb.name] = (srcb.dr_sem, srcb.dr_base + 16 * srcb.dr_cnt)
        idxbuf.rd["pool"] = self.cnt["pool"] + 1
        return ins

    def finish(self, outs=()):
        for d in outs:
            for sname, (sem, val) in d.pw.items():
                self._wait("sp", sem, val, sname)
        for e in self.ENG:
            if e != "sp" and self.cnt[e]:
                self._wait("sp", self.sem[e], self.cnt[e], "E" + e)

    def close(self):
        self.es.close()


import os
DBG = ''


FR = mybir.dt.float32r
NEG = -30000.0


def emit_consts(k):
    C = {}
    C["ones32"] = k.sbuf("c_ones32", [128, 128], F32)
    k.op("pool", lambda e: e.memset(C["ones32"][:], 1.0), writes=[C["ones32"]])
    C["ident32"] = k.sbuf("c_ident32", [128, 128], F32)
    k.op("pool", lambda e: e.memset(C["ident32"][:], 0.0), writes=[C["ident32"]])
    k.op("pool", lambda e: e.affine_select(out=C["ident32"][:], in_=C["ident32"][:], pattern=[[-1, 128]], compare_op=ALU.not_equal,
                                            fill=1.0, base=0, channel_multiplier=1), reads=[C["ident32"]], writes=[C["ident32"]])
    C["ident16"] = k.sbuf("c_ident16", [128, 128], BF16)
    k.op("dve", lambda e: e.tensor_copy(out=C["ident16"][:], in_=C["ident32"][:]), reads=[C["ident32"]], writes=[C["ident16"]])
    C["U32"] = k.sbuf("c_U32", [128, 128], F32)
    k.op("pool", lambda e: e.affine_select(out=C["U32"][:], in_=C["ones32"][:], pattern=[[1, 128]], compare_op=ALU.is_ge,
                                            fill=0.0, base=0, channel_multiplier=-1), reads=[C["ones32"]], writes=[C["U32"]])
    C["Lst32"] = k.sbuf("c_Lst32", [128, 128], F32)
    k.op("pool", lambda e: e.affine_select(out=C["Lst32"][:], in_=C["ones32"][:], pattern=[[1, 128]], compare_op=ALU.is_ge,
                                            fill=0.0, base=-1, channel_multiplier=-1), reads=[C["ones32"]], writes=[C["Lst32"]])
    z32 = k.sbuf("c_z32", [128, 128], F32)
    k.op("pool", lambda e: e.memset(z32[:], 0.0), writes=[z32])
    caus32 = k.sbuf("c_caus32", [128, 128], F32)
    k.op("pool", lambda e: e.affine_select(out=caus32[:], in_=z32[:], pattern=[[1, 128]], compare_op=ALU.is_ge,
                                            fill=NEG, base=0, channel_multiplier=-1), reads=[z32], writes=[caus32])
    C["caus16"] = k.sbuf("c_caus16", [128, 128], BF16)
    k.op("dve", lambda e: e.tensor_copy(out=C["caus16"][:], in_=caus32[:]), reads=[caus32], writes=[C["caus16"]])
    C["sel16"] = k.sbuf("c_sel16", [32, 32, 128], BF16)
    with k.scope():
      sel32 = k.sbuf("c_sel32", [32, 32, 128], F32)
      k.op("pool", lambda e: e.memset(sel32[:], 1.0), writes=[sel32])
      k.op("pool", lambda e: e.affine_select(out=sel32[:], in_=sel32[:], pattern=[[-1, 32], [0, 128]], compare_op=ALU.is_equal,
                                            fill=0.0, base=0, channel_multiplier=1), reads=[sel32], writes=[sel32])
      k.op("dve", lambda e: e.tensor_copy(out=C["sel16"][:], in_=sel32[:]), reads=[sel32], writes=[C["sel16"]])
    return C


def emit_attn(k, C, T, qT_d, kT_d, v_d, cos_d, sin_d, y_d, y_col0, nheads, src=None, dstbuf=None):
    NBLK = T // 256
    NKT = T // 128
    RC = min(T, 2048)
    scale = 128 ** -0.5
    q32 = k.sbuf("a_q32", [128, T], F32); k32 = k.sbuf("a_k32", [128, T], F32)
    q16 = k.sbuf("a_q16", [128, T], BF16); k16 = k.sbuf("a_k16", [128, T], BF16)
    swp = k.sbuf("a_swp", [32, RC], F32); cs = k.sbuf("a_cos", [32, RC], F32); sn = k.sbuf("a_sin", [32, RC], F32)
    rt = k.sbuf("a_rt", [32, RC], F32)
    V1 = k.sbuf("a_V1", [128, NKT, 129], BF16)
    kmean = k.sbuf("a_kmean", [128, NBLK], F32)
    gate = k.sbuf("a_gate", [128, 32], F32)
    mx8 = k.sbuf("a_mx8", [128, 8], F32)
    biasq = k.sbuf("a_biasq", [128, 32], F32)
    maskT = k.sbuf("a_maskT", [32, 256], BF16)
    PT = [k.sbuf(f"a_PT{i}", [128, 256], BF16) for i in range(3)]
    yo = [k.sbuf(f"a_yo{i}", [128, 128], F32) for i in range(2)]
    rec = k.sbuf("a_rec", [128, 1], F32)
    ps_s = [k.psum(f"a_ps_s{i}", [128, 256]) for i in range(2)]
    ps_o = [k.psum(f"a_ps_o{i}", [128, 129]) for i in range(4)]
    ps_g = k.psum("a_ps_g", [128, 32])
    ps_t = k.psum("a_ps_t", [32, 128])
    k.op("pool", lambda e: e.memset(gate[:], -1e30), writes=[gate])
    k.op("pool", lambda e: e.memset(V1[:, :, 128:129], 1.0), writes=[V1])
    si = 0; oi = 0; yi = 0
    for h in range(nheads):
        for (dst32, srcd) in ((q32, qT_d), (k32, kT_d)):
            k.dma("sp", dst32[:], srcd[h * 128:(h + 1) * 128, :], src=src, dst=dst32)
            for c0 in range(0, T, RC):
                k.dma("sp", swp[0:16, :], srcd[h * 128 + 16:h * 128 + 32, c0:c0 + RC], src=src, dst=swp)
                k.dma("sp", swp[16:32, :], srcd[h * 128:h * 128 + 16, c0:c0 + RC], src=src, dst=swp)
                k.dma("sp", cs[:], cos_d[:, c0:c0 + RC], dst=cs)
                k.dma("sp", sn[:], sin_d[:, c0:c0 + RC], dst=sn)
                k.op("dve", lambda e: e.tensor_tensor(out=rt[:], in0=dst32[0:32, c0:c0 + RC], in1=cs[:], op=ALU.mult), reads=[dst32, cs], writes=[rt])
                k.op("dve", lambda e: e.tensor_tensor(out=swp[:], in0=swp[:], in1=sn[:], op=ALU.mult), reads=[swp, sn], writes=[swp])
                k.op("dve", lambda e: e.tensor_tensor(out=dst32[0:32, c0:c0 + RC], in0=rt[:], in1=swp[:], op=ALU.add), reads=[rt, swp], writes=[dst32])
        k.op("act", lambda e: e.copy(out=q16[:], in_=q32[:]), reads=[q32], writes=[q16])
        k.op("act", lambda e: e.copy(out=k16[:], in_=k32[:]), reads=[k32], writes=[k16])
        k.op("dve", lambda e: e.tensor_reduce(out=kmean[:], in_=k32[:].rearrange("p (b s) -> p b s", s=256), op=ALU.add, axis=AX.X),
             reads=[k32], writes=[kmean])
        k.op("dve", lambda e: e.tensor_scalar(out=kmean[:], in0=kmean[:], scalar1=1.0 / 256, scalar2=None, op0=ALU.mult), reads=[kmean], writes=[kmean])
        k.dma("pool", V1[:, :, 0:128], v_d[:, h * 128:(h + 1) * 128].rearrange("(n p) d -> p n d", p=128), src=src, dst=V1)
        for Q in range(NBLK):
            q0 = Q * 256
            use_mask = Q > 3
            if use_mask and True:
                for half in range(2):
                    qs = slice(q0 + half * 128, q0 + half * 128 + 128)
                    k.op("pe", lambda e: e.matmul(ps_g[:, 0:NBLK], lhsT=q32[:, qs], rhs=kmean[:, 0:NBLK], start=True, stop=True), reads=[q32, kmean], writes=[ps_g])
                    k.op("dve", lambda e: e.tensor_copy(out=gate[:, 0:Q], in_=ps_g[:, 0:Q]), reads=[ps_g], writes=[gate])
                    k.op("dve", lambda e: e.max(out=mx8[:], in_=gate[:, 0:max(Q, 8)]), reads=[gate], writes=[mx8])
                    k.op("dve", lambda e: e.tensor_scalar(out=biasq[:], in0=gate[:], scalar1=mx8[:, 2:3], scalar2=1.0, op0=ALU.is_ge, op1=ALU.subtract),
                         reads=[gate, mx8], writes=[biasq])
                    k.op("dve", lambda e: e.tensor_scalar(out=biasq[:], in0=biasq[:], scalar1=-NEG, scalar2=None, op0=ALU.mult), reads=[biasq], writes=[biasq])
                    k.op("pe", lambda e: e.transpose(out=ps_t[:], in_=biasq[:], identity=C["ident32"][:]), reads=[biasq, C["ident32"]], writes=[ps_t])
                    k.op("act", lambda e: e.copy(out=maskT[:, half * 128:(half + 1) * 128], in_=ps_t[:]), reads=[ps_t], writes=[maskT])
            po = [ps_o[(oi + i) % 4] for i in range(2)]; oi += 2
            jobs = [("past", j, kt) for j in range(Q) for kt in range(2)]
            for half in range(2):
                jobs += [("own", half, kt) for kt in range(half + 1)]
            firstjob = [None, None]; lastjob = [None, None]
            for ji, (kind, a, kt) in enumerate(jobs):
                halves = (0, 1) if kind == "past" else (a,)
                for hf in halves:
                    if firstjob[hf] is None:
                        firstjob[hf] = ji
                    lastjob[hf] = ji

            def emit_S(job):
                nonlocal si
                kind, a, kt = job
                ps = ps_s[si % 2]; pt = PT[si % 3]; si += 1
                if kind == "past":
                    kti = a * 2 + kt
                    k.op("pe", lambda e: e.matmul(ps[:], lhsT=k16[:, kti * 128:(kti + 1) * 128], rhs=q16[:, q0:q0 + 256], start=True, stop=not use_mask),
                         reads=[k16, q16], writes=[ps], inc=not use_mask)
                    if use_mask:
                        k.op("pe", lambda e: e.matmul(ps[:], lhsT=C["sel16"][:, a, :], rhs=maskT[:], start=False, stop=True), reads=[C["sel16"], maskT], writes=[ps])
                    k.op("act", lambda e: e.activation(out=pt[:], in_=ps[:], func=AF.Exp, scale=scale), reads=[ps], writes=[pt])
                else:
                    half = a
                    qs = slice(q0 + half * 128, q0 + half * 128 + 128)
                    kti = Q * 2 + kt
                    diag = (kt == half)
                    k.op("pe", lambda e: e.matmul(ps[:, 0:128], lhsT=k16[:, kti * 128:(kti + 1) * 128], rhs=q16[:, qs], start=True, stop=not diag),
                         reads=[k16, q16], writes=[ps], inc=not diag)
                    if diag:
                        k.op("pe", lambda e: e.matmul(ps[:, 0:128], lhsT=C["ident16"][:], rhs=C["caus16"][:], start=False, stop=True),
                             reads=[C["ident16"], C["caus16"]], writes=[ps])
                    k.op("act", lambda e: e.activation(out=pt[:, 0:128], in_=ps[:, 0:128], func=AF.Exp, scale=scale), reads=[ps], writes=[pt])
                return pt

            def emit_PV(ji, job, pt):
                kind, a, kt = job
                if kind == "past":
                    kti = a * 2 + kt
                    for half in range(2):
                        lastf = (lastjob[half] == ji)
                        k.op("pe", lambda e: e.matmul(po[half][:], lhsT=pt[:, half * 128:(half + 1) * 128], rhs=V1[:, kti, :], start=(firstjob[half] == ji), stop=lastf),
                             reads=[pt, V1], writes=[po[half]], inc=lastf)
                else:
                    half = a
                    kti = Q * 2 + kt
                    lastf = (lastjob[half] == ji)
                    k.op("pe", lambda e: e.matmul(po[half][:], lhsT=pt[:, 0:128], rhs=V1[:, kti, :], start=(firstjob[half] == ji), stop=lastf),
                         reads=[pt, V1], writes=[po[half]], inc=lastf)

            pts = {0: emit_S(jobs[0])}
            for ji, job in enumerate(jobs):
                if ji + 1 < len(jobs):
                    pts[ji + 1] = emit_S(jobs[ji + 1])
                emit_PV(ji, job, pts.pop(ji))
            for half in range(2):
                y = yo[yi % 2]; yi += 1
                k.op("dve", lambda e: e.reciprocal(out=rec[:], in_=po[half][:, 128:129]), reads=[po[half]], writes=[rec])
                k.op("dve", lambda e: e.tensor_scalar(out=y[:], in0=po[half][:, 0:128], scalar1=rec[:, 0:1], scalar2=None, op0=ALU.mult), reads=[po[half], rec], writes=[y])
                k.dma("sp", y_d[q0 + half * 128:q0 + half * 128 + 128, y_col0 + h * 128:y_col0 + (h + 1) * 128], y[:], src=y, dst=y_d)


FR = mybir.dt.float32r


def emit_ssd(k, C, T, xbcT_d, ptm_d, z_col0, dt_col0, prm, y_d, y_col0, groups=(0, 1), src=None):
    NCH = T // 128
    cw = k.sbuf("s_cw", [128, 6, 4], F32); cb = k.sbuf("s_cb", [128, 6, 1], F32)
    dtb = k.sbuf("s_dtb", [128, 8], F32); aneg = k.sbuf("s_aneg", [128, 8], F32); dsk = k.sbuf("s_dsk", [128, 8], F32)
    dskx = k.sbuf("s_dskx", [128, 8, 64], F32)
    nrm = k.sbuf("s_nrm", [128, 512], F32)
    cin = [k.sbuf(f"s_cin{i}", [128, 6, 131], F32) for i in range(2)]
    acc = k.sbuf("s_acc", [128, 6, 128], F32); tap = k.sbuf("s_tap", [128, 6, 128], F32)
    xc = k.sbuf("s_xc", [128, 6, 128], F32)
    xcr = k.sbuf("s_xcr", [128, 2, 128], FR)
    x_tm = k.sbuf("s_xtm", [128, 8, 64], F32)
    B_tm = k.sbuf("s_Btm", [128, 128], FR)
    dtr = k.sbuf("s_dtr", [128, 8], F32); dt = k.sbuf("s_dt", [128, 8], F32); dA = k.sbuf("s_dA", [128, 8], F32)
    dArep = k.sbuf("s_dArep", [128, 8, 128], F32)
    acum = k.sbuf("s_acum", [128, 8], F32); tot = k.sbuf("s_tot", [128, 8], F32)
    dec = k.sbuf("s_dec", [128, 8, 128], F32)
    CBm = k.sbuf("s_CBm", [128, 128], F32)
    Mt = k.sbuf("s_Mt", [128, 8, 128], FR)
    xdt = k.sbuf("s_xdt", [128, 8, 64], FR)
    ea = k.sbuf("s_ea", [128, 8], F32); wend = k.sbuf("s_wend", [128, 8], F32); etot = k.sbuf("s_etot", [128, 8], F32)
    xw = k.sbuf("s_xw", [128, 8, 64], FR)
    ST32 = k.sbuf("s_ST32", [128, 8, 64], F32); STr = k.sbuf("s_STr", [128, 8, 64], FR)
    y1 = k.sbuf("s_y1", [128, 8, 64], F32); t2 = k.sbuf("s_t2", [128, 8, 64], F32)
    zt = [k.sbuf(f"s_zt{i}", [128, 512], F32) for i in range(2)]
    junk = k.sbuf("s_junk", [128, 512], F32)
    ss = k.sbuf("s_ss", [128, 1], F32)
    yo = [k.sbuf(f"s_yo{i}", [128, 512], F32) for i in range(2)]
    p_x = k.psum("s_p_x", [128, 512]); p_b = k.psum("s_p_b", [128, 128]); p_cb = k.psum("s_p_cb", [128, 128])
    p_ac = k.psum("s_p_ac", [128, 16]); p_abc = k.psum("s_p_abc", [128, 1024])
    p_y = k.psum("s_p_y", [128, 512]); p_yi = k.psum("s_p_yi", [128, 512])
    for g in groups:
        xr0 = g * 512; br0 = 1024 + g * 128; cr0 = 1280 + g * 128
        k.dma("sp", cw[:], prm["convw"][g], dst=cw)
        k.dma("sp", cb[:], prm["convb"][g], dst=cb)
        k.dma("sp", dtb[:], prm["dtb"][:, g * 8:(g + 1) * 8], dst=dtb)
        k.dma("sp", aneg[:], prm["alog"][:, g * 8:(g + 1) * 8], dst=aneg)
        k.dma("sp", dsk[:], prm["dskip"][:, g * 8:(g + 1) * 8], dst=dsk)
        k.dma("sp", nrm[:], prm["norm"][:, g * 512:(g + 1) * 512], dst=nrm)
        k.op("act", lambda e: e.activation(out=aneg[:], in_=aneg[:], func=AF.Exp), reads=[aneg], writes=[aneg])
        k.op("dve", lambda e: e.tensor_scalar(out=aneg[:], in0=aneg[:], scalar1=-1.0, scalar2=None, op0=ALU.mult), reads=[aneg], writes=[aneg])
        k.op("dve", lambda e: e.tensor_copy(out=dskx[:], in_=dsk[:].unsqueeze(2).to_broadcast([128, 8, 64])), reads=[dsk], writes=[dskx])
        k.op("pool", lambda e: e.memset(ST32[:], 0.0), writes=[ST32])
        for c in range(NCH):
            t0 = c * 128
            ci = cin[c % 2]
            lo = 3 if c == 0 else 0
            if c == 0:
                k.op("pool", lambda e: e.memset(ci[:, :, 0:3], 0.0), writes=[ci])
            k.dma("sp", ci[:, 0:4, lo:131], xbcT_d[xr0:xr0 + 512, t0 - 3 + lo:t0 + 128].rearrange("(i p) t -> p i t", p=128), src=src, dst=ci)
            k.dma("sp", ci[:, 4, lo:131], xbcT_d[br0:br0 + 128, t0 - 3 + lo:t0 + 128], src=src, dst=ci)
            k.dma("sp", ci[:, 5, lo:131], xbcT_d[cr0:cr0 + 128, t0 - 3 + lo:t0 + 128], src=src, dst=ci)
            z_ = zt[c % 2]
            k.dma("sp", z_[:], ptm_d[t0:t0 + 128, z_col0 + g * 512:z_col0 + (g + 1) * 512], src=src, dst=z_)
            k.dma("sp", dtr[:], ptm_d[t0:t0 + 128, dt_col0 + g * 8:dt_col0 + (g + 1) * 8], src=src, dst=dtr)
            k.op("dve", lambda e: e.tensor_tensor(out=acc[:], in0=ci[:, :, 3:131], in1=cw[:, :, 3:4].to_broadcast([128, 6, 128]), op=ALU.mult), reads=[ci, cw], writes=[acc])
            k.op("dve", lambda e: e.tensor_tensor(out=acc[:], in0=acc[:], in1=cb[:].to_broadcast([128, 6, 128]), op=ALU.add), reads=[acc, cb], writes=[acc])
            for j in range(3):
                k.op("pool", lambda e: e.tensor_tensor(out=tap[:], in0=ci[:, :, j:j + 128], in1=cw[:, :, j:j + 1].to_broadcast([128, 6, 128]), op=ALU.mult), reads=[ci, cw], writes=[tap])
                k.op("dve", lambda e: e.tensor_tensor(out=acc[:], in0=acc[:], in1=tap[:], op=ALU.add), reads=[acc, tap], writes=[acc])
            k.op("act", lambda e: e.activation(out=xc[:], in_=acc[:], func=AF.Silu), reads=[acc], writes=[xc])
            k.op("dve", lambda e: e.tensor_copy(out=xcr[:], in_=xc[:, 4:6, :]), reads=[xc], writes=[xcr])
            for i in range(4):
                k.op("pe", lambda e: e.transpose(out=p_x[:, i * 128:(i + 1) * 128], in_=xc[:, i, :], identity=C["ident32"][:]), reads=[xc, C["ident32"]], writes=[p_x], inc=(i == 3))
            k.op("act", lambda e: e.copy(out=x_tm[:].rearrange("p h d -> p (h d)"), in_=p_x[:]), reads=[p_x], writes=[x_tm])
            k.op("pe", lambda e: e.transpose(out=p_b[:], in_=xc[:, 4, :], identity=C["ident32"][:]), reads=[xc, C["ident32"]], writes=[p_b])
            k.op("dve", lambda e: e.tensor_copy(out=B_tm[:], in_=p_b[:]), reads=[p_b], writes=[B_tm])
            k.op("dve", lambda e: e.tensor_tensor(out=dt[:], in0=dtr[:], in1=dtb[:], op=ALU.add), reads=[dtr, dtb], writes=[dt])
            k.op("act", lambda e: e.activation(out=dt[:], in_=dt[:], func=AF.Exp), reads=[dt], writes=[dt])
            k.op("act", lambda e: e.activation(out=dt[:], in_=dt[:], func=AF.Ln, bias=1.0), reads=[dt], writes=[dt])
            k.op("dve", lambda e: e.tensor_tensor(out=dA[:], in0=dt[:], in1=aneg[:], op=ALU.mult), reads=[dt, aneg], writes=[dA])
            k.op("pe", lambda e: e.matmul(p_ac[:, 0:8], lhsT=C["U32"][:], rhs=dA[:], start=True, stop=True), reads=[C["U32"], dA], writes=[p_ac])
            k.op("pe", lambda e: e.matmul(p_ac[:, 8:16], lhsT=C["ones32"][:], rhs=dA[:], start=True, stop=True), reads=[C["ones32"], dA], writes=[p_ac])
            k.op("dve", lambda e: e.tensor_copy(out=acum[:], in_=p_ac[:, 0:8]), reads=[p_ac], writes=[acum])
            k.op("dve", lambda e: e.tensor_copy(out=tot[:], in_=p_ac[:, 8:16]), reads=[p_ac], writes=[tot])
            k.op("dve", lambda e: e.tensor_copy(out=dArep[:], in_=dA[:].unsqueeze(2).to_broadcast([128, 8, 128])), reads=[dA], writes=[dArep])
            for h in range(8):
                k.op("pe", lambda e: e.matmul(p_abc[:, h * 128:(h + 1) * 128], lhsT=dArep[:, h, :], rhs=C["U32"][:], start=True, stop=True),
                     reads=[dArep, C["U32"]], writes=[p_abc], inc=(h == 7))
            k.op("dve", lambda e: e.tensor_tensor(out=dec[:], in0=p_abc[:].rearrange("p (h l) -> p h l", h=8), in1=acum[:].unsqueeze(2).to_broadcast([128, 8, 128]), op=ALU.subtract),
                 reads=[p_abc, acum], writes=[dec])
            k.op("dve", lambda e: e.tensor_scalar(out=dec[:], in0=dec[:], scalar1=0.0, scalar2=None, op0=ALU.min), reads=[dec], writes=[dec])
            k.op("act", lambda e: e.activation(out=dec[:], in_=dec[:], func=AF.Exp), reads=[dec], writes=[dec])
            k.op("pe", lambda e: e.matmul(p_cb[:], lhsT=xcr[:, 0, :], rhs=xcr[:, 1, :], start=True, stop=True), reads=[xcr], writes=[p_cb])
            k.op("dve", lambda e: e.tensor_tensor(out=CBm[:], in0=p_cb[:], in1=C["U32"][:], op=ALU.mult), reads=[p_cb, C["U32"]], writes=[CBm])
            k.op("dve", lambda e: e.tensor_tensor(out=Mt[:], in0=dec[:], in1=CBm[:].unsqueeze(1).to_broadcast([128, 8, 128]), op=ALU.mult), reads=[dec, CBm], writes=[Mt])
            k.op("pool", lambda e: e.tensor_tensor(out=xdt[:], in0=x_tm[:], in1=dt[:].unsqueeze(2).to_broadcast([128, 8, 64]), op=ALU.mult), reads=[x_tm, dt], writes=[xdt])
            for h in range(8):
                k.op("pe", lambda e: e.matmul(p_y[:, h * 64:(h + 1) * 64], lhsT=Mt[:, h, :], rhs=xdt[:, h, :], start=True, stop=True), reads=[Mt, xdt], writes=[p_y], inc=(h == 7))
            k.op("dve", lambda e: e.tensor_copy(out=STr[:], in_=ST32[:]), reads=[ST32], writes=[STr])
            k.op("pe", lambda e: e.matmul(p_yi[:], lhsT=xcr[:, 1, :], rhs=STr[:].rearrange("p h d -> p (h d)"), start=True, stop=True), reads=[xcr, STr], writes=[p_yi])
            k.op("act", lambda e: e.activation(out=ea[:], in_=acum[:], func=AF.Exp), reads=[acum], writes=[ea])
            k.op("dve", lambda e: e.tensor_tensor(out=y1[:], in0=p_yi[:].rearrange("p (h d) -> p h d", h=8), in1=ea[:].unsqueeze(2).to_broadcast([128, 8, 64]), op=ALU.mult), reads=[p_yi, ea], writes=[y1])
            k.op("dve", lambda e: e.tensor_tensor(out=y1[:], in0=y1[:], in1=p_y[:].rearrange("p (h d) -> p h d", h=8), op=ALU.add), reads=[y1, p_y], writes=[y1])
            k.op("pool", lambda e: e.tensor_tensor(out=t2[:], in0=x_tm[:], in1=dskx[:], op=ALU.mult), reads=[x_tm, dskx], writes=[t2])
            k.op("dve", lambda e: e.tensor_tensor(out=y1[:], in0=y1[:], in1=t2[:], op=ALU.add), reads=[y1, t2], writes=[y1])
            k.op("act", lambda e: e.activation(out=z_[:], in_=z_[:], func=AF.Silu), reads=[z_], writes=[z_])
            k.op("dve", lambda e: e.tensor_tensor(out=y1[:].rearrange("p h d -> p (h d)"), in0=y1[:].rearrange("p h d -> p (h d)"), in1=z_[:], op=ALU.mult), reads=[y1, z_], writes=[y1])
            k.op("act", lambda e: e.activation(out=junk[:], in_=y1[:].rearrange("p h d -> p (h d)"), func=AF.Square, accum_out=ss[:]), reads=[y1], writes=[junk, ss])
            k.op("dve", lambda e: e.tensor_scalar(out=ss[:], in0=ss[:], scalar1=1.0 / 512, scalar2=1e-6, op0=ALU.mult, op1=ALU.add), reads=[ss], writes=[ss])
            k.op("act", lambda e: e.activation(out=ss[:], in_=ss[:], func=AF.Sqrt), reads=[ss], writes=[ss])
            k.op("dve", lambda e: e.reciprocal(out=ss[:], in_=ss[:]), reads=[ss], writes=[ss])
            y_ = yo[c % 2]
            k.op("dve", lambda e: e.scalar_tensor_tensor(out=y_[:], in0=y1[:].rearrange("p h d -> p (h d)"), scalar=ss[:, 0:1], in1=nrm[:], op0=ALU.mult, op1=ALU.mult), reads=[y1, ss, nrm], writes=[y_])
            k.dma("sp", y_d[t0:t0 + 128, y_col0 + g * 512:y_col0 + (g + 1) * 512], y_[:], src=y_, dst=y_d)
            k.op("dve", lambda e: e.tensor_tensor(out=wend[:], in0=tot[:], in1=acum[:], op=ALU.subtract), reads=[tot, acum], writes=[wend])
            k.op("act", lambda e: e.activation(out=wend[:], in_=wend[:], func=AF.Exp), reads=[wend], writes=[wend])
            k.op("dve", lambda e: e.tensor_tensor(out=wend[:], in0=wend[:], in1=dt[:], op=ALU.mult), reads=[wend, dt], writes=[wend])
            k.op("pool", lambda e: e.tensor_tensor(out=xw[:], in0=x_tm[:], in1=wend[:].unsqueeze(2).to_broadcast([128, 8, 64]), op=ALU.mult), reads=[x_tm, wend], writes=[xw])
            k.op("pe", lambda e: e.matmul(p_x[:], lhsT=B_tm[:], rhs=xw[:].rearrange("p h d -> p (h d)"), start=True, stop=True), reads=[B_tm, xw], writes=[p_x])
            k.op("act", lambda e: e.activation(out=etot[:], in_=tot[:], func=AF.Exp), reads=[tot], writes=[etot])
            k.op("dve", lambda e: e.tensor_tensor(out=ST32[:], in0=ST32[:], in1=etot[:].unsqueeze(2).to_broadcast([128, 8, 64]), op=ALU.mult), reads=[ST32, etot], writes=[ST32])
            k.op("dve", lambda e: e.tensor_tensor(out=ST32[:], in0=ST32[:], in1=p_x[:].rearrange("p (h d) -> p h d", h=8), op=ALU.add), reads=[ST32, p_x], writes=[ST32])


def emit_sgu(k, C, T, ptm_d, uv_col0, prm, y_d, y_col0, src=None):
    NCH = T // 128
    lng = k.sbuf("g_lng", [128, 512], F32); lnb = k.sbuf("g_lnb", [128, 512], F32)
    wT = k.sbuf("g_wT", [128, 4, 128], F32); bs = k.sbuf("g_bs", [128, 4], F32)
    uv = [k.sbuf(f"g_uv{i}", [128, 1024], F32) for i in range(2)]
    guv = k.sbuf("g_guv", [128, 1024], F32)
    st = k.sbuf("g_st", [128, 6], F32); mv = k.sbuf("g_mv", [128, 2], F32); rs = k.sbuf("g_rs", [128, 1], F32)
    vn = k.sbuf("g_vn", [128, 512], F32)
    yo = [k.sbuf(f"g_yo{i}", [128, 512], F32) for i in range(2)]
    ps = k.psum("g_ps", [128, 512])
    k.dma("sp", lng[:], prm["lng"], dst=lng); k.dma("sp", lnb[:], prm["lnb"], dst=lnb)
    k.dma("sp", wT[:], prm["wT"].rearrange("g s t -> s g t"), dst=wT); k.dma("sp", bs[:], prm["bs"], dst=bs)
    k.op("pool", lambda e: e.affine_select(out=wT[:], in_=wT[:], pattern=[[0, 4], [1, 128]], compare_op=ALU.is_ge, fill=0.0, base=0, channel_multiplier=-1),
         reads=[wT], writes=[wT])
    for c in range(NCH):
        t0 = c * 128
        uv_ = uv[c % 2]; y_ = yo[c % 2]
        k.dma("sp", uv_[:], ptm_d[t0:t0 + 128, uv_col0:uv_col0 + 1024], src=src, dst=uv_)
        k.op("act", lambda e: e.activation(out=guv[:], in_=uv_[:], func=AF.Gelu_apprx_tanh), reads=[uv_], writes=[guv])
        k.op("dve", lambda e: e.bn_stats(out=st[:], in_=guv[:, 512:1024]), reads=[guv], writes=[st])
        k.op("dve", lambda e: e.bn_aggr(out=mv[:], in_=st[:]), reads=[st], writes=[mv])
        k.op("dve", lambda e: e.tensor_scalar(out=rs[:], in0=mv[:, 1:2], scalar1=1e-6, scalar2=None, op0=ALU.add), reads=[mv], writes=[rs])
        k.op("act", lambda e: e.activation(out=rs[:], in_=rs[:], func=AF.Sqrt), reads=[rs], writes=[rs])
        k.op("dve", lambda e: e.reciprocal(out=rs[:], in_=rs[:]), reads=[rs], writes=[rs])
        k.op("dve", lambda e: e.tensor_scalar(out=vn[:], in0=guv[:, 512:1024], scalar1=mv[:, 0:1], scalar2=rs[:, 0:1], op0=ALU.subtract, op1=ALU.mult), reads=[guv, mv, rs], writes=[vn])
        k.op("dve", lambda e: e.tensor_tensor(out=vn[:], in0=vn[:], in1=lng[:], op=ALU.mult), reads=[vn, lng], writes=[vn])
        k.op("dve", lambda e: e.tensor_tensor(out=vn[:], in0=vn[:], in1=lnb[:], op=ALU.add), reads=[vn, lnb], writes=[vn])
        for g in range(4):
            k.op("pe", lambda e: e.matmul(ps[:, g * 128:(g + 1) * 128], lhsT=wT[:, g, :], rhs=vn[:, g * 128:(g + 1) * 128], start=True, stop=True), reads=[wT, vn], writes=[ps], inc=(g == 3))
        for g in range(4):
            k.op("dve", lambda e: e.scalar_tensor_tensor(out=y_[:, g * 128:(g + 1) * 128], in0=ps[:, g * 128:(g + 1) * 128], scalar=bs[:, g:g + 1], in1=guv[:, g * 128:(g + 1) * 128],
                                                          op0=ALU.add, op1=ALU.mult), reads=[ps, bs, guv], writes=[y_])
        k.dma("sp", y_d[t0:t0 + 128, y_col0:y_col0 + 512], y_[:], src=y_, dst=y_d)


FR = mybir.dt.float32r
U32 = mybir.dt.uint32
I32 = mybir.dt.int32
D = 2048


def emit_post(k, C, T, xT_d, y_d, wo_d, vec_d, wr_d, br_d, wg_d, wu_d, wd_d, out_d, final=False, fn_d=None, xsrc=None):
    NT = T // 128
    BS = 128
    SUB = BS // 128
    NB = 2 * T // BS + 32
    TT = 256
    NSUB = TT // 128
    xTv = xT_d.rearrange("(c p) t -> p c t", p=128)
    X1 = k.dram("p_X1", [D, T], F32); X1v = X1.t.rearrange("(c p) t -> p c t", p=128)
    H2 = k.dram("p_H2", [T, D], F32)
    Xd = k.dram("p_Xd", [NB * BS, D], F32)
    Yd = k.dram("p_Yd", [NB * BS, D], F32)
    outv = out_d.t.rearrange("(c p) t -> p c t", p=128)
    vt = k.sbuf("p_vt", [128, 5, 16], F32); a2 = k.sbuf("p_a2", [128, 16], F32)
    AB = k.sbuf("p_AB", [128, NT, 64], F32)
    CUM = k.sbuf("p_CUM", [128, NT, 32], F32)
    GT = k.sbuf("p_GT", [128, NT, 2], F32)
    carry = k.sbuf("p_carry", [128, 32], F32)
    k.dma("sp", vt[:], vec_d, dst=vt)
    k.op("dve", lambda e: e.scalar_tensor_tensor(out=a2[:], in0=vt[:, 2, :], scalar=1.0, in1=vt[:, 1, :], op0=ALU.add, op1=ALU.mult), reads=[vt], writes=[a2])
    k.op("pool", lambda e: e.memset(carry[:], 0.0), writes=[carry])
    with k.scope():
        ystage = k.sbuf("pa_ystage", [128, NSUB, D], F32)
        yT16 = k.sbuf("pa_yT16", [128, 16, TT], BF16)
        xacc = k.sbuf("pa_xacc", [128, 16, TT], F32)
        tmp = k.sbuf("pa_tmp", [128, 16, TT], F32)
        rstd = k.sbuf("pa_rstd", [128, TT], F32)
        wo = [k.sbuf(f"pa_wo{i}", [128, 16, 128], BF16) for i in range(3)]
        wr = k.sbuf("pa_wr", [128, 16, 36], F32); br = k.sbuf("pa_br", [128, 36], F32)
        hrow = [k.sbuf(f"pa_hrow{i}", [128, D], F32) for i in range(2)]
        lg = k.sbuf("pa_lg", [128, 36], F32)
        m4 = k.sbuf("pa_m4", [128, 1], F32); s4 = k.sbuf("pa_s4", [128, 1], F32); e4 = k.sbuf("pa_e4", [128, 4], F32)
        oh4 = k.sbuf("pa_oh4", [128, 4], F32)
        fs = k.sbuf("pa_fs", [128, 8], F32); mx8 = k.sbuf("pa_mx8", [128, 8], F32); e8 = k.sbuf("pa_e8", [128, 8], F32)
        selA = k.sbuf("pa_selA", [128, 8], F32); selB = k.sbuf("pa_selB", [128, 8], F32)
        nl1 = k.sbuf("pa_nl1", [128, 1], F32); den = k.sbuf("pa_den", [128, 1], F32); e2v = k.sbuf("pa_e2v", [128, 1], F32)
        Msum = k.sbuf("pa_Msum", [128, 32], F32)
        p_t = [k.psum(f"pa_p_t{i}", [128, 512]) for i in range(2)]
        p_m = [k.psum(f"pa_p_m{i}", [128, TT]) for i in range(2)]
        p_s = k.psum("pa_p_s", [128, TT])
        p_r = k.psum("pa_p_r", [128, 36])
        p_c = k.psum("pa_p_c", [128, 64])
        k.dma("sp", wr[:], wr_d, dst=wr); k.dma("sp", br[:], br_d, dst=br)
        ti = 0; mi = 0; hi = 0
        for t in range(T // TT):
            t0 = t * TT
            k.dma("sp", xacc[:], xTv[:, :, t0:t0 + TT], src=xsrc, dst=xacc)
            k.dma("sp", ystage[:], y_d.t[t0:t0 + TT, :].rearrange("(s p) d -> p s d", p=128), src=y_d, dst=ystage)
            for c in range(16):
                pt = p_t[ti % 2]; ti += 1
                for s_ in range(NSUB):
                    k.op("pe", lambda e: e.transpose(out=pt[:, s_ * 128:(s_ + 1) * 128], in_=ystage[:, s_, c * 128:(c + 1) * 128], identity=C["ident32"][:]),
                         reads=[ystage, C["ident32"]], writes=[pt], inc=(s_ == NSUB - 1))
                if c % 2 == 0:
                    k.op("act", lambda e: e.copy(out=yT16[:, c, :], in_=pt[:, 0:TT]), reads=[pt], writes=[yT16])
                else:
                    k.op("dve", lambda e: e.tensor_copy(out=yT16[:, c, :], in_=pt[:, 0:TT]), reads=[pt], writes=[yT16])
            for d in range(16):
                w_ = wo[mi % 3]; pm = p_m[mi % 2]; mi += 1
                k.dma("pool", w_[:], wo_d[d], dst=w_)
                for c in range(16):
                    k.op("pe", lambda e: e.matmul(pm[:], lhsT=w_[:, c, :], rhs=yT16[:, c, :], start=(c == 0), stop=(c == 15)), reads=[w_, yT16], writes=[pm], inc=(c == 15))
                k.op("dve", lambda e: e.scalar_tensor_tensor(out=xacc[:, d, :], in0=pm[:], scalar=vt[:, 0, d:d + 1], in1=xacc[:, d, :], op0=ALU.mult, op1=ALU.add),
                     reads=[pm, vt, xacc], writes=[xacc])
            k.dma("sp", X1v[:, :, t0:t0 + TT], xacc[:], src=xacc, dst=X1)
            k.op("act", lambda e: e.activation(out=tmp[:], in_=xacc[:], func=AF.Square), reads=[xacc], writes=[tmp])
            for c in range(16):
                k.op("pe", lambda e: e.matmul(p_s[:], lhsT=C["ones32"][:], rhs=tmp[:, c, :], start=(c == 0), stop=(c == 15)), reads=[C["ones32"], tmp], writes=[p_s], inc=(c == 15))
            k.op("dve", lambda e: e.tensor_scalar(out=rstd[:], in0=p_s[:], scalar1=1.0 / D, scalar2=1e-6, op0=ALU.mult, op1=ALU.add), reads=[p_s], writes=[rstd])
            k.op("act", lambda e: e.activation(out=rstd[:], in_=rstd[:], func=AF.Sqrt), reads=[rstd], writes=[rstd])
            k.op("dve", lambda e: e.reciprocal(out=rstd[:], in_=rstd[:]), reads=[rstd], writes=[rstd])
            for c in range(16):
                k.op("dve", lambda e: e.scalar_tensor_tensor(out=tmp[:, c, :], in0=xacc[:, c, :], scalar=a2[:, c:c + 1], in1=rstd[:], op0=ALU.mult, op1=ALU.mult),
                     reads=[xacc, a2, rstd], writes=[tmp])
                k.op("act", lambda e: e.activation(out=tmp[:, c, :], in_=tmp[:, c, :], func=AF.Identity, bias=vt[:, 3, c:c + 1]), reads=[tmp, vt], writes=[tmp])
            for s_ in range(NSUB):
                n = t * NSUB + s_
                ts_ = slice(s_ * 128, (s_ + 1) * 128)
                for c in range(16):
                    k.op("pe", lambda e: e.matmul(p_r[:], lhsT=tmp[:, c, ts_], rhs=wr[:, c, :], start=(c == 0), stop=(c == 15)), reads=[tmp, wr], writes=[p_r], inc=(c == 15))
                k.op("dve", lambda e: e.tensor_tensor(out=lg[:], in0=p_r[:], in1=br[:], op=ALU.add), reads=[p_r, br], writes=[lg])
                k.op("dve", lambda e: e.tensor_reduce(out=m4[:], in_=lg[:, 0:4], op=ALU.max, axis=AX.X), reads=[lg], writes=[m4])
                k.op("dve", lambda e: e.tensor_scalar(out=oh4[:], in0=lg[:, 0:4], scalar1=m4[:, 0:1], scalar2=None, op0=ALU.is_ge), reads=[lg, m4], writes=[oh4])
                k.op("dve", lambda e: e.tensor_scalar(out=e4[:], in0=lg[:, 0:4], scalar1=m4[:, 0:1], scalar2=None, op0=ALU.subtract), reads=[lg, m4], writes=[e4])
                k.op("act", lambda e: e.activation(out=e4[:], in_=e4[:], func=AF.Exp), reads=[e4], writes=[e4])
                k.op("dve", lambda e: e.tensor_reduce(out=s4[:], in_=e4[:], op=ALU.add, axis=AX.X), reads=[e4], writes=[s4])
                k.op("dve", lambda e: e.tensor_scalar(out=fs[:], in0=lg[:, 4:12], scalar1=oh4[:, 0:1], scalar2=None, op0=ALU.mult), reads=[lg, oh4], writes=[fs])
                for g in range(1, 4):
                    k.op("dve", lambda e: e.scalar_tensor_tensor(out=fs[:], in0=lg[:, 4 + 8 * g:12 + 8 * g], scalar=oh4[:, g:g + 1], in1=fs[:], op0=ALU.mult, op1=ALU.add),
                         reads=[lg, oh4, fs], writes=[fs])
                k.op("dve", lambda e: e.max(out=mx8[:], in_=fs[:]), reads=[fs], writes=[mx8])
                k.op("dve", lambda e: e.tensor_scalar(out=selA[:], in0=fs[:], scalar1=mx8[:, 0:1], scalar2=None, op0=ALU.is_ge), reads=[fs, mx8], writes=[selA])
                k.op("dve", lambda e: e.tensor_scalar(out=selB[:], in0=fs[:], scalar1=mx8[:, 1:2], scalar2=None, op0=ALU.is_ge), reads=[fs, mx8], writes=[selB])
                k.op("dve", lambda e: e.tensor_tensor(out=selB[:], in0=selB[:], in1=selA[:], op=ALU.subtract), reads=[selB, selA], writes=[selB])
                k.op("dve", lambda e: e.tensor_tensor(out=e2v[:], in0=mx8[:, 1:2], in1=mx8[:, 0:1], op=ALU.subtract), reads=[mx8], writes=[e2v])
                k.op("act", lambda e: e.activation(out=e2v[:], in_=e2v[:], func=AF.Exp), reads=[e2v], writes=[e2v])
                k.op("dve", lambda e: e.scalar_tensor_tensor(out=den[:], in0=e2v[:], scalar=1.0, in1=s4[:], op0=ALU.add, op1=ALU.mult), reads=[e2v, s4], writes=[den])
                k.op("dve", lambda e: e.reciprocal(out=GT[:, n, 0:1], in_=den[:]), reads=[den], writes=[GT])
                k.op("dve", lambda e: e.tensor_tensor(out=GT[:, n, 1:2], in0=GT[:, n, 0:1], in1=e2v[:], op=ALU.mult), reads=[GT, e2v], writes=[GT])
                for g in range(4):
                    k.op("dve", lambda e: e.tensor_scalar(out=AB[:, n, 8 * g:8 * g + 8], in0=selA[:], scalar1=oh4[:, g:g + 1], scalar2=None, op0=ALU.mult), reads=[selA, oh4], writes=[AB])
                    k.op("dve", lambda e: e.tensor_scalar(out=AB[:, n, 32 + 8 * g:40 + 8 * g], in0=selB[:], scalar1=oh4[:, g:g + 1], scalar2=None, op0=ALU.mult), reads=[selB, oh4], writes=[AB])
                k.op("dve", lambda e: e.tensor_tensor(out=Msum[:], in0=AB[:, n, 0:32], in1=AB[:, n, 32:64], op=ALU.add), reads=[AB], writes=[Msum])
                k.op("pe", lambda e: e.matmul(p_c[:, 0:32], lhsT=C["Lst32"][:], rhs=Msum[:], start=True, stop=True), reads=[C["Lst32"], Msum], writes=[p_c])
                k.op("pe", lambda e: e.matmul(p_c[:, 32:64], lhsT=C["ones32"][:], rhs=Msum[:], start=True, stop=True), reads=[C["ones32"], Msum], writes=[p_c])
                k.op("dve", lambda e: e.tensor_tensor(out=CUM[:, n, :], in0=p_c[:, 0:32], in1=carry[:], op=ALU.add), reads=[p_c, carry], writes=[CUM])
                k.op("dve", lambda e: e.tensor_tensor(out=carry[:], in0=carry[:], in1=p_c[:, 32:64], op=ALU.add), reads=[carry, p_c], writes=[carry])
                hr = hrow[hi % 2]; hi += 1
                for q4 in range(4):
                    pt = p_t[ti % 2]; ti += 1
                    for cc in range(4):
                        c = q4 * 4 + cc
                        k.op("pe", lambda e: e.transpose(out=pt[:, cc * 128:(cc + 1) * 128], in_=tmp[:, c, ts_], identity=C["ident32"][:]), reads=[tmp, C["ident32"]], writes=[pt], inc=(cc == 3))
                    if q4 % 2 == 0:
                        k.op("act", lambda e: e.copy(out=hr[:, q4 * 512:(q4 + 1) * 512], in_=pt[:]), reads=[pt], writes=[hr])
                    else:
                        k.op("dve", lambda e: e.tensor_copy(out=hr[:, q4 * 512:(q4 + 1) * 512], in_=pt[:]), reads=[pt], writes=[hr])
                k.dma("sp", H2.t[t0 + s_ * 128:t0 + (s_ + 1) * 128, :], hr[:], src=hr, dst=H2)
    pstart = k.sbuf("p_pstart", [128, 32], F32)
    IDXW = k.sbuf("p_IDXW", [128, NB, 4], I32)
    DEST = k.sbuf("p_DEST", [128, NT, 2], I32)
    with k.scope():
        pc = k.sbuf("pb_pc", [128, 32], F32); pend = k.sbuf("pb_pend", [128, 32], F32)
        onesr = k.sbuf("pb_onesr", [128, 32], F32)
        I128 = k.sbuf("pb_I128", [128, NB], F32); BE = k.sbuf("pb_BE", [128, NB], F32)
        fcp = k.sbuf("pb_fcp", [128, 4], F32); idxf = k.sbuf("pb_idxf", [128, NB, 4], F32)
        dsum = k.sbuf("pb_dsum", [128, 32], F32); dj = k.sbuf("pb_dj", [128, 32], F32); dtmp = k.sbuf("pb_dtmp", [128, NT, 2], F32)
        k.op("pool", lambda e: e.iota(I128[:], pattern=[[128, NB]], base=0, channel_multiplier=0, allow_small_or_imprecise_dtypes=True), writes=[I128])
        for ex in range(32):
            k.op("dve", lambda e: e.tensor_scalar(out=BE[:], in0=I128[:], scalar1=carry[:, ex:ex + 1], scalar2=0.0, op0=ALU.is_lt, op1=ALU.add, accum_out=pc[:, ex:ex + 1]),
                 reads=[I128, carry], writes=[BE, pc])
        k.op("dve", lambda e: e.tensor_scalar(out=pc[:], in0=pc[:], scalar1=128.0, scalar2=None, op0=ALU.mult), reads=[pc], writes=[pc])
        k.op("pool", lambda e: e.memset(onesr[:], 1.0), writes=[onesr])
        k.op("dve", lambda e: e.tensor_tensor_scan(out=pend[:], data0=onesr[:], data1=pc[:], initial=0.0, op0=ALU.mult, op1=ALU.add), reads=[onesr, pc], writes=[pend])
        k.op("dve", lambda e: e.tensor_tensor(out=pstart[:], in0=pend[:], in1=pc[:], op=ALU.subtract), reads=[pend, pc], writes=[pstart])
        k.op("pool", lambda e: e.memset(BE[:], 0.0), writes=[BE])
        for ex in range(32):
            k.op("dve", lambda e: e.scalar_tensor_tensor(out=BE[:], in0=I128[:], scalar=pend[:, ex:ex + 1], in1=BE[:], op0=ALU.is_ge, op1=ALU.add), reads=[I128, pend, BE], writes=[BE])
        k.op("dve", lambda e: e.tensor_scalar(out=BE[:], in0=BE[:], scalar1=31.0, scalar2=512.0, op0=ALU.min, op1=ALU.mult), reads=[BE], writes=[BE])
        k.op("pool", lambda e: e.iota(fcp[:], pattern=[[128, 4]], base=0, channel_multiplier=1, allow_small_or_imprecise_dtypes=True), writes=[fcp])
        k.op("dve", lambda e: e.tensor_tensor(out=idxf[:], in0=BE[:].unsqueeze(2).to_broadcast([128, NB, 4]), in1=fcp[:].unsqueeze(1).to_broadcast([128, NB, 4]), op=ALU.add), reads=[BE, fcp], writes=[idxf])
        k.op("dve", lambda e: e.tensor_copy(out=IDXW[:], in_=idxf[:]), reads=[idxf], writes=[IDXW])
        for n in range(NT):
            k.op("dve", lambda e: e.tensor_tensor(out=dsum[:], in0=CUM[:, n, :], in1=pstart[:], op=ALU.add), reads=[CUM, pstart], writes=[dsum])
            for j in range(2):
                k.op("dve", lambda e: e.tensor_tensor(out=dj[:], in0=dsum[:], in1=AB[:, n, 32 * j:32 * j + 32], op=ALU.mult), reads=[dsum, AB], writes=[dj])
                k.op("dve", lambda e: e.tensor_reduce(out=dtmp[:, n, j:j + 1], in_=dj[:], op=ALU.add, axis=AX.X), reads=[dj], writes=[dtmp])
        k.op("dve", lambda e: e.tensor_copy(out=DEST[:], in_=dtmp[:]), reads=[dtmp], writes=[DEST])
    with k.scope():
        zr = k.sbuf("pc_zero", [128, D], F32)
        k.op("pool", lambda e: e.memset(zr[:], 0.0), writes=[zr])
        for r in range(NB * SUB):
            k.dma("sp", Xd.t[r * 128:(r + 1) * 128, :], zr[:], src=zr, dst=Xd)
    with k.scope():
        hr = [k.sbuf(f"pc_hr{i}", [128, D], F32) for i in range(3)]
        for n in range(NT):
            h_ = hr[n % 3]
            k.dma("pool", h_[:], H2.t[n * 128:(n + 1) * 128, :], src=H2, dst=h_)
            for j in range(2):
                k.indirect("pool", Xd, h_, DEST, out_ap=Xd.t[:, :], out_idx=DEST[:, n, j:j + 1], in_ap=h_[:])
    with k.scope():
        xb = [k.sbuf(f"pd_xb{i}", [128, D], F32) for i in range(2)]
        xbT = [k.sbuf(f"pd_xbT{i}", [128, 16, BS], BF16) for i in range(2)]
        wg = [k.sbuf(f"pd_wg{fc}", [128, 2048], BF16) for fc in range(4)]
        wu = [k.sbuf(f"pd_wu{fc}", [128, 2048], BF16) for fc in range(4)]
        wd = [[k.sbuf(f"pd_wd{i}_{fc}", [128, 2048], BF16) for fc in range(4)] for i in range(2)]
        sg = k.sbuf("pd_sg", [128, BS], F32)
        hid = [k.sbuf(f"pd_hid{i}", [128, 4, BS], BF16) for i in range(2)]
        yb = [k.sbuf(f"pd_yb{i}", [128, D], F32) for i in range(2)]
        p_t = [k.psum(f"pd_p_t{i}", [128, 512]) for i in range(2)]
        p_g = [k.psum(f"pd_p_g{i}", [128, BS]) for i in range(2)]
        p_u = [k.psum(f"pd_p_u{i}", [128, BS]) for i in range(2)]
        p_d = [k.psum(f"pd_p_d{i}", [128, 512]) for i in range(2)]
        ti = 0; gi = 0; di = 0; xi = 0; yi = 0
        for i in range(NB):
            xT_ = xbT[i % 2]; wd_ = wd[i % 2]; hid_ = hid[i % 2]
            for fc in range(4):
                k.indirect("pool", wg[fc], None, IDXW, out_ap=wg[fc][:], in_ap=wg_d[:, :], in_idx=IDXW[:, i, fc:fc + 1])
                k.indirect("pool", wu[fc], None, IDXW, out_ap=wu[fc][:], in_ap=wu_d[:, :], in_idx=IDXW[:, i, fc:fc + 1])
            for fc in range(4):
                k.indirect("pool", wd_[fc], None, IDXW, out_ap=wd_[fc][:], in_ap=wd_d[:, :], in_idx=IDXW[:, i, fc:fc + 1])
            for s_ in range(SUB):
                x_ = xb[xi % 2]; xi += 1
                k.dma("sp", x_[:], Xd.t[i * BS + s_ * 128:i * BS + (s_ + 1) * 128, :], src=Xd, dst=x_)
                for q4 in range(4):
                    pt = p_t[ti % 2]; ti += 1
                    for cc in range(4):
                        c = q4 * 4 + cc
                        k.op("pe", lambda e: e.transpose(out=pt[:, cc * 128:(cc + 1) * 128], in_=x_[:, c * 128:(c + 1) * 128], identity=C["ident32"][:]), reads=[x_, C["ident32"]], writes=[pt], inc=(cc == 3))
                    if q4 % 2 == 0:
                        k.op("act", lambda e: e.copy(out=xT_[:, q4 * 4:(q4 + 1) * 4, s_ * 128:(s_ + 1) * 128], in_=pt[:].rearrange("p (c t) -> p c t", c=4)), reads=[pt], writes=[xT_])
                    else:
                        k.op("dve", lambda e: e.tensor_copy(out=xT_[:, q4 * 4:(q4 + 1) * 4, s_ * 128:(s_ + 1) * 128], in_=pt[:].rearrange("p (c t) -> p c t", c=4)), reads=[pt], writes=[xT_])
            for fc in range(4):
                pg = p_g[gi % 2]; pu = p_u[gi % 2]; gi += 1
                for c in range(16):
                    k.op("pe", lambda e: e.matmul(pg[:], lhsT=wg[fc][:, c * 128:(c + 1) * 128], rhs=xT_[:, c, :], start=(c == 0), stop=(c == 15)), reads=[wg[fc], xT_], writes=[pg], inc=(c == 15))
                for c in range(16):
                    k.op("pe", lambda e: e.matmul(pu[:], lhsT=wu[fc][:, c * 128:(c + 1) * 128], rhs=xT_[:, c, :], start=(c == 0), stop=(c == 15)), reads=[wu[fc], xT_], writes=[pu], inc=(c == 15))
                k.op("act", lambda e: e.activation(out=sg[:], in_=pg[:], func=AF.Silu), reads=[pg], writes=[sg])
                k.op("dve", lambda e: e.tensor_tensor(out=hid_[:, fc, :], in0=sg[:], in1=pu[:], op=ALU.mult), reads=[sg, pu], writes=[hid_])
            for s_ in range(SUB):
                y_ = yb[yi % 2]; yi += 1
                for dq in range(4):
                    pd = p_d[di % 2]; di += 1
                    for fc in range(4):
                        k.op("pe", lambda e: e.matmul(pd[:], lhsT=hid_[:, fc, s_ * 128:(s_ + 1) * 128], rhs=wd_[fc][:, dq * 512:(dq + 1) * 512], start=(fc == 0), stop=(fc == 3)), reads=[hid_, wd_[fc]], writes=[pd], inc=(fc == 3))
                    if dq % 2 == 0:
                        k.op("act", lambda e: e.copy(out=y_[:, dq * 512:(dq + 1) * 512], in_=pd[:]), reads=[pd], writes=[y_])
                    else:
                        k.op("dve", lambda e: e.tensor_copy(out=y_[:, dq * 512:(dq + 1) * 512], in_=pd[:]), reads=[pd], writes=[y_])
                k.dma("sp", Yd.t[i * BS + s_ * 128:i * BS + (s_ + 1) * 128, :], y_[:], src=y_, dst=Yd)
    with k.scope():
        y1 = [k.sbuf(f"pe_y1{i}", [128, D], F32) for i in range(2)]
        y2 = [k.sbuf(f"pe_y2{i}", [128, D], F32) for i in range(2)]
        x1 = [k.sbuf(f"pe_x1{i}", [128, 16, 128], F32) for i in range(2)]
        sq = k.sbuf("pe_sq", [128, 16, 128], F32); rs = k.sbuf("pe_rs", [128, 128], F32)
        fn17 = k.sbuf("pe_fn17", [128, 17], F32); fnv = k.sbuf("pe_fn", [128, 16], F32)
        p_t = [k.psum(f"pe_p_t{i}", [128, 512]) for i in range(2)]
        p_s = k.psum("pe_p_s", [128, 128])
        alp = k.sbuf("pe_alp", [128, 1], F32); oma = k.sbuf("pe_oma", [128, 1], F32); scl = k.sbuf("pe_scl", [128, 128], F32)
        k.dma("sp", fn17[:], fn_d, dst=fn17)
        k.op("dve", lambda e: e.tensor_copy(out=alp[:], in_=fn17[:, 16:17]), reads=[fn17], writes=[alp])
        k.op("dve", lambda e: e.tensor_scalar(out=fnv[:], in0=fn17[:, 0:16], scalar1=alp[:, 0:1], scalar2=None, op0=ALU.mult), reads=[fn17, alp], writes=[fnv])
        k.op("dve", lambda e: e.tensor_scalar(out=oma[:], in0=alp[:], scalar1=-1.0, scalar2=1.0, op0=ALU.mult, op1=ALU.add), reads=[alp], writes=[oma])
        ti = 0
        for n in range(NT):
            a_ = y1[n % 2]; b_ = y2[n % 2]; x_ = x1[n % 2]
            k.indirect("pool", a_, Yd, DEST, out_ap=a_[:], in_ap=Yd.t[:, :], in_idx=DEST[:, n, 0:1])
            k.indirect("pool", b_, Yd, DEST, out_ap=b_[:], in_ap=Yd.t[:, :], in_idx=DEST[:, n, 1:2])
            k.dma("sp", x_[:], X1v[:, :, n * 128:(n + 1) * 128], src=X1, dst=x_)
            k.op("dve", lambda e: e.tensor_scalar(out=a_[:], in0=a_[:], scalar1=GT[:, n, 0:1], scalar2=None, op0=ALU.mult), reads=[a_, GT], writes=[a_])
            k.op("dve", lambda e: e.scalar_tensor_tensor(out=a_[:], in0=b_[:], scalar=GT[:, n, 1:2], in1=a_[:], op0=ALU.mult, op1=ALU.add), reads=[b_, GT, a_], writes=[a_])
            for q4 in range(4):
                pt = p_t[ti % 2]; ti += 1
                for cc in range(4):
                    c = q4 * 4 + cc
                    k.op("pe", lambda e: e.transpose(out=pt[:, cc * 128:(cc + 1) * 128], in_=a_[:, c * 128:(c + 1) * 128], identity=C["ident32"][:]), reads=[a_, C["ident32"]], writes=[pt], inc=(cc == 3))
                for cc in range(4):
                    c = q4 * 4 + cc
                    k.op("dve", lambda e: e.scalar_tensor_tensor(out=x_[:, c, :], in0=pt[:, cc * 128:(cc + 1) * 128], scalar=vt[:, 4, c:c + 1], in1=x_[:, c, :], op0=ALU.mult, op1=ALU.add),
                         reads=[pt, vt, x_], writes=[x_])
            if True:
                k.op("act", lambda e: e.activation(out=sq[:], in_=x_[:], func=AF.Square), reads=[x_], writes=[sq])
                for c in range(16):
                    k.op("pe", lambda e: e.matmul(p_s[:], lhsT=C["ones32"][:], rhs=sq[:, c, :], start=(c == 0), stop=(c == 15)), reads=[C["ones32"], sq], writes=[p_s], inc=(c == 15))
                k.op("dve", lambda e: e.tensor_scalar(out=rs[:], in0=p_s[:], scalar1=1.0 / D, scalar2=1e-6, op0=ALU.mult, op1=ALU.add), reads=[p_s], writes=[rs])
                k.op("act", lambda e: e.activation(out=rs[:], in_=rs[:], func=AF.Sqrt), reads=[rs], writes=[rs])
                k.op("dve", lambda e: e.reciprocal(out=rs[:], in_=rs[:]), reads=[rs], writes=[rs])
                for c in range(16):
                    k.op("dve", lambda e: e.tensor_scalar(out=scl[:], in0=rs[:], scalar1=fnv[:, c:c + 1], scalar2=oma[:, 0:1], op0=ALU.mult, op1=ALU.add), reads=[rs, fnv, oma], writes=[scl])
                    k.op("dve", lambda e: e.tensor_tensor(out=x_[:, c, :], in0=x_[:, c, :], in1=scl[:], op=ALU.mult), reads=[x_, scl], writes=[x_])
            k.dma("sp", outv[:, :, n * 128:(n + 1) * 128], x_[:], src=x_, dst=out_d)


D = 2048
TM_BLOCKS = [(0, 512), (512, 512), (1024, 512), (1536, 512), (2048, 512), (2560, 16)]


def emit_pre(k, C, T, xT_d, wfm_d, wtm_d, wtm_last_d, vec_d, pfm, ptm, xsrc=None):
    TT = 512
    xTv = xT_d.rearrange("(c p) t -> p c t", p=128)
    vt = k.sbuf("r_vt", [128, 3, 16], F32); acol = k.sbuf("r_acol", [128, 16], F32)
    xt = k.sbuf("r_xt", [128, 16, TT], F32); tmp = k.sbuf("r_tmp", [128, 16, TT], F32)
    hT = k.sbuf("r_hT", [128, 16, TT], BF16); rstd = k.sbuf("r_rstd", [128, TT], F32)
    wb = [k.sbuf(f"r_wb{i}", [128, 16, 128], BF16) for i in range(3)]
    wt = [k.sbuf(f"r_wt{i}", [128, 16, 512], BF16) for i in range(2)]
    ob = [k.sbuf(f"r_ob{i}", [128, TT], F32) for i in range(3)]
    pss = k.psum("r_pss", [128, TT]); psm = [k.psum(f"r_psm{i}", [128, TT]) for i in range(4)]
    k.dma("sp", vt[:], vec_d, dst=vt)
    k.op("dve", lambda e: e.scalar_tensor_tensor(out=acol[:], in0=vt[:, 1, :], scalar=1.0, in1=vt[:, 0, :], op0=ALU.add, op1=ALU.mult), reads=[vt], writes=[acol])
    wi = 0; ti = 0
    for t in range(T // TT):
        t0 = t * TT
        k.dma("sp", xt[:], xTv[:, :, t0:t0 + TT], src=xsrc, dst=xt)
        k.op("act", lambda e: e.activation(out=tmp[:], in_=xt[:], func=AF.Square), reads=[xt], writes=[tmp])
        for c in range(16):
            k.op("pe", lambda e: e.matmul(pss[:], lhsT=C["ones32"][:], rhs=tmp[:, c, :], start=(c == 0), stop=(c == 15)), reads=[C["ones32"], tmp], writes=[pss], inc=(c == 15))
        k.op("dve", lambda e: e.tensor_scalar(out=rstd[:], in0=pss[:], scalar1=1.0 / D, scalar2=1e-6, op0=ALU.mult, op1=ALU.add), reads=[pss], writes=[rstd])
        k.op("act", lambda e: e.activation(out=rstd[:], in_=rstd[:], func=AF.Sqrt), reads=[rstd], writes=[rstd])
        k.op("dve", lambda e: e.reciprocal(out=rstd[:], in_=rstd[:]), reads=[rstd], writes=[rstd])
        for c in range(16):
            k.op("dve", lambda e: e.scalar_tensor_tensor(out=tmp[:, c, :], in0=xt[:, c, :], scalar=acol[:, c:c + 1], in1=rstd[:], op0=ALU.mult, op1=ALU.mult), reads=[xt, acol, rstd], writes=[tmp])
            k.op("act", lambda e: e.activation(out=hT[:, c, :], in_=tmp[:, c, :], func=AF.Identity, bias=vt[:, 2, c:c + 1]), reads=[tmp, vt], writes=[hT])
        for m in range(20):
            wj = wb[wi % 3]; p = psm[wi % 4]; o = ob[wi % 3]; wi += 1
            k.dma("pool", wj[:], wfm_d[m], dst=wj)
            for c in range(16):
                k.op("pe", lambda e: e.matmul(p[:], lhsT=wj[:, c, :], rhs=hT[:, c, :], start=(c == 0), stop=(c == 15)), reads=[wj, hT], writes=[p], inc=(c == 15))
            if m % 2 == 0:
                k.op("dve", lambda e: e.tensor_copy(out=o[:], in_=p[:]), reads=[p], writes=[o])
            else:
                k.op("act", lambda e: e.copy(out=o[:], in_=p[:]), reads=[p], writes=[o])
            k.dma("sp", pfm.t[m * 128:(m + 1) * 128, t0:t0 + TT], o[:], src=o, dst=pfm)
        for bi, (c0, n) in enumerate(TM_BLOCKS):
            wj = wt[ti % 2]; ti += 1
            if n == 512:
                k.dma("pool", wj[:], wtm_d[bi], dst=wj)
            else:
                k.dma("pool", wj[:, :, 0:n], wtm_last_d, dst=wj)
            for s_ in range(TT // 128):
                p = psm[wi % 4]; o = ob[wi % 3]; wi += 1
                for c in range(16):
                    k.op("pe", lambda e: e.matmul(p[:, 0:n], lhsT=hT[:, c, s_ * 128:(s_ + 1) * 128], rhs=wj[:, c, 0:n], start=(c == 0), stop=(c == 15)), reads=[hT, wj], writes=[p], inc=(c == 15))
                if s_ % 2 == 0:
                    k.op("dve", lambda e: e.tensor_copy(out=o[:, 0:n], in_=p[:, 0:n]), reads=[p], writes=[o])
                else:
                    k.op("act", lambda e: e.copy(out=o[:, 0:n], in_=p[:, 0:n]), reads=[p], writes=[o])
                k.dma("sp", ptm.t[t0 + s_ * 128:t0 + (s_ + 1) * 128, c0:c0 + n], o[:, 0:n], src=o, dst=ptm)


NCH = 16


T_SEQ = 8192
LAYER_INS = [("wfm", [20, 128, 16, 128]), ("wtm", [5, 128, 16, 512]), ("wtl", [128, 16, 16]),
             ("sgu_lng", [128, 512]), ("sgu_lnb", [128, 512]), ("sgu_wT", [4, 128, 128]), ("sgu_bs", [128, 4]),
             ("ssd_convw", [2, 128, 6, 4]), ("ssd_convb", [2, 128, 6, 1]), ("ssd_dtb", [128, 16]), ("ssd_alog", [128, 16]),
             ("ssd_dskip", [128, 16]), ("ssd_norm", [128, 1024]),
             ("wo", [16, 128, 16, 128]), ("wr", [128, 16, 36]), ("br", [128, 36]),
             ("wg", [32 * 512, 2048]), ("wu", [32 * 512, 2048]), ("wd", [32 * 512, 2048]), ("fn", [128, 17])]


def emit_mod(k, C, wada_d, cT_d, bias_d, ncols_d, VEC1, VEC2, nlayers):
    NJ = nlayers * 96
    ct = k.sbuf("m_ct", [128, 16, 2], F32); bt = k.sbuf("m_bt", [128, NJ], F32); res = k.sbuf("m_res", [128, NJ], F32)
    nct = k.sbuf("m_nct", [128, 2 * nlayers, 16], F32)
    wb = [k.sbuf(f"m_wb{i}", [128, 16, 128], F32) for i in range(3)]
    ps = [k.psum(f"m_ps{i}", [128, 2]) for i in range(2)]
    v1 = k.sbuf("m_v1", [128, nlayers, 3, 16], F32); v2 = k.sbuf("m_v2", [128, nlayers, 5, 16], F32)
    k.dma("sp", ct[:], cT_d, dst=ct); k.dma("sp", bt[:], bias_d, dst=bt); k.dma("sp", nct[:], ncols_d, dst=nct)
    k.op("act", lambda e: e.activation(out=ct[:], in_=ct[:], func=AF.Silu), reads=[ct], writes=[ct])
    for j in range(NJ):
        wj = wb[j % 3]; p = ps[j % 2]
        k.dma("sp", wj[:], wada_d[j], dst=wj)
        for c in range(16):
            k.op("pe", lambda e: e.matmul(p[:], lhsT=wj[:, c, :], rhs=ct[:, c, :], start=(c == 0), stop=(c == 15)), reads=[wj, ct], writes=[p], inc=(c == 15))
        k.op("dve", lambda e: e.tensor_tensor(out=res[:, j:j + 1], in0=p[:, 0:1], in1=bt[:, j:j + 1], op=ALU.add), reads=[p, bt], writes=[res])
    for l in range(nlayers):
        b0 = l * 96
        for (dst, slot, srcap) in ((v1, 0, nct[:, l, :]), (v1, 1, res[:, b0 + 16:b0 + 32]), (v1, 2, res[:, b0:b0 + 16]),
                                   (v2, 0, res[:, b0 + 32:b0 + 48]), (v2, 1, nct[:, nlayers + l, :]), (v2, 2, res[:, b0 + 64:b0 + 80]),
                                   (v2, 3, res[:, b0 + 48:b0 + 64]), (v2, 4, res[:, b0 + 80:b0 + 96])):
            k.op("dve", lambda e: e.tensor_copy(out=dst[:, l, slot, :], in_=srcap), reads=[res, nct], writes=[dst])
    k.dma("sp", VEC1.t.rearrange("l p a c -> p l a c"), v1[:], src=v1, dst=VEC1)
    k.dma("sp", VEC2.t.rearrange("l p a c -> p l a c"), v2[:], src=v2, dst=VEC2)


def build_fused(T=T_SEQ, nlayers=4):
    nc = bass.Bass("TRN2", target_bir_lowering=False)
    k = K(nc, same_engine_sync=True)
    A = lambda n, s: nc.dram_tensor(n, s, F32, kind="ExternalInput").ap()
    xT = A("xT", [2048, T])
    cos = A("cos", [32, T]); sin = A("sin", [32, T])
    wada = A("wada", [nlayers * 96, 128, 16, 128]); cTd = A("cT", [128, 16, 2]); biasd = A("bias", [128, nlayers * 96]); ncols = A("ncols", [128, 2 * nlayers, 16])
    L = [{n: A(f"{n}_l{l}", shp) for n, shp in LAYER_INS} for l in range(nlayers)]
    out = k.dram("xo", [2048, T], F32, kind="ExternalOutput")
    VEC1 = k.dram("vec1", [nlayers, 128, 3, 16], F32); VEC2 = k.dram("vec2", [nlayers, 128, 5, 16], F32)
    XB = [k.dram("xres", [2048, T], F32) for _ in range(2)]
    pfm = k.dram("pfm", [2560, T], F32); ptm = k.dram("ptm", [T, 2576], F32); y_d = k.dram("ymix", [T, 2048], F32)
    C = emit_consts(k)
    with k.scope():
        emit_mod(k, C, wada, cTd, biasd, ncols, VEC1, VEC2, nlayers)
    for l in range(nlayers):
        W = L[l]
        xin = xT if l == 0 else XB[(l - 1) % 2].t
        xout = out if l == nlayers - 1 else XB[l % 2]
        with k.scope():
            emit_pre(k, C, T, xin, W["wfm"], W["wtm"], W["wtl"], VEC1.t[l], pfm, ptm)
        with k.scope():
            emit_sgu(k, C, T, ptm.t, 0, {"lng": W["sgu_lng"], "lnb": W["sgu_lnb"], "wT": W["sgu_wT"], "bs": W["sgu_bs"]}, y_d, 0)
        with k.scope():
            emit_attn(k, C, T, pfm.t[0:512, :], pfm.t[512:1024, :], ptm.t[:, 1024:1536], cos, sin, y_d, 512, 4)
        with k.scope():
            emit_ssd(k, C, T, pfm.t[1024:2560, :], ptm.t, 1536, 2560,
                     {"convw": W["ssd_convw"], "convb": W["ssd_convb"], "dtb": W["ssd_dtb"], "alog": W["ssd_alog"], "dskip": W["ssd_dskip"], "norm": W["ssd_norm"]}, y_d, 1024)
        with k.scope():
            emit_post(k, C, T, xin, y_d, W["wo"], VEC2.t[l], W["wr"], W["br"], W["wg"], W["wu"], W["wd"], xout, final=True, fn_d=W["fn"])
    k.finish(outs=[out]); k.close()
    return nc


def _wl(Wc, nb=128):
    n = Wc.shape[1] // nb
    return np.ascontiguousarray(Wc.reshape(16, 128, n, nb).transpose(2, 1, 0, 3))


def _col(v):
    return v.reshape(16, 128).T


def _rep(v):
    return np.ascontiguousarray(np.broadcast_to(np.asarray(v, np.float32).reshape(1, -1), (128, v.size)))


def _wgl(w):
    return np.ascontiguousarray(w.reshape(32, 16, 128, 4, 128).transpose(0, 3, 2, 1, 4).reshape(32 * 512, 2048))


def _conv_tiles(a):
    out = []
    for g in range(2):
        rows = [a[g * 512 + i * 128:g * 512 + (i + 1) * 128] for i in range(4)] + [a[1024 + g * 128:1024 + (g + 1) * 128], a[1280 + g * 128:1280 + (g + 1) * 128]]
        out.append(np.stack(rows, axis=1))
    return np.ascontiguousarray(np.stack(out, 0)).astype(np.float32)


def _rot_tables(T):
    pos = np.arange(T, dtype=np.float32)
    inv = (np.float32(500000.0) ** (-np.arange(0, 32, 2, dtype=np.float32) / np.float32(32))).astype(np.float32)
    ang = pos[:, None] * inv[None, :]
    c, s = np.cos(ang).astype(np.float32), np.sin(ang).astype(np.float32)
    return np.ascontiguousarray(np.concatenate([c, c], 1).T), np.ascontiguousarray(np.concatenate([-s, s], 1).T)


def _layer_shared(l, p, last):
    f = lambda n: np.asarray(p[n][l], np.float32)
    w_in = f("w_in")
    W_fm = np.concatenate([w_in[:, 1024:2048], w_in[:, 3584:5120]], 1)
    W_tm = np.concatenate([w_in[:, 0:1024], w_in[:, 2048:2560], w_in[:, 2560:3584], w_in[:, 5120:5136]], 1)
    Wr = np.concatenate([f("w_coarse")] + [f("w_fine")[g] for g in range(4)], axis=1)
    return {
        "wfm": _wl(W_fm), "wtm": _wl(W_tm[:, :2560], 512), "wtl": np.ascontiguousarray(W_tm[:, 2560:].reshape(16, 128, 16).transpose(1, 0, 2)),
        "sgu_lng": _rep(f("sgu_ln_g")), "sgu_lnb": _rep(f("sgu_ln_b")), "sgu_wT": np.ascontiguousarray(f("sgu_w").transpose(0, 2, 1)),
        "sgu_bs": np.ascontiguousarray(f("sgu_b").T),
        "ssd_convw": _conv_tiles(np.ascontiguousarray(f("conv_w").T)), "ssd_convb": _conv_tiles(f("conv_b")[:, None]),
        "ssd_dtb": _rep(f("dt_bias")), "ssd_alog": _rep(f("a_log")), "ssd_dskip": _rep(f("d_skip")), "ssd_norm": _rep(f("ssm_norm")),
        "wo": _wl(f("w_out")), "wr": np.ascontiguousarray(Wr.reshape(16, 128, 36).transpose(1, 0, 2)),
        "br": _rep(np.concatenate([f("b_coarse"), f("b_fine").reshape(-1)])),
        "wg": _wgl(f("w_gate")), "wu": _wgl(f("w_up")), "wd": np.ascontiguousarray(f("w_down").reshape(32 * 512, 2048)),
        "fn": np.ascontiguousarray(np.concatenate([_col(np.asarray(p["final_norm"], np.float32)), np.full((128, 1), 1.0 if last else 0.0, np.float32)], 1)),
    }


def _fused_inputs(p, T=T_SEQ, layers=(0, 1, 2, 3), nb=4, xT_list=None, total_layers=4):
    x = np.asarray(p["x"], np.float32); c = np.asarray(p["c"], np.float32)
    cosT, sinT = _rot_tables(T)
    w_ada = np.asarray(p["w_ada"], np.float32); b_ada = np.asarray(p["b_ada"], np.float32)
    nl = len(layers)
    W = np.concatenate([w_ada[l] for l in layers], axis=1)
    shared = {"cos": cosT, "sin": sinT, "wada": _wl(W),
              "bias": np.ascontiguousarray(np.concatenate([b_ada[l] for l in layers]).reshape(nl * 96, 128).T),
              "ncols": np.ascontiguousarray(np.stack([_col(np.asarray(p["norm1"][l], np.float32)) for l in layers] +
                                                     [_col(np.asarray(p["norm2"][l], np.float32)) for l in layers], 1))}
    for li, l in enumerate(layers):
        for n, a in _layer_shared(l, p, last=(l == total_layers - 1)).items():
            shared[f"{n}_l{li}"] = a
    ins = []
    for b in range(nb):
        d = dict(shared)
        d["xT"] = np.ascontiguousarray(x[b, :T].T) if xT_list is None else xT_list[b]
        cc = _col(c[b])[:, :, None]
        d["cT"] = np.ascontiguousarray(np.concatenate([cc, cc], 2))
        ins.append(d)
    return ins


LAUNCHES = [[0, 1], [2, 3]]
_NC_CACHE = {}


def kernel(**p):
    from concourse.bass_utils import run_bass_kernel_spmd
    xT = None
    for layers in LAUNCHES:
        nl = len(layers)
        if nl not in _NC_CACHE:
            _NC_CACHE[nl] = build_fused(T_SEQ, nl)
        ins = _fused_inputs(p, T_SEQ, layers, 4, xT)
        res = run_bass_kernel_spmd(_NC_CACHE[nl], ins, core_ids=[0, 1, 2, 3])
        xT = [res.results[b]["xo"] for b in range(4)]
    return np.ascontiguousarray(np.stack([xT[b].T for b in range(4)], 0)).astype(np.float32)
```

```python
import numpy as np
from contextlib import ExitStack
import concourse.bass as bass
import concourse.mybir as mybir

F32 = mybir.dt.float32
BF16 = mybir.dt.bfloat16
I32 = mybir.dt.int32
U32 = mybir.dt.uint32
AF = mybir.ActivationFunctionType
ALU = mybir.AluOpType
AX = mybir.AxisListType


class Buf:
    __slots__ = ("t", "name", "w", "rd", "dw_sem", "dw_cnt", "dr_sem", "dr_cnt", "dw_base", "dr_base")

    def __init__(self, t, name):
        self.t = t
        self.name = name
        self.w = None
        self.rd = {}
        self.dw_sem = None
        self.dw_cnt = 0
        self.dr_sem = None
        self.dr_cnt = 0
        self.dw_base = 0
        self.dr_base = 0

    def __getitem__(self, idx):
        return self.t[idx]


class DBuf:
    def __init__(self, t, name):
        self.t = t
        self.name = name
        self.pw = {}
        self.pr = {}

    def __getitem__(self, idx):
        return self.t[idx]


class K:
    ENG = ("pe", "dve", "act", "pool", "sp")

    def __init__(self, nc, same_engine_sync=True):
        self.nc = nc
        self.es = ExitStack()
        self.E = {"pe": nc.tensor, "dve": nc.vector, "act": nc.scalar, "pool": nc.gpsimd, "sp": nc.sync}
        self.sem = {e: self.es.enter_context(nc.semaphore("sem_" + e)) for e in self.ENG}
        self.cnt = {e: 0 for e in self.ENG}
        self.seen = {}
        self.same = same_engine_sync
        self.cur = self.es
        self.dma_all = {}
        self.dbufs = []
        self.sem_pool = []
        self.scope_bufs = [[]]
        self.uid = 0
        self.nsem = len(self.ENG)

    def sbuf(self, name, shape, dt):
        self.uid += 1
        name = name + "_" + str(self.uid)
        t = self.cur.enter_context(self.nc.sbuf_tensor(name, list(shape), dt))
        b = Buf(t, name)
        self.scope_bufs[-1].append(b)
        return b

    def psum(self, name, shape, dt=F32):
        self.uid += 1
        name = name + "_" + str(self.uid)
        t = self.cur.enter_context(self.nc.psum_tensor(name, list(shape), dt))
        return Buf(t, name)

    def barrier(self):
        for e in self.ENG:
            for e2 in self.ENG:
                if e2 != e and e2 != "sp" and self.cnt[e2]:
                    self._wait(e, self.sem[e2], self.cnt[e2], "E" + e2)
            for key, (sem, val) in self.dma_all.items():
                self._wait(e, sem, val, key)

    def scope(self):
        from contextlib import contextmanager

        @contextmanager
        def _s():
            prev = self.cur
            st = ExitStack()
            self.cur = st
            self.scope_bufs.append([])
            try:
                yield
            finally:
                self.barrier()
                for b in self.scope_bufs.pop():
                    if b.dw_sem is not None:
                        self.sem_pool.append((b.dw_sem, b.dw_base + 16 * b.dw_cnt))
                    if b.dr_sem is not None:
                        self.sem_pool.append((b.dr_sem, b.dr_base + 16 * b.dr_cnt))
                self.dma_all = {}
                for d in self.dbufs:
                    d.pw = {}
                    d.pr = {}
                self.seen = {kk: v for kk, v in self.seen.items() if kk[1].startswith("E")}
                self.cur = prev
                st.close()
        return _s()

    def dram(self, name, shape, dt, kind="Internal"):
        if kind == "Internal":
            self.uid += 1
            name = name + "_" + str(self.uid)
        t = self.nc.dram_tensor(name, list(shape), dt, kind=kind).ap()
        d = DBuf(t, name)
        self.dbufs.append(d)
        return d

    def view(self, buf_t, name):
        return Buf(buf_t, name)

    def _newsem(self, name):
        if self.sem_pool:
            return self.sem_pool.pop()
        self.nsem += 1
        return self.es.enter_context(self.nc.semaphore("s" + str(self.nsem))), 0

    def _wait(self, eng, sem, val, key):
        if val <= 0:
            return
        k = (eng, key)
        if self.seen.get(k, 0) >= val:
            return
        self.seen[k] = val
        self.E[eng].wait_ge(sem, val)

    def _wait_eng(self, eng, dep):
        if dep is None:
            return
        e2, c = dep
        if e2 == eng and (not self.same or eng in ("pe", "sp")):
            return
        self._wait(eng, self.sem[e2], c, "E" + e2)

    def _deps_read(self, eng, b):
        self._wait_eng(eng, b.w)
        if b.dw_cnt:
            self._wait(eng, b.dw_sem, b.dw_base + 16 * b.dw_cnt, "DW" + b.name)

    def _deps_write(self, eng, b):
        self._wait_eng(eng, b.w)
        for e2, c in b.rd.items():
            if e2 != eng:
                self._wait_eng(eng, (e2, c))
        if b.dw_cnt:
            self._wait(eng, b.dw_sem, b.dw_base + 16 * b.dw_cnt, "DW" + b.name)
        if b.dr_cnt:
            self._wait(eng, b.dr_sem, b.dr_base + 16 * b.dr_cnt, "DR" + b.name)

    def op(self, eng, fn, reads=(), writes=(), inc=True):
        for b in reads:
            self._deps_read(eng, b)
        for b in writes:
            self._deps_write(eng, b)
        ins = fn(self.E[eng])
        if inc:
            self.cnt[eng] += 1
            ins.then_inc(self.sem[eng], 1)
            c = self.cnt[eng]
        else:
            c = self.cnt[eng] + 1
        for b in reads:
            b.rd[eng] = c
        for b in writes:
            b.w = (eng, c)
            b.rd = {}
        return ins

    def dma(self, q, out_ap, in_ap, src=None, dst=None, **kw):
        s_sb = isinstance(src, Buf)
        d_sb = isinstance(dst, Buf)
        assert s_sb != d_sb, "exactly one side must be an SBUF Buf"
        if d_sb:
            self._deps_write(q, dst)
            if isinstance(src, DBuf):
                for sname, (sem, val) in src.pw.items():
                    self._wait(q, sem, val, sname)
        else:
            self._deps_read(q, src)
            if isinstance(dst, DBuf):
                for d in (dst.pw, dst.pr):
                    for sname, (sem, val) in d.items():
                        self._wait(q, sem, val, sname)
        ins = self.E[q].dma_start(out=out_ap, in_=in_ap, **kw)
        if d_sb:
            if dst.dw_sem is None:
                dst.dw_sem, dst.dw_base = self._newsem("dw_" + dst.name)
            dst.dw_cnt += 1
            ins.then_inc(dst.dw_sem, 16)
            self.dma_all["DW" + dst.name] = (dst.dw_sem, dst.dw_base + 16 * dst.dw_cnt)
            dst.rd = {}
            if isinstance(src, DBuf):
                src.pr["DW" + dst.name] = (dst.dw_sem, dst.dw_base + 16 * dst.dw_cnt)
        else:
            if src.dr_sem is None:
                src.dr_sem, src.dr_base = self._newsem("dr_" + src.name)
            src.dr_cnt += 1
            ins.then_inc(src.dr_sem, 16)
            self.dma_all["DR" + src.name] = (src.dr_sem, src.dr_base + 16 * src.dr_cnt)
            if isinstance(dst, DBuf):
                dst.pw["DR" + src.name] = (src.dr_sem, src.dr_base + 16 * src.dr_cnt)
                dst.pr = {}
        return ins

    def indirect(self, q, sb, dram, idxbuf, out_ap, in_ap, out_idx=None, in_idx=None, bound=None):
        g = self.E["pool"]
        self._deps_read("pool", idxbuf)
        if in_idx is not None:
            dst = sb
            self._deps_write("pool", dst)
            if isinstance(dram, DBuf):
                for sname, (sem, val) in dram.pw.items():
                    self._wait("pool", sem, val, sname)
            ins = g.indirect_dma_start(out=out_ap, out_offset=None, in_=in_ap, in_offset=bass.IndirectOffsetOnAxis(ap=in_idx, axis=0))
            if dst.dw_sem is None:
                dst.dw_sem, dst.dw_base = self._newsem("dw_" + dst.name)
            dst.dw_cnt += 1
            ins.then_inc(dst.dw_sem, 16)
            self.dma_all["DW" + dst.name] = (dst.dw_sem, dst.dw_base + 16 * dst.dw_cnt)
            dst.rd = {}
            if isinstance(dram, DBuf):
                dram.pr["DW" + dst.name] = (dst.dw_sem, dst.dw_base + 16 * dst.dw_cnt)
        else:
            srcb, dd = dram, sb
            self._deps_read("pool", srcb)
            for sname, (sem, val) in dd.pr.items():
                self._wait("pool", sem, val, sname)
            kw = {}
            if bound is not None:
                kw = dict(bounds_check=bound, oob_is_err=False)
            ins = g.indirect_dma_start(out=out_ap, out_offset=bass.IndirectOffsetOnAxis(ap=out_idx, axis=0), in_=in_ap, in_offset=None, **kw)
            if srcb.dr_sem is None:
                srcb.dr_sem, srcb.dr_base = self._newsem("dr_" + srcb.name)
            srcb.dr_cnt += 1
            ins.then_inc(srcb.dr_sem, 16)
            self.dma_all["DR" + srcb.name] = (srcb.dr_sem, srcb.dr_base + 16 * srcb.dr_cnt)
            dd.pw["DR" + srcb.name] = (srcb.dr_sem, srcb.dr_base + 16 * srcb.dr_cnt)
        idxbuf.rd["pool"] = self.cnt["pool"] + 1
        return ins

    def finish(self, outs=()):
        for d in outs:
            for sname, (sem, val) in d.pw.items():
                self._wait("sp", sem, val, sname)
        for e in self.ENG:
            if e != "sp" and self.cnt[e]:
                self._wait("sp", self.sem[e], self.cnt[e], "E" + e)

    def close(self):
        self.es.close()


import os
DBG = ''


FR = mybir.dt.float32r
NEG = -30000.0


def emit_consts(k):
    C = {}
    C["ones32"] = k.sbuf("c_ones32", [128, 128], F32)
    k.op("pool", lambda e: e.memset(C["ones32"][:], 1.0), writes=[C["ones32"]])
    C["ident32"] = k.sbuf("c_ident32", [128, 128], F32)
    k.op("pool", lambda e: e.memset(C["ident32"][:], 0.0), writes=[C["ident32"]])
    k.op("pool", lambda e: e.affine_select(out=C["ident32"][:], in_=C["ident32"][:], pattern=[[-1, 128]], compare_op=ALU.not_equal,
                                            fill=1.0, base=0, channel_multiplier=1), reads=[C["ident32"]], writes=[C["ident32"]])
    C["ident16"] = k.sbuf("c_ident16", [128, 128], BF16)
    k.op("dve", lambda e: e.tensor_copy(out=C["ident16"][:], in_=C["ident32"][:]), reads=[C["ident32"]], writes=[C["ident16"]])
    C["U32"] = k.sbuf("c_U32", [128, 128], F32)
    k.op("pool", lambda e: e.affine_select(out=C["U32"][:], in_=C["ones32"][:], pattern=[[1, 128]], compare_op=ALU.is_ge,
                                            fill=0.0, base=0, channel_multiplier=-1), reads=[C["ones32"]], writes=[C["U32"]])
    C["Lst32"] = k.sbuf("c_Lst32", [128, 128], F32)
    k.op("pool", lambda e: e.affine_select(out=C["Lst32"][:], in_=C["ones32"][:], pattern=[[1, 128]], compare_op=ALU.is_ge,
                                            fill=0.0, base=-1, channel_multiplier=-1), reads=[C["ones32"]], writes=[C["Lst32"]])
    z32 = k.sbuf("c_z32", [128, 128], F32)
    k.op("pool", lambda e: e.memset(z32[:], 0.0), writes=[z32])
    caus32 = k.sbuf("c_caus32", [128, 128], F32)
    k.op("pool", lambda e: e.affine_select(out=caus32[:], in_=z32[:], pattern=[[1, 128]], compare_op=ALU.is_ge,
                                            fill=NEG, base=0, channel_multiplier=-1), reads=[z32], writes=[caus32])
    C["caus16"] = k.sbuf("c_caus16", [128, 128], BF16)
    k.op("dve", lambda e: e.tensor_copy(out=C["caus16"][:], in_=caus32[:]), reads=[caus32], writes=[C["caus16"]])
    C["sel16"] = k.sbuf("c_sel16", [32, 32, 128], BF16)
    with k.scope():
      sel32 = k.sbuf("c_sel32", [32, 32, 128], F32)
      k.op("pool", lambda e: e.memset(sel32[:], 1.0), writes=[sel32])
      k.op("pool", lambda e: e.affine_select(out=sel32[:], in_=sel32[:], pattern=[[-1, 32], [0, 128]], compare_op=ALU.is_equal,
                                            fill=0.0, base=0, channel_multiplier=1), reads=[sel32], writes=[sel32])
      k.op("dve", lambda e: e.tensor_copy(out=C["sel16"][:], in_=sel32[:]), reads=[sel32], writes=[C["sel16"]])
    return C


def emit_attn(k, C, T, qT_d, kT_d, v_d, cos_d, sin_d, y_d, y_col0, nheads, src=None, dstbuf=None):
    NBLK = T // 256
    NKT = T // 128
    RC = min(T, 2048)
    scale = 128 ** -0.5
    q32 = k.sbuf("a_q32", [128, T], F32); k32 = k.sbuf("a_k32", [128, T], F32)
    q16 = k.sbuf("a_q16", [128, T], BF16); k16 = k.sbuf("a_k16", [128, T], BF16)
    swp = k.sbuf("a_swp", [32, RC], F32); cs = k.sbuf("a_cos", [32, RC], F32); sn = k.sbuf("a_sin", [32, RC], F32)
    rt = k.sbuf("a_rt", [32, RC], F32)
    V1 = k.sbuf("a_V1", [128, NKT, 129], BF16)
    kmean = k.sbuf("a_kmean", [128, NBLK], F32)
    gate = k.sbuf("a_gate", [128, 32], F32)
    mx8 = k.sbuf("a_mx8", [128, 8], F32)
    biasq = k.sbuf("a_biasq", [128, 32], F32)
    maskT = k.sbuf("a_maskT", [32, 256], BF16)
    PT = [k.sbuf(f"a_PT{i}", [128, 256], BF16) for i in range(3)]
    yo = [k.sbuf(f"a_yo{i}", [128, 128], F32) for i in range(2)]
    rec = k.sbuf("a_rec", [128, 1], F32)
    ps_s = [k.psum(f"a_ps_s{i}", [128, 256]) for i in range(2)]
    ps_o = [k.psum(f"a_ps_o{i}", [128, 129]) for i in range(4)]
    ps_g = k.psum("a_ps_g", [128, 32])
    ps_t = k.psum("a_ps_t", [32, 128])
    k.op("pool", lambda e: e.memset(gate[:], -1e30), writes=[gate])
    k.op("pool", lambda e: e.memset(V1[:, :, 128:129], 1.0), writes=[V1])
    si = 0; oi = 0; yi = 0
    for h in range(nheads):
        for (dst32, srcd) in ((q32, qT_d), (k32, kT_d)):
            k.dma("sp", dst32[:], srcd[h * 128:(h + 1) * 128, :], src=src, dst=dst32)
            for c0 in range(0, T, RC):
                k.dma("sp", swp[0:16, :], srcd[h * 128 + 16:h * 128 + 32, c0:c0 + RC], src=src, dst=swp)
                k.dma("sp", swp[16:32, :], srcd[h * 128:h * 128 + 16, c0:c0 + RC], src=src, dst=swp)
                k.dma("sp", cs[:], cos_d[:, c0:c0 + RC], dst=cs)
                k.dma("sp", sn[:], sin_d[:, c0:c0 + RC], dst=sn)
                k.op("dve", lambda e: e.tensor_tensor(out=rt[:], in0=dst32[0:32, c0:c0 + RC], in1=cs[:], op=ALU.mult), reads=[dst32, cs], writes=[rt])
                k.op("dve", lambda e: e.tensor_tensor(out=swp[:], in0=swp[:], in1=sn[:], op=ALU.mult), reads=[swp, sn], writes=[swp])
                k.op("dve", lambda e: e.tensor_tensor(out=dst32[0:32, c0:c0 + RC], in0=rt[:], in1=swp[:], op=ALU.add), reads=[rt, swp], writes=[dst32])
        k.op("act", lambda e: e.copy(out=q16[:], in_=q32[:]), reads=[q32], writes=[q16])
        k.op("act", lambda e: e.copy(out=k16[:], in_=k32[:]), reads=[k32], writes=[k16])
        k.op("dve", lambda e: e.tensor_reduce(out=kmean[:], in_=k32[:].rearrange("p (b s) -> p b s", s=256), op=ALU.add, axis=AX.X),
             reads=[k32], writes=[kmean])
        k.op("dve", lambda e: e.tensor_scalar(out=kmean[:], in0=kmean[:], scalar1=1.0 / 256, scalar2=None, op0=ALU.mult), reads=[kmean], writes=[kmean])
        k.dma("pool", V1[:, :, 0:128], v_d[:, h * 128:(h + 1) * 128].rearrange("(n p) d -> p n d", p=128), src=src, dst=V1)
        for Q in range(NBLK):
            q0 = Q * 256
            use_mask = Q > 3
            if use_mask and True:
                for half in range(2):
                    qs = slice(q0 + half * 128, q0 + half * 128 + 128)
                    k.op("pe", lambda e: e.matmul(ps_g[:, 0:NBLK], lhsT=q32[:, qs], rhs=kmean[:, 0:NBLK], start=True, stop=True), reads=[q32, kmean], writes=[ps_g])
                    k.op("dve", lambda e: e.tensor_copy(out=gate[:, 0:Q], in_=ps_g[:, 0:Q]), reads=[ps_g], writes=[gate])
                    k.op("dve", lambda e: e.max(out=mx8[:], in_=gate[:, 0:max(Q, 8)]), reads=[gate], writes=[mx8])
                    k.op("dve", lambda e: e.tensor_scalar(out=biasq[:], in0=gate[:], scalar1=mx8[:, 2:3], scalar2=1.0, op0=ALU.is_ge, op1=ALU.subtract),
                         reads=[gate, mx8], writes=[biasq])
                    k.op("dve", lambda e: e.tensor_scalar(out=biasq[:], in0=biasq[:], scalar1=-NEG, scalar2=None, op0=ALU.mult), reads=[biasq], writes=[biasq])
                    k.op("pe", lambda e: e.transpose(out=ps_t[:], in_=biasq[:], identity=C["ident32"][:]), reads=[biasq, C["ident32"]], writes=[ps_t])
                    k.op("act", lambda e: e.copy(out=maskT[:, half * 128:(half + 1) * 128], in_=ps_t[:]), reads=[ps_t], writes=[maskT])
            po = [ps_o[(oi + i) % 4] for i in range(2)]; oi += 2
            jobs = [("past", j, kt) for j in range(Q) for kt in range(2)]
            for half in range(2):
                jobs += [("own", half, kt) for kt in range(half + 1)]
            firstjob = [None, None]; lastjob = [None, None]
            for ji, (kind, a, kt) in enumerate(jobs):
                halves = (0, 1) if kind == "past" else (a,)
                for hf in halves:
                    if firstjob[hf] is None:
                        firstjob[hf] = ji
                    lastjob[hf] = ji

            def emit_S(job):
                nonlocal si
                kind, a, kt = job
                ps = ps_s[si % 2]; pt = PT[si % 3]; si += 1
                if kind == "past":
                    kti = a * 2 + kt
                    k.op("pe", lambda e: e.matmul(ps[:], lhsT=k16[:, kti * 128:(kti + 1) * 128], rhs=q16[:, q0:q0 + 256], start=True, stop=not use_mask),
                         reads=[k16, q16], writes=[ps], inc=not use_mask)
                    if use_mask:
                        k.op("pe", lambda e: e.matmul(ps[:], lhsT=C["sel16"][:, a, :], rhs=maskT[:], start=False, stop=True), reads=[C["sel16"], maskT], writes=[ps])
                    k.op("act", lambda e: e.activation(out=pt[:], in_=ps[:], func=AF.Exp, scale=scale), reads=[ps], writes=[pt])
                else:
                    half = a
                    qs = slice(q0 + half * 128, q0 + half * 128 + 128)
                    kti = Q * 2 + kt
                    diag = (kt == half)
                    k.op("pe", lambda e: e.matmul(ps[:, 0:128], lhsT=k16[:, kti * 128:(kti + 1) * 128], rhs=q16[:, qs], start=True, stop=not diag),
                         reads=[k16, q16], writes=[ps], inc=not diag)
                    if diag:
                        k.op("pe", lambda e: e.matmul(ps[:, 0:128], lhsT=C["ident16"][:], rhs=C["caus16"][:], start=False, stop=True),
                             reads=[C["ident16"], C["caus16"]], writes=[ps])
                    k.op("act", lambda e: e.activation(out=pt[:, 0:128], in_=ps[:, 0:128], func=AF.Exp, scale=scale), reads=[ps], writes=[pt])
                return pt

            def emit_PV(ji, job, pt):
                kind, a, kt = job
                if kind == "past":
                    kti = a * 2 + kt
                    for half in range(2):
                        lastf = (lastjob[half] == ji)
                        k.op("pe", lambda e: e.matmul(po[half][:], lhsT=pt[:, half * 128:(half + 1) * 128], rhs=V1[:, kti, :], start=(firstjob[half] == ji), stop=lastf),
                             reads=[pt, V1], writes=[po[half]], inc=lastf)
                else:
                    half = a
                    kti = Q * 2 + kt
                    lastf = (lastjob[half] == ji)
                    k.op("pe", lambda e: e.matmul(po[half][:], lhsT=pt[:, 0:128], rhs=V1[:, kti, :], start=(firstjob[half] == ji), stop=lastf),
                         reads=[pt, V1], writes=[po[half]], inc=lastf)

            pts = {0: emit_S(jobs[0])}
            for ji, job in enumerate(jobs):
                if ji + 1 < len(jobs):
                    pts[ji + 1] = emit_S(jobs[ji + 1])
                emit_PV(ji, job, pts.pop(ji))
            for half in range(2):
                y = yo[yi % 2]; yi += 1
                k.op("dve", lambda e: e.reciprocal(out=rec[:], in_=po[half][:, 128:129]), reads=[po[half]], writes=[rec])
                k.op("dve", lambda e: e.tensor_scalar(out=y[:], in0=po[half][:, 0:128], scalar1=rec[:, 0:1], scalar2=None, op0=ALU.mult), reads=[po[half], rec], writes=[y])
                k.dma("sp", y_d[q0 + half * 128:q0 + half * 128 + 128, y_col0 + h * 128:y_col0 + (h + 1) * 128], y[:], src=y, dst=y_d)


FR = mybir.dt.float32r


def emit_ssd(k, C, T, xbcT_d, ptm_d, z_col0, dt_col0, prm, y_d, y_col0, groups=(0, 1), src=None):
    NCH = T // 128
    cw = k.sbuf("s_cw", [128, 6, 4], F32); cb = k.sbuf("s_cb", [128, 6, 1], F32)
    dtb = k.sbuf("s_dtb", [128, 8], F32); aneg = k.sbuf("s_aneg", [128, 8], F32); dsk = k.sbuf("s_dsk", [128, 8], F32)
    dskx = k.sbuf("s_dskx", [128, 8, 64], F32)
    nrm = k.sbuf("s_nrm", [128, 512], F32)
    cin = [k.sbuf(f"s_cin{i}", [128, 6, 131], F32) for i in range(2)]
    acc = k.sbuf("s_acc", [128, 6, 128], F32); tap = k.sbuf("s_tap", [128, 6, 128], F32)
    xc = k.sbuf("s_xc", [128, 6, 128], F32)
    xcr = k.sbuf("s_xcr", [128, 2, 128], FR)
    x_tm = k.sbuf("s_xtm", [128, 8, 64], F32)
    B_tm = k.sbuf("s_Btm", [128, 128], FR)
    dtr = k.sbuf("s_dtr", [128, 8], F32); dt = k.sbuf("s_dt", [128, 8], F32); dA = k.sbuf("s_dA", [128, 8], F32)
    dArep = k.sbuf("s_dArep", [128, 8, 128], F32)
    acum = k.sbuf("s_acum", [128, 8], F32); tot = k.sbuf("s_tot", [128, 8], F32)
    dec = k.sbuf("s_dec", [128, 8, 128], F32)
    CBm = k.sbuf("s_CBm", [128, 128], F32)
    Mt = k.sbuf("s_Mt", [128, 8, 128], FR)
    xdt = k.sbuf("s_xdt", [128, 8, 64], FR)
    ea = k.sbuf("s_ea", [128, 8], F32); wend = k.sbuf("s_wend", [128, 8], F32); etot = k.sbuf("s_etot", [128, 8], F32)
    xw = k.sbuf("s_xw", [128, 8, 64], FR)
    ST32 = k.sbuf("s_ST32", [128, 8, 64], F32); STr = k.sbuf("s_STr", [128, 8, 64], FR)
    y1 = k.sbuf("s_y1", [128, 8, 64], F32); t2 = k.sbuf("s_t2", [128, 8, 64], F32)
    zt = [k.sbuf(f"s_zt{i}", [128, 512], F32) for i in range(2)]
    junk = k.sbuf("s_junk", [128, 512], F32)
    ss = k.sbuf("s_ss", [128, 1], F32)
    yo = [k.sbuf(f"s_yo{i}", [128, 512], F32) for i in range(2)]
    p_x = k.psum("s_p_x", [128, 512]); p_b = k.psum("s_p_b", [128, 128]); p_cb = k.psum("s_p_cb", [128, 128])
    p_ac = k.psum("s_p_ac", [128, 16]); p_abc = k.psum("s_p_abc", [128, 1024])
    p_y = k.psum("s_p_y", [128, 512]); p_yi = k.psum("s_p_yi", [128, 512])
    for g in groups:
        xr0 = g * 512; br0 = 1024 + g * 128; cr0 = 1280 + g * 128
        k.dma("sp", cw[:], prm["convw"][g], dst=cw)
        k.dma("sp", cb[:], prm["convb"][g], dst=cb)
        k.dma("sp", dtb[:], prm["dtb"][:, g * 8:(g + 1) * 8], dst=dtb)
        k.dma("sp", aneg[:], prm["alog"][:, g * 8:(g + 1) * 8], dst=aneg)
        k.dma("sp", dsk[:], prm["dskip"][:, g * 8:(g + 1) * 8], dst=dsk)
        k.dma("sp", nrm[:], prm["norm"][:, g * 512:(g + 1) * 512], dst=nrm)
        k.op("act", lambda e: e.activation(out=aneg[:], in_=aneg[:], func=AF.Exp), reads=[aneg], writes=[aneg])
        k.op("dve", lambda e: e.tensor_scalar(out=aneg[:], in0=aneg[:], scalar1=-1.0, scalar2=None, op0=ALU.mult), reads=[aneg], writes=[aneg])
        k.op("dve", lambda e: e.tensor_copy(out=dskx[:], in_=dsk[:].unsqueeze(2).to_broadcast([128, 8, 64])), reads=[dsk], writes=[dskx])
        k.op("pool", lambda e: e.memset(ST32[:], 0.0), writes=[ST32])
        for c in range(NCH):
            t0 = c * 128
            ci = cin[c % 2]
            lo = 3 if c == 0 else 0
            if c == 0:
                k.op("pool", lambda e: e.memset(ci[:, :, 0:3], 0.0), writes=[ci])
            k.dma("sp", ci[:, 0:4, lo:131], xbcT_d[xr0:xr0 + 512, t0 - 3 + lo:t0 + 128].rearrange("(i p) t -> p i t", p=128), src=src, dst=ci)
            k.dma("sp", ci[:, 4, lo:131], xbcT_d[br0:br0 + 128, t0 - 3 + lo:t0 + 128], src=src, dst=ci)
            k.dma("sp", ci[:, 5, lo:131], xbcT_d[cr0:cr0 + 128, t0 - 3 + lo:t0 + 128], src=src, dst=ci)
            z_ = zt[c % 2]
            k.dma("sp", z_[:], ptm_d[t0:t0 + 128, z_col0 + g * 512:z_col0 + (g + 1) * 512], src=src, dst=z_)
            k.dma("sp", dtr[:], ptm_d[t0:t0 + 128, dt_col0 + g * 8:dt_col0 + (g + 1) * 8], src=src, dst=dtr)
            k.op("dve", lambda e: e.tensor_tensor(out=acc[:], in0=ci[:, :, 3:131], in1=cw[:, :, 3:4].to_broadcast([128, 6, 128]), op=ALU.mult), reads=[ci, cw], writes=[acc])
            k.op("dve", lambda e: e.tensor_tensor(out=acc[:], in0=acc[:], in1=cb[:].to_broadcast([128, 6, 128]), op=ALU.add), reads=[acc, cb], writes=[acc])
            for j in range(3):
                k.op("pool", lambda e: e.tensor_tensor(out=tap[:], in0=ci[:, :, j:j + 128], in1=cw[:, :, j:j + 1].to_broadcast([128, 6, 128]), op=ALU.mult), reads=[ci, cw], writes=[tap])
                k.op("dve", lambda e: e.tensor_tensor(out=acc[:], in0=acc[:], in1=tap[:], op=ALU.add), reads=[acc, tap], writes=[acc])
            k.op("act", lambda e: e.activation(out=xc[:], in_=acc[:], func=AF.Silu), reads=[acc], writes=[xc])
            k.op("dve", lambda e: e.tensor_copy(out=xcr[:], in_=xc[:, 4:6, :]), reads=[xc], writes=[xcr])
            for i in range(4):
                k.op("pe", lambda e: e.transpose(out=p_x[:, i * 128:(i + 1) * 128], in_=xc[:, i, :], identity=C["ident32"][:]), reads=[xc, C["ident32"]], writes=[p_x], inc=(i == 3))
            k.op("act", lambda e: e.copy(out=x_tm[:].rearrange("p h d -> p (h d)"), in_=p_x[:]), reads=[p_x], writes=[x_tm])
            k.op("pe", lambda e: e.transpose(out=p_b[:], in_=xc[:, 4, :], identity=C["ident32"][:]), reads=[xc, C["ident32"]], writes=[p_b])
            k.op("dve", lambda e: e.tensor_copy(out=B_tm[:], in_=p_b[:]), reads=[p_b], writes=[B_tm])
            k.op("dve", lambda e: e.tensor_tensor(out=dt[:], in0=dtr[:], in1=dtb[:], op=ALU.add), reads=[dtr, dtb], writes=[dt])
            k.op("act", lambda e: e.activation(out=dt[:], in_=dt[:], func=AF.Exp), reads=[dt], writes=[dt])
            k.op("act", lambda e: e.activation(out=dt[:], in_=dt[:], func=AF.Ln, bias=1.0), reads=[dt], writes=[dt])
            k.op("dve", lambda e: e.tensor_tensor(out=dA[:], in0=dt[:], in1=aneg[:], op=ALU.mult), reads=[dt, aneg], writes=[dA])
            k.op("pe", lambda e: e.matmul(p_ac[:, 0:8], lhsT=C["U32"][:], rhs=dA[:], start=True, stop=True), reads=[C["U32"], dA], writes=[p_ac])
            k.op("pe", lambda e: e.matmul(p_ac[:, 8:16], lhsT=C["ones32"][:], rhs=dA[:], start=True, stop=True), reads=[C["ones32"], dA], writes=[p_ac])
            k.op("dve", lambda e: e.tensor_copy(out=acum[:], in_=p_ac[:, 0:8]), reads=[p_ac], writes=[acum])
            k.op("dve", lambda e: e.tensor_copy(out=tot[:], in_=p_ac[:, 8:16]), reads=[p_ac], writes=[tot])
            k.op("dve", lambda e: e.tensor_copy(out=dArep[:], in_=dA[:].unsqueeze(2).to_broadcast([128, 8, 128])), reads=[dA], writes=[dArep])
            for h in range(8):
                k.op("pe", lambda e: e.matmul(p_abc[:, h * 128:(h + 1) * 128], lhsT=dArep[:, h, :], rhs=C["U32"][:], start=True, stop=True),
                     reads=[dArep, C["U32"]], writes=[p_abc], inc=(h == 7))
            k.op("dve", lambda e: e.tensor_tensor(out=dec[:], in0=p_abc[:].rearrange("p (h l) -> p h l", h=8), in1=acum[:].unsqueeze(2).to_broadcast([128, 8, 128]), op=ALU.subtract),
                 reads=[p_abc, acum], writes=[dec])
            k.op("dve", lambda e: e.tensor_scalar(out=dec[:], in0=dec[:], scalar1=0.0, scalar2=None, op0=ALU.min), reads=[dec], writes=[dec])
            k.op("act", lambda e: e.activation(out=dec[:], in_=dec[:], func=AF.Exp), reads=[dec], writes=[dec])
            k.op("pe", lambda e: e.matmul(p_cb[:], lhsT=xcr[:, 0, :], rhs=xcr[:, 1, :], start=True, stop=True), reads=[xcr], writes=[p_cb])
            k.op("dve", lambda e: e.tensor_tensor(out=CBm[:], in0=p_cb[:], in1=C["U32"][:], op=ALU.mult), reads=[p_cb, C["U32"]], writes=[CBm])
            k.op("dve", lambda e: e.tensor_tensor(out=Mt[:], in0=dec[:], in1=CBm[:].unsqueeze(1).to_broadcast([128, 8, 128]), op=ALU.mult), reads=[dec, CBm], writes=[Mt])
            k.op("pool", lambda e: e.tensor_tensor(out=xdt[:], in0=x_tm[:], in1=dt[:].unsqueeze(2).to_broadcast([128, 8, 64]), op=ALU.mult), reads=[x_tm, dt], writes=[xdt])
            for h in range(8):
                k.op("pe", lambda e: e.matmul(p_y[:, h * 64:(h + 1) * 64], lhsT=Mt[:, h, :], rhs=xdt[:, h, :], start=True, stop=True), reads=[Mt, xdt], writes=[p_y], inc=(h == 7))
            k.op("dve", lambda e: e.tensor_copy(out=STr[:], in_=ST32[:]), reads=[ST32], writes=[STr])
            k.op("pe", lambda e: e.matmul(p_yi[:], lhsT=xcr[:, 1, :], rhs=STr[:].rearrange("p h d -> p (h d)"), start=True, stop=True), reads=[xcr, STr], writes=[p_yi])
            k.op("act", lambda e: e.activation(out=ea[:], in_=acum[:], func=AF.Exp), reads=[acum], writes=[ea])
            k.op("dve", lambda e: e.tensor_tensor(out=y1[:], in0=p_yi[:].rearrange("p (h d) -> p h d", h=8), in1=ea[:].unsqueeze(2).to_broadcast([128, 8, 64]), op=ALU.mult), reads=[p_yi, ea], writes=[y1])
            k.op("dve", lambda e: e.tensor_tensor(out=y1[:], in0=y1[:], in1=p_y[:].rearrange("p (h d) -> p h d", h=8), op=ALU.add), reads=[y1, p_y], writes=[y1])
            k.op("pool", lambda e: e.tensor_tensor(out=t2[:], in0=x_tm[:], in1=dskx[:], op=ALU.mult), reads=[x_tm, dskx], writes=[t2])
            k.op("dve", lambda e: e.tensor_tensor(out=y1[:], in0=y1[:], in1=t2[:], op=ALU.add), reads=[y1, t2], writes=[y1])
            k.op("act", lambda e: e.activation(out=z_[:], in_=z_[:], func=AF.Silu), reads=[z_], writes=[z_])
            k.op("dve", lambda e: e.tensor_tensor(out=y1[:].rearrange("p h d -> p (h d)"), in0=y1[:].rearrange("p h d -> p (h d)"), in1=z_[:], op=ALU.mult), reads=[y1, z_], writes=[y1])
            k.op("act", lambda e: e.activation(out=junk[:], in_=y1[:].rearrange("p h d -> p (h d)"), func=AF.Square, accum_out=ss[:]), reads=[y1], writes=[junk, ss])
            k.op("dve", lambda e: e.tensor_scalar(out=ss[:], in0=ss[:], scalar1=1.0 / 512, scalar2=1e-6, op0=ALU.mult, op1=ALU.add), reads=[ss], writes=[ss])
            k.op("act", lambda e: e.activation(out=ss[:], in_=ss[:], func=AF.Sqrt), reads=[ss], writes=[ss])
            k.op("dve", lambda e: e.reciprocal(out=ss[:], in_=ss[:]), reads=[ss], writes=[ss])
            y_ = yo[c % 2]
            k.op("dve", lambda e: e.scalar_tensor_tensor(out=y_[:], in0=y1[:].rearrange("p h d -> p (h d)"), scalar=ss[:, 0:1], in1=nrm[:], op0=ALU.mult, op1=ALU.mult), reads=[y1, ss, nrm], writes=[y_])
            k.dma("sp", y_d[t0:t0 + 128, y_col0 + g * 512:y_col0 + (g + 1) * 512], y_[:], src=y_, dst=y_d)
            k.op("dve", lambda e: e.tensor_tensor(out=wend[:], in0=tot[:], in1=acum[:], op=ALU.subtract), reads=[tot, acum], writes=[wend])
            k.op("act", lambda e: e.activation(out=wend[:], in_=wend[:], func=AF.Exp), reads=[wend], writes=[wend])
            k.op("dve", lambda e: e.tensor_tensor(out=wend[:], in0=wend[:], in1=dt[:], op=ALU.mult), reads=[wend, dt], writes=[wend])
            k.op("pool", lambda e: e.tensor_tensor(out=xw[:], in0=x_tm[:], in1=wend[:].unsqueeze(2).to_broadcast([128, 8, 64]), op=ALU.mult), reads=[x_tm, wend], writes=[xw])
            k.op("pe", lambda e: e.matmul(p_x[:], lhsT=B_tm[:], rhs=xw[:].rearrange("p h d -> p (h d)"), start=True, stop=True), reads=[B_tm, xw], writes=[p_x])
            k.op("act", lambda e: e.activation(out=etot[:], in_=tot[:], func=AF.Exp), reads=[tot], writes=[etot])
            k.op("dve", lambda e: e.tensor_tensor(out=ST32[:], in0=ST32[:], in1=etot[:].unsqueeze(2).to_broadcast([128, 8, 64]), op=ALU.mult), reads=[ST32, etot], writes=[ST32])
            k.op("dve", lambda e: e.tensor_tensor(out=ST32[:], in0=ST32[:], in1=p_x[:].rearrange("p (h d) -> p h d", h=8), op=ALU.add), reads=[ST32, p_x], writes=[ST32])


def emit_sgu(k, C, T, ptm_d, uv_col0, prm, y_d, y_col0, src=None):
    NCH = T // 128
    lng = k.sbuf("g_lng", [128, 512], F32); lnb = k.sbuf("g_lnb", [128, 512], F32)
    wT = k.sbuf("g_wT", [128, 4, 128], F32); bs = k.sbuf("g_bs", [128, 4], F32)
    uv = [k.sbuf(f"g_uv{i}", [128, 1024], F32) for i in range(2)]
    guv = k.sbuf("g_guv", [128, 1024], F32)
    st = k.sbuf("g_st", [128, 6], F32); mv = k.sbuf("g_mv", [128, 2], F32); rs = k.sbuf("g_rs", [128, 1], F32)
    vn = k.sbuf("g_vn", [128, 512], F32)
    yo = [k.sbuf(f"g_yo{i}", [128, 512], F32) for i in range(2)]
    ps = k.psum("g_ps", [128, 512])
    k.dma("sp", lng[:], prm["lng"], dst=lng); k.dma("sp", lnb[:], prm["lnb"], dst=lnb)
    k.dma("sp", wT[:], prm["wT"].rearrange("g s t -> s g t"), dst=wT); k.dma("sp", bs[:], prm["bs"], dst=bs)
    k.op("pool", lambda e: e.affine_select(out=wT[:], in_=wT[:], pattern=[[0, 4], [1, 128]], compare_op=ALU.is_ge, fill=0.0, base=0, channel_multiplier=-1),
         reads=[wT], writes=[wT])
    for c in range(NCH):
        t0 = c * 128
        uv_ = uv[c % 2]; y_ = yo[c % 2]
        k.dma("sp", uv_[:], ptm_d[t0:t0 + 128, uv_col0:uv_col0 + 1024], src=src, dst=uv_)
        k.op("act", lambda e: e.activation(out=guv[:], in_=uv_[:], func=AF.Gelu_apprx_tanh), reads=[uv_], writes=[guv])
        k.op("dve", lambda e: e.bn_stats(out=st[:], in_=guv[:, 512:1024]), reads=[guv], writes=[st])
        k.op("dve", lambda e: e.bn_aggr(out=mv[:], in_=st[:]), reads=[st], writes=[mv])
        k.op("dve", lambda e: e.tensor_scalar(out=rs[:], in0=mv[:, 1:2], scalar1=1e-6, scalar2=None, op0=ALU.add), reads=[mv], writes=[rs])
        k.op("act", lambda e: e.activation(out=rs[:], in_=rs[:], func=AF.Sqrt), reads=[rs], writes=[rs])
        k.op("dve", lambda e: e.reciprocal(out=rs[:], in_=rs[:]), reads=[rs], writes=[rs])
        k.op("dve", lambda e: e.tensor_scalar(out=vn[:], in0=guv[:, 512:1024], scalar1=mv[:, 0:1], scalar2=rs[:, 0:1], op0=ALU.subtract, op1=ALU.mult), reads=[guv, mv, rs], writes=[vn])
        k.op("dve", lambda e: e.tensor_tensor(out=vn[:], in0=vn[:], in1=lng[:], op=ALU.mult), reads=[vn, lng], writes=[vn])
        k.op("dve", lambda e: e.tensor_tensor(out=vn[:], in0=vn[:], in1=lnb[:], op=ALU.add), reads=[vn, lnb], writes=[vn])
        for g in range(4):
            k.op("pe", lambda e: e.matmul(ps[:, g * 128:(g + 1) * 128], lhsT=wT[:, g, :], rhs=vn[:, g * 128:(g + 1) * 128], start=True, stop=True), reads=[wT, vn], writes=[ps], inc=(g == 3))
        for g in range(4):
            k.op("dve", lambda e: e.scalar_tensor_tensor(out=y_[:, g * 128:(g + 1) * 128], in0=ps[:, g * 128:(g + 1) * 128], scalar=bs[:, g:g + 1], in1=guv[:, g * 128:(g + 1) * 128],
                                                          op0=ALU.add, op1=ALU.mult), reads=[ps, bs, guv], writes=[y_])
        k.dma("sp", y_d[t0:t0 + 128, y_col0:y_col0 + 512], y_[:], src=y_, dst=y_d)


FR = mybir.dt.float32r
U32 = mybir.dt.uint32
I32 = mybir.dt.int32
D = 2048


def emit_post(k, C, T, xT_d, y_d, wo_d, vec_d, wr_d, br_d, wg_d, wu_d, wd_d, out_d, final=False, fn_d=None, xsrc=None):
    NT = T // 128
    BS = 512
    SUB = BS // 128
    NB = 2 * T // BS + 32
    TT = 256
    NSUB = TT // 128
    xTv = xT_d.rearrange("(c p) t -> p c t", p=128)
    X1 = k.dram("p_X1", [D, T], F32); X1v = X1.t.rearrange("(c p) t -> p c t", p=128)
    H2 = k.dram("p_H2", [T, D], F32)
    Xd = k.dram("p_Xd", [NB * BS, D], F32)
    Yd = k.dram("p_Yd", [NB * BS, D], F32)
    outv = out_d.t.rearrange("(c p) t -> p c t", p=128)
    vt = k.sbuf("p_vt", [128, 5, 16], F32); a2 = k.sbuf("p_a2", [128, 16], F32)
    AB = k.sbuf("p_AB", [128, NT, 64], F32)
    CUM = k.sbuf("p_CUM", [128, NT, 32], F32)
    GT = k.sbuf("p_GT", [128, NT, 2], F32)
    carry = k.sbuf("p_carry", [128, 32], F32)
    k.dma("sp", vt[:], vec_d, dst=vt)
    k.op("dve", lambda e: e.scalar_tensor_tensor(out=a2[:], in0=vt[:, 2, :], scalar=1.0, in1=vt[:, 1, :], op0=ALU.add, op1=ALU.mult), reads=[vt], writes=[a2])
    k.op("pool", lambda e: e.memset(carry[:], 0.0), writes=[carry])
    with k.scope():
        ystage = k.sbuf("pa_ystage", [128, NSUB, D], F32)
        yT16 = k.sbuf("pa_yT16", [128, 16, TT], BF16)
        xacc = k.sbuf("pa_xacc", [128, 16, TT], F32)
        tmp = k.sbuf("pa_tmp", [128, 16, TT], F32)
        rstd = k.sbuf("pa_rstd", [128, TT], F32)
        wo = [k.sbuf(f"pa_wo{i}", [128, 16, 128], BF16) for i in range(3)]
        wr = k.sbuf("pa_wr", [128, 16, 36], F32); br = k.sbuf("pa_br", [128, 36], F32)
        hrow = [k.sbuf(f"pa_hrow{i}", [128, D], F32) for i in range(2)]
        lg = k.sbuf("pa_lg", [128, 36], F32)
        m4 = k.sbuf("pa_m4", [128, 1], F32); s4 = k.sbuf("pa_s4", [128, 1], F32); e4 = k.sbuf("pa_e4", [128, 4], F32)
        oh4 = k.sbuf("pa_oh4", [128, 4], F32)
        fs = k.sbuf("pa_fs", [128, 8], F32); mx8 = k.sbuf("pa_mx8", [128, 8], F32); e8 = k.sbuf("pa_e8", [128, 8], F32)
        selA = k.sbuf("pa_selA", [128, 8], F32); selB = k.sbuf("pa_selB", [128, 8], F32)
        nl1 = k.sbuf("pa_nl1", [128, 1], F32); den = k.sbuf("pa_den", [128, 1], F32); e2v = k.sbuf("pa_e2v", [128, 1], F32)
        Msum = k.sbuf("pa_Msum", [128, 32], F32)
        p_t = [k.psum(f"pa_p_t{i}", [128, 512]) for i in range(2)]
        p_m = [k.psum(f"pa_p_m{i}", [128, TT]) for i in range(2)]
        p_s = k.psum("pa_p_s", [128, TT])
        p_r = k.psum("pa_p_r", [128, 36])
        p_c = k.psum("pa_p_c", [128, 64])
        k.dma("sp", wr[:], wr_d, dst=wr); k.dma("sp", br[:], br_d, dst=br)
        ti = 0; mi = 0; hi = 0
        for t in range(T // TT):
            t0 = t * TT
            k.dma("sp", xacc[:], xTv[:, :, t0:t0 + TT], src=xsrc, dst=xacc)
            k.dma("sp", ystage[:], y_d.t[t0:t0 + TT, :].rearrange("(s p) d -> p s d", p=128), src=y_d, dst=ystage)
            for c in range(16):
                pt = p_t[ti % 2]; ti += 1
                for s_ in range(NSUB):
                    k.op("pe", lambda e: e.transpose(out=pt[:, s_ * 128:(s_ + 1) * 128], in_=ystage[:, s_, c * 128:(c + 1) * 128], identity=C["ident32"][:]),
                         reads=[ystage, C["ident32"]], writes=[pt], inc=(s_ == NSUB - 1))
                if c % 2 == 0:
                    k.op("act", lambda e: e.copy(out=yT16[:, c, :], in_=pt[:, 0:TT]), reads=[pt], writes=[yT16])
                else:
                    k.op("dve", lambda e: e.tensor_copy(out=yT16[:, c, :], in_=pt[:, 0:TT]), reads=[pt], writes=[yT16])
            for d in range(16):
                w_ = wo[mi % 3]; pm = p_m[mi % 2]; mi += 1
                k.dma("pool", w_[:], wo_d[d], dst=w_)
                for c in range(16):
                    k.op("pe", lambda e: e.matmul(pm[:], lhsT=w_[:, c, :], rhs=yT16[:, c, :], start=(c == 0), stop=(c == 15)), reads=[w_, yT16], writes=[pm], inc=(c == 15))
                k.op("dve", lambda e: e.scalar_tensor_tensor(out=xacc[:, d, :], in0=pm[:], scalar=vt[:, 0, d:d + 1], in1=xacc[:, d, :], op0=ALU.mult, op1=ALU.add),
                     reads=[pm, vt, xacc], writes=[xacc])
            k.dma("sp", X1v[:, :, t0:t0 + TT], xacc[:], src=xacc, dst=X1)
            k.op("act", lambda e: e.activation(out=tmp[:], in_=xacc[:], func=AF.Square), reads=[xacc], writes=[tmp])
            for c in range(16):
                k.op("pe", lambda e: e.matmul(p_s[:], lhsT=C["ones32"][:], rhs=tmp[:, c, :], start=(c == 0), stop=(c == 15)), reads=[C["ones32"], tmp], writes=[p_s], inc=(c == 15))
            k.op("dve", lambda e: e.tensor_scalar(out=rstd[:], in0=p_s[:], scalar1=1.0 / D, scalar2=1e-6, op0=ALU.mult, op1=ALU.add), reads=[p_s], writes=[rstd])
            k.op("act", lambda e: e.activation(out=rstd[:], in_=rstd[:], func=AF.Sqrt), reads=[rstd], writes=[rstd])
            k.op("dve", lambda e: e.reciprocal(out=rstd[:], in_=rstd[:]), reads=[rstd], writes=[rstd])
            for c in range(16):
                k.op("dve", lambda e: e.scalar_tensor_tensor(out=tmp[:, c, :], in0=xacc[:, c, :], scalar=a2[:, c:c + 1], in1=rstd[:], op0=ALU.mult, op1=ALU.mult),
                     reads=[xacc, a2, rstd], writes=[tmp])
                k.op("act", lambda e: e.activation(out=tmp[:, c, :], in_=tmp[:, c, :], func=AF.Identity, bias=vt[:, 3, c:c + 1]), reads=[tmp, vt], writes=[tmp])
            for s_ in range(NSUB):
                n = t * NSUB + s_
                ts_ = slice(s_ * 128, (s_ + 1) * 128)
                for c in range(16):
                    k.op("pe", lambda e: e.matmul(p_r[:], lhsT=tmp[:, c, ts_], rhs=wr[:, c, :], start=(c == 0), stop=(c == 15)), reads=[tmp, wr], writes=[p_r], inc=(c == 15))
                k.op("dve", lambda e: e.tensor_tensor(out=lg[:], in0=p_r[:], in1=br[:], op=ALU.add), reads=[p_r, br], writes=[lg])
                k.op("dve", lambda e: e.tensor_reduce(out=m4[:], in_=lg[:, 0:4], op=ALU.max, axis=AX.X), reads=[lg], writes=[m4])
                k.op("dve", lambda e: e.tensor_scalar(out=oh4[:], in0=lg[:, 0:4], scalar1=m4[:, 0:1], scalar2=None, op0=ALU.is_ge), reads=[lg, m4], writes=[oh4])
                k.op("dve", lambda e: e.tensor_scalar(out=e4[:], in0=lg[:, 0:4], scalar1=m4[:, 0:1], scalar2=None, op0=ALU.subtract), reads=[lg, m4], writes=[e4])
                k.op("act", lambda e: e.activation(out=e4[:], in_=e4[:], func=AF.Exp), reads=[e4], writes=[e4])
                k.op("dve", lambda e: e.tensor_reduce(out=s4[:], in_=e4[:], op=ALU.add, axis=AX.X), reads=[e4], writes=[s4])
                k.op("dve", lambda e: e.tensor_scalar(out=fs[:], in0=lg[:, 4:12], scalar1=oh4[:, 0:1], scalar2=None, op0=ALU.mult), reads=[lg, oh4], writes=[fs])
                for g in range(1, 4):
                    k.op("dve", lambda e: e.scalar_tensor_tensor(out=fs[:], in0=lg[:, 4 + 8 * g:12 + 8 * g], scalar=oh4[:, g:g + 1], in1=fs[:], op0=ALU.mult, op1=ALU.add),
                         reads=[lg, oh4, fs], writes=[fs])
                k.op("dve", lambda e: e.max(out=mx8[:], in_=fs[:]), reads=[fs], writes=[mx8])
                k.op("dve", lambda e: e.tensor_scalar(out=selA[:], in0=fs[:], scalar1=mx8[:, 0:1], scalar2=None, op0=ALU.is_ge), reads=[fs, mx8], writes=[selA])
                k.op("dve", lambda e: e.tensor_scalar(out=selB[:], in0=fs[:], scalar1=mx8[:, 1:2], scalar2=None, op0=ALU.is_ge), reads=[fs, mx8], writes=[selB])
                k.op("dve", lambda e: e.tensor_tensor(out=selB[:], in0=selB[:], in1=selA[:], op=ALU.subtract), reads=[selB, selA], writes=[selB])
                k.op("dve", lambda e: e.tensor_tensor(out=e2v[:], in0=mx8[:, 1:2], in1=mx8[:, 0:1], op=ALU.subtract), reads=[mx8], writes=[e2v])
                k.op("act", lambda e: e.activation(out=e2v[:], in_=e2v[:], func=AF.Exp), reads=[e2v], writes=[e2v])
                k.op("dve", lambda e: e.scalar_tensor_tensor(out=den[:], in0=e2v[:], scalar=1.0, in1=s4[:], op0=ALU.add, op1=ALU.mult), reads=[e2v, s4], writes=[den])
                k.op("dve", lambda e: e.reciprocal(out=GT[:, n, 0:1], in_=den[:]), reads=[den], writes=[GT])
                k.op("dve", lambda e: e.tensor_tensor(out=GT[:, n, 1:2], in0=GT[:, n, 0:1], in1=e2v[:], op=ALU.mult), reads=[GT, e2v], writes=[GT])
                for g in range(4):
                    k.op("dve", lambda e: e.tensor_scalar(out=AB[:, n, 8 * g:8 * g + 8], in0=selA[:], scalar1=oh4[:, g:g + 1], scalar2=None, op0=ALU.mult), reads=[selA, oh4], writes=[AB])
                    k.op("dve", lambda e: e.tensor_scalar(out=AB[:, n, 32 + 8 * g:40 + 8 * g], in0=selB[:], scalar1=oh4[:, g:g + 1], scalar2=None, op0=ALU.mult), reads=[selB, oh4], writes=[AB])
                k.op("dve", lambda e: e.tensor_tensor(out=Msum[:], in0=AB[:, n, 0:32], in1=AB[:, n, 32:64], op=ALU.add), reads=[AB], writes=[Msum])
                k.op("pe", lambda e: e.matmul(p_c[:, 0:32], lhsT=C["Lst32"][:], rhs=Msum[:], start=True, stop=True), reads=[C["Lst32"], Msum], writes=[p_c])
                k.op("pe", lambda e: e.matmul(p_c[:, 32:64], lhsT=C["ones32"][:], rhs=Msum[:], start=True, stop=True), reads=[C["ones32"], Msum], writes=[p_c])
                k.op("dve", lambda e: e.tensor_tensor(out=CUM[:, n, :], in0=p_c[:, 0:32], in1=carry[:], op=ALU.add), reads=[p_c, carry], writes=[CUM])
                k.op("dve", lambda e: e.tensor_tensor(out=carry[:], in0=carry[:], in1=p_c[:, 32:64], op=ALU.add), reads=[carry, p_c], writes=[carry])
                hr = hrow[hi % 2]; hi += 1
                for q4 in range(4):
                    pt = p_t[ti % 2]; ti += 1
                    for cc in range(4):
                        c = q4 * 4 + cc
                        k.op("pe", lambda e: e.transpose(out=pt[:, cc * 128:(cc + 1) * 128], in_=tmp[:, c, ts_], identity=C["ident32"][:]), reads=[tmp, C["ident32"]], writes=[pt], inc=(cc == 3))
                    if q4 % 2 == 0:
                        k.op("act", lambda e: e.copy(out=hr[:, q4 * 512:(q4 + 1) * 512], in_=pt[:]), reads=[pt], writes=[hr])
                    else:
                        k.op("dve", lambda e: e.tensor_copy(out=hr[:, q4 * 512:(q4 + 1) * 512], in_=pt[:]), reads=[pt], writes=[hr])
                k.dma("sp", H2.t[t0 + s_ * 128:t0 + (s_ + 1) * 128, :], hr[:], src=hr, dst=H2)
    pstart = k.sbuf("p_pstart", [128, 32], F32)
    IDXW = k.sbuf("p_IDXW", [128, NB, 4], I32)
    DEST = k.sbuf("p_DEST", [128, NT, 2], I32)
    with k.scope():
        pc = k.sbuf("pb_pc", [128, 32], F32); pend = k.sbuf("pb_pend", [128, 32], F32)
        onesr = k.sbuf("pb_onesr", [128, 32], F32)
        I128 = k.sbuf("pb_I128", [128, NB], F32); BE = k.sbuf("pb_BE", [128, NB], F32)
        fcp = k.sbuf("pb_fcp", [128, 4], F32); idxf = k.sbuf("pb_idxf", [128, NB, 4], F32)
        dsum = k.sbuf("pb_dsum", [128, 32], F32); dj = k.sbuf("pb_dj", [128, 32], F32); dtmp = k.sbuf("pb_dtmp", [128, NT, 2], F32)
        k.op("pool", lambda e: e.iota(I128[:], pattern=[[BS, NB]], base=0, channel_multiplier=0, allow_small_or_imprecise_dtypes=True), writes=[I128])
        for ex in range(32):
            k.op("dve", lambda e: e.tensor_scalar(out=BE[:], in0=I128[:], scalar1=carry[:, ex:ex + 1], scalar2=0.0, op0=ALU.is_lt, op1=ALU.add, accum_out=pc[:, ex:ex + 1]),
                 reads=[I128, carry], writes=[BE, pc])
        k.op("dve", lambda e: e.tensor_scalar(out=pc[:], in0=pc[:], scalar1=float(BS), scalar2=None, op0=ALU.mult), reads=[pc], writes=[pc])
        k.op("pool", lambda e: e.memset(onesr[:], 1.0), writes=[onesr])
        k.op("dve", lambda e: e.tensor_tensor_scan(out=pend[:], data0=onesr[:], data1=pc[:], initial=0.0, op0=ALU.mult, op1=ALU.add), reads=[onesr, pc], writes=[pend])
        k.op("dve", lambda e: e.tensor_tensor(out=pstart[:], in0=pend[:], in1=pc[:], op=ALU.subtract), reads=[pend, pc], writes=[pstart])
        k.op("pool", lambda e: e.memset(BE[:], 0.0), writes=[BE])
        for ex in range(32):
            k.op("dve", lambda e: e.scalar_tensor_tensor(out=BE[:], in0=I128[:], scalar=pend[:, ex:ex + 1], in1=BE[:], op0=ALU.is_ge, op1=ALU.add), reads=[I128, pend, BE], writes=[BE])
        k.op("dve", lambda e: e.tensor_scalar(out=BE[:], in0=BE[:], scalar1=31.0, scalar2=512.0, op0=ALU.min, op1=ALU.mult), reads=[BE], writes=[BE])
        k.op("pool", lambda e: e.iota(fcp[:], pattern=[[128, 4]], base=0, channel_multiplier=1, allow_small_or_imprecise_dtypes=True), writes=[fcp])
        k.op("dve", lambda e: e.tensor_tensor(out=idxf[:], in0=BE[:].unsqueeze(2).to_broadcast([128, NB, 4]), in1=fcp[:].unsqueeze(1).to_broadcast([128, NB, 4]), op=ALU.add), reads=[BE, fcp], writes=[idxf])
        k.op("dve", lambda e: e.tensor_copy(out=IDXW[:], in_=idxf[:]), reads=[idxf], writes=[IDXW])
        for n in range(NT):
            k.op("dve", lambda e: e.tensor_tensor(out=dsum[:], in0=CUM[:, n, :], in1=pstart[:], op=ALU.add), reads=[CUM, pstart], writes=[dsum])
            for j in range(2):
                k.op("dve", lambda e: e.tensor_tensor(out=dj[:], in0=dsum[:], in1=AB[:, n, 32 * j:32 * j + 32], op=ALU.mult), reads=[dsum, AB], writes=[dj])
                k.op("dve", lambda e: e.tensor_reduce(out=dtmp[:, n, j:j + 1], in_=dj[:], op=ALU.add, axis=AX.X), reads=[dj], writes=[dtmp])
        k.op("dve", lambda e: e.tensor_copy(out=DEST[:], in_=dtmp[:]), reads=[dtmp], writes=[DEST])
    with k.scope():
        zr = k.sbuf("pc_zero", [128, D], F32)
        k.op("pool", lambda e: e.memset(zr[:], 0.0), writes=[zr])
        for r in range(NB * SUB):
            k.dma("sp", Xd.t[r * 128:(r + 1) * 128, :], zr[:], src=zr, dst=Xd)
    with k.scope():
        hr = [k.sbuf(f"pc_hr{i}", [128, D], F32) for i in range(3)]
        for n in range(NT):
            h_ = hr[n % 3]
            k.dma("pool", h_[:], H2.t[n * 128:(n + 1) * 128, :], src=H2, dst=h_)
            for j in range(2):
                k.indirect("pool", Xd, h_, DEST, out_ap=Xd.t[:, :], out_idx=DEST[:, n, j:j + 1], in_ap=h_[:])
    with k.scope():
        xb = [k.sbuf(f"pd_xb{i}", [128, D], F32) for i in range(2)]
        xbT = [k.sbuf(f"pd_xbT{i}", [128, 16, BS], BF16) for i in range(2)]
        wg = [k.sbuf(f"pd_wg{fc}", [128, 2048], BF16) for fc in range(4)]
        wu = [k.sbuf(f"pd_wu{fc}", [128, 2048], BF16) for fc in range(4)]
        wd = [[k.sbuf(f"pd_wd{i}_{fc}", [128, 2048], BF16) for fc in range(4)] for i in range(2)]
        sg = k.sbuf("pd_sg", [128, BS], F32)
        hid = [k.sbuf(f"pd_hid{i}", [128, 4, BS], BF16) for i in range(2)]
        yb = [k.sbuf(f"pd_yb{i}", [128, D], F32) for i in range(2)]
        p_t = [k.psum(f"pd_p_t{i}", [128, 512]) for i in range(2)]
        p_g = [k.psum(f"pd_p_g{i}", [128, BS]) for i in range(2)]
        p_u = [k.psum(f"pd_p_u{i}", [128, BS]) for i in range(2)]
        p_d = [k.psum(f"pd_p_d{i}", [128, 512]) for i in range(2)]
        ti = 0; gi = 0; di = 0; xi = 0; yi = 0
        for i in range(NB):
            xT_ = xbT[i % 2]; wd_ = wd[i % 2]; hid_ = hid[i % 2]
            for fc in range(4):
                k.indirect("pool", wg[fc], None, IDXW, out_ap=wg[fc][:], in_ap=wg_d[:, :], in_idx=IDXW[:, i, fc:fc + 1])
                k.indirect("pool", wu[fc], None, IDXW, out_ap=wu[fc][:], in_ap=wu_d[:, :], in_idx=IDXW[:, i, fc:fc + 1])
            for fc in range(4):
                k.indirect("pool", wd_[fc], None, IDXW, out_ap=wd_[fc][:], in_ap=wd_d[:, :], in_idx=IDXW[:, i, fc:fc + 1])
            for s_ in range(SUB):
                x_ = xb[xi % 2]; xi += 1
                k.dma("sp", x_[:], Xd.t[i * BS + s_ * 128:i * BS + (s_ + 1) * 128, :], src=Xd, dst=x_)
                for q4 in range(4):
                    pt = p_t[ti % 2]; ti += 1
                    for cc in range(4):
                        c = q4 * 4 + cc
                        k.op("pe", lambda e: e.transpose(out=pt[:, cc * 128:(cc + 1) * 128], in_=x_[:, c * 128:(c + 1) * 128], identity=C["ident32"][:]), reads=[x_, C["ident32"]], writes=[pt], inc=(cc == 3))
                    if q4 % 2 == 0:
                        k.op("act", lambda e: e.copy(out=xT_[:, q4 * 4:(q4 + 1) * 4, s_ * 128:(s_ + 1) * 128], in_=pt[:].rearrange("p (c t) -> p c t", c=4)), reads=[pt], writes=[xT_])
                    else:
                        k.op("dve", lambda e: e.tensor_copy(out=xT_[:, q4 * 4:(q4 + 1) * 4, s_ * 128:(s_ + 1) * 128], in_=pt[:].rearrange("p (c t) -> p c t", c=4)), reads=[pt], writes=[xT_])
            for fc in range(4):
                pg = p_g[gi % 2]; pu = p_u[gi % 2]; gi += 1
                for c in range(16):
                    k.op("pe", lambda e: e.matmul(pg[:], lhsT=wg[fc][:, c * 128:(c + 1) * 128], rhs=xT_[:, c, :], start=(c == 0), stop=(c == 15)), reads=[wg[fc], xT_], writes=[pg], inc=(c == 15))
                for c in range(16):
                    k.op("pe", lambda e: e.matmul(pu[:], lhsT=wu[fc][:, c * 128:(c + 1) * 128], rhs=xT_[:, c, :], start=(c == 0), stop=(c == 15)), reads=[wu[fc], xT_], writes=[pu], inc=(c == 15))
                k.op("act", lambda e: e.activation(out=sg[:], in_=pg[:], func=AF.Silu), reads=[pg], writes=[sg])
                k.op("dve", lambda e: e.tensor_tensor(out=hid_[:, fc, :], in0=sg[:], in1=pu[:], op=ALU.mult), reads=[sg, pu], writes=[hid_])
            for s_ in range(SUB):
                y_ = yb[yi % 2]; yi += 1
                for dq in range(4):
                    pd = p_d[di % 2]; di += 1
                    for fc in range(4):
                        k.op("pe", lambda e: e.matmul(pd[:], lhsT=hid_[:, fc, s_ * 128:(s_ + 1) * 128], rhs=wd_[fc][:, dq * 512:(dq + 1) * 512], start=(fc == 0), stop=(fc == 3)), reads=[hid_, wd_[fc]], writes=[pd], inc=(fc == 3))
                    if dq % 2 == 0:
                        k.op("act", lambda e: e.copy(out=y_[:, dq * 512:(dq + 1) * 512], in_=pd[:]), reads=[pd], writes=[y_])
                    else:
                        k.op("dve", lambda e: e.tensor_copy(out=y_[:, dq * 512:(dq + 1) * 512], in_=pd[:]), reads=[pd], writes=[y_])
                k.dma("sp", Yd.t[i * BS + s_ * 128:i * BS + (s_ + 1) * 128, :], y_[:], src=y_, dst=Yd)
    with k.scope():
        y1 = [k.sbuf(f"pe_y1{i}", [128, D], F32) for i in range(2)]
        y2 = [k.sbuf(f"pe_y2{i}", [128, D], F32) for i in range(2)]
        x1 = [k.sbuf(f"pe_x1{i}", [128, 16, 128], F32) for i in range(2)]
        sq = k.sbuf("pe_sq", [128, 16, 128], F32); rs = k.sbuf("pe_rs", [128, 128], F32)
        fn17 = k.sbuf("pe_fn17", [128, 17], F32); fnv = k.sbuf("pe_fn", [128, 16], F32)
        p_t = [k.psum(f"pe_p_t{i}", [128, 512]) for i in range(2)]
        p_s = k.psum("pe_p_s", [128, 128])
        alp = k.sbuf("pe_alp", [128, 1], F32); oma = k.sbuf("pe_oma", [128, 1], F32); scl = k.sbuf("pe_scl", [128, 128], F32)
        k.dma("sp", fn17[:], fn_d, dst=fn17)
        k.op("dve", lambda e: e.tensor_copy(out=alp[:], in_=fn17[:, 16:17]), reads=[fn17], writes=[alp])
        k.op("dve", lambda e: e.tensor_scalar(out=fnv[:], in0=fn17[:, 0:16], scalar1=alp[:, 0:1], scalar2=None, op0=ALU.mult), reads=[fn17, alp], writes=[fnv])
        k.op("dve", lambda e: e.tensor_scalar(out=oma[:], in0=alp[:], scalar1=-1.0, scalar2=1.0, op0=ALU.mult, op1=ALU.add), reads=[alp], writes=[oma])
        ti = 0
        for n in range(NT):
            a_ = y1[n % 2]; b_ = y2[n % 2]; x_ = x1[n % 2]
            k.indirect("pool", a_, Yd, DEST, out_ap=a_[:], in_ap=Yd.t[:, :], in_idx=DEST[:, n, 0:1])
            k.indirect("pool", b_, Yd, DEST, out_ap=b_[:], in_ap=Yd.t[:, :], in_idx=DEST[:, n, 1:2])
            k.dma("sp", x_[:], X1v[:, :, n * 128:(n + 1) * 128], src=X1, dst=x_)
            k.op("dve", lambda e: e.tensor_scalar(out=a_[:], in0=a_[:], scalar1=GT[:, n, 0:1], scalar2=None, op0=ALU.mult), reads=[a_, GT], writes=[a_])
            k.op("dve", lambda e: e.scalar_tensor_tensor(out=a_[:], in0=b_[:], scalar=GT[:, n, 1:2], in1=a_[:], op0=ALU.mult, op1=ALU.add), reads=[b_, GT, a_], writes=[a_])
            for q4 in range(4):
                pt = p_t[ti % 2]; ti += 1
                for cc in range(4):
                    c = q4 * 4 + cc
                    k.op("pe", lambda e: e.transpose(out=pt[:, cc * 128:(cc + 1) * 128], in_=a_[:, c * 128:(c + 1) * 128], identity=C["ident32"][:]), reads=[a_, C["ident32"]], writes=[pt], inc=(cc == 3))
                for cc in range(4):
                    c = q4 * 4 + cc
                    k.op("dve", lambda e: e.scalar_tensor_tensor(out=x_[:, c, :], in0=pt[:, cc * 128:(cc + 1) * 128], scalar=vt[:, 4, c:c + 1], in1=x_[:, c, :], op0=ALU.mult, op1=ALU.add),
                         reads=[pt, vt, x_], writes=[x_])
            if True:
                k.op("act", lambda e: e.activation(out=sq[:], in_=x_[:], func=AF.Square), reads=[x_], writes=[sq])
                for c in range(16):
                    k.op("pe", lambda e: e.matmul(p_s[:], lhsT=C["ones32"][:], rhs=sq[:, c, :], start=(c == 0), stop=(c == 15)), reads=[C["ones32"], sq], writes=[p_s], inc=(c == 15))
                k.op("dve", lambda e: e.tensor_scalar(out=rs[:], in0=p_s[:], scalar1=1.0 / D, scalar2=1e-6, op0=ALU.mult, op1=ALU.add), reads=[p_s], writes=[rs])
                k.op("act", lambda e: e.activation(out=rs[:], in_=rs[:], func=AF.Sqrt), reads=[rs], writes=[rs])
                k.op("dve", lambda e: e.reciprocal(out=rs[:], in_=rs[:]), reads=[rs], writes=[rs])
                for c in range(16):
                    k.op("dve", lambda e: e.tensor_scalar(out=scl[:], in0=rs[:], scalar1=fnv[:, c:c + 1], scalar2=oma[:, 0:1], op0=ALU.mult, op1=ALU.add), reads=[rs, fnv, oma], writes=[scl])
                    k.op("dve", lambda e: e.tensor_tensor(out=x_[:, c, :], in0=x_[:, c, :], in1=scl[:], op=ALU.mult), reads=[x_, scl], writes=[x_])
            k.dma("sp", outv[:, :, n * 128:(n + 1) * 128], x_[:], src=x_, dst=out_d)


D = 2048
TM_BLOCKS = [(0, 512), (512, 512), (1024, 512), (1536, 512), (2048, 512), (2560, 16)]


def emit_pre(k, C, T, xT_d, wfm_d, wtm_d, wtm_last_d, vec_d, pfm, ptm, xsrc=None):
    TT = 512
    xTv = xT_d.rearrange("(c p) t -> p c t", p=128)
    vt = k.sbuf("r_vt", [128, 3, 16], F32); acol = k.sbuf("r_acol", [128, 16], F32)
    xt = k.sbuf("r_xt", [128, 16, TT], F32); tmp = k.sbuf("r_tmp", [128, 16, TT], F32)
    hT = k.sbuf("r_hT", [128, 16, TT], BF16); rstd = k.sbuf("r_rstd", [128, TT], F32)
    wb = [k.sbuf(f"r_wb{i}", [128, 16, 128], BF16) for i in range(3)]
    wt = [k.sbuf(f"r_wt{i}", [128, 16, 512], BF16) for i in range(2)]
    ob = [k.sbuf(f"r_ob{i}", [128, TT], F32) for i in range(3)]
    pss = k.psum("r_pss", [128, TT]); psm = [k.psum(f"r_psm{i}", [128, TT]) for i in range(4)]
    k.dma("sp", vt[:], vec_d, dst=vt)
    k.op("dve", lambda e: e.scalar_tensor_tensor(out=acol[:], in0=vt[:, 1, :], scalar=1.0, in1=vt[:, 0, :], op0=ALU.add, op1=ALU.mult), reads=[vt], writes=[acol])
    wi = 0; ti = 0
    for t in range(T // TT):
        t0 = t * TT
        k.dma("sp", xt[:], xTv[:, :, t0:t0 + TT], src=xsrc, dst=xt)
        k.op("act", lambda e: e.activation(out=tmp[:], in_=xt[:], func=AF.Square), reads=[xt], writes=[tmp])
        for c in range(16):
            k.op("pe", lambda e: e.matmul(pss[:], lhsT=C["ones32"][:], rhs=tmp[:, c, :], start=(c == 0), stop=(c == 15)), reads=[C["ones32"], tmp], writes=[pss], inc=(c == 15))
        k.op("dve", lambda e: e.tensor_scalar(out=rstd[:], in0=pss[:], scalar1=1.0 / D, scalar2=1e-6, op0=ALU.mult, op1=ALU.add), reads=[pss], writes=[rstd])
        k.op("act", lambda e: e.activation(out=rstd[:], in_=rstd[:], func=AF.Sqrt), reads=[rstd], writes=[rstd])
        k.op("dve", lambda e: e.reciprocal(out=rstd[:], in_=rstd[:]), reads=[rstd], writes=[rstd])
        for c in range(16):
            k.op("dve", lambda e: e.scalar_tensor_tensor(out=tmp[:, c, :], in0=xt[:, c, :], scalar=acol[:, c:c + 1], in1=rstd[:], op0=ALU.mult, op1=ALU.mult), reads=[xt, acol, rstd], writes=[tmp])
            k.op("act", lambda e: e.activation(out=hT[:, c, :], in_=tmp[:, c, :], func=AF.Identity, bias=vt[:, 2, c:c + 1]), reads=[tmp, vt], writes=[hT])
        for m in range(20):
            wj = wb[wi % 3]; p = psm[wi % 4]; o = ob[wi % 3]; wi += 1
            k.dma("pool", wj[:], wfm_d[m], dst=wj)
            for c in range(16):
                k.op("pe", lambda e: e.matmul(p[:], lhsT=wj[:, c, :], rhs=hT[:, c, :], start=(c == 0), stop=(c == 15)), reads=[wj, hT], writes=[p], inc=(c == 15))
            if m % 2 == 0:
                k.op("dve", lambda e: e.tensor_copy(out=o[:], in_=p[:]), reads=[p], writes=[o])
            else:
                k.op("act", lambda e: e.copy(out=o[:], in_=p[:]), reads=[p], writes=[o])
            k.dma("sp", pfm.t[m * 128:(m + 1) * 128, t0:t0 + TT], o[:], src=o, dst=pfm)
        for bi, (c0, n) in enumerate(TM_BLOCKS):
            wj = wt[ti % 2]; ti += 1
            if n == 512:
                k.dma("pool", wj[:], wtm_d[bi], dst=wj)
            else:
                k.dma("pool", wj[:, :, 0:n], wtm_last_d, dst=wj)
            for s_ in range(TT // 128):
                p = psm[wi % 4]; o = ob[wi % 3]; wi += 1
                for c in range(16):
                    k.op("pe", lambda e: e.matmul(p[:, 0:n], lhsT=hT[:, c, s_ * 128:(s_ + 1) * 128], rhs=wj[:, c, 0:n], start=(c == 0), stop=(c == 15)), reads=[hT, wj], writes=[p], inc=(c == 15))
                if s_ % 2 == 0:
                    k.op("dve", lambda e: e.tensor_copy(out=o[:, 0:n], in_=p[:, 0:n]), reads=[p], writes=[o])
                else:
                    k.op("act", lambda e: e.copy(out=o[:, 0:n], in_=p[:, 0:n]), reads=[p], writes=[o])
                k.dma("sp", ptm.t[t0 + s_ * 128:t0 + (s_ + 1) * 128, c0:c0 + n], o[:, 0:n], src=o, dst=ptm)


NCH = 16


T_SEQ = 8192
LAYER_INS = [("wfm", [20, 128, 16, 128]), ("wtm", [5, 128, 16, 512]), ("wtl", [128, 16, 16]),
             ("sgu_lng", [128, 512]), ("sgu_lnb", [128, 512]), ("sgu_wT", [4, 128, 128]), ("sgu_bs", [128, 4]),
             ("ssd_convw", [2, 128, 6, 4]), ("ssd_convb", [2, 128, 6, 1]), ("ssd_dtb", [128, 16]), ("ssd_alog", [128, 16]),
             ("ssd_dskip", [128, 16]), ("ssd_norm", [128, 1024]),
             ("wo", [16, 128, 16, 128]), ("wr", [128, 16, 36]), ("br", [128, 36]),
             ("wg", [32 * 512, 2048]), ("wu", [32 * 512, 2048]), ("wd", [32 * 512, 2048]), ("fn", [128, 17])]


def emit_mod(k, C, wada_d, cT_d, bias_d, ncols_d, VEC1, VEC2, nlayers):
    NJ = nlayers * 96
    ct = k.sbuf("m_ct", [128, 16, 2], F32); bt = k.sbuf("m_bt", [128, NJ], F32); res = k.sbuf("m_res", [128, NJ], F32)
    nct = k.sbuf("m_nct", [128, 2 * nlayers, 16], F32)
    wb = [k.sbuf(f"m_wb{i}", [128, 16, 128], F32) for i in range(3)]
    ps = [k.psum(f"m_ps{i}", [128, 2]) for i in range(2)]
    v1 = k.sbuf("m_v1", [128, nlayers, 3, 16], F32); v2 = k.sbuf("m_v2", [128, nlayers, 5, 16], F32)
    k.dma("sp", ct[:], cT_d, dst=ct); k.dma("sp", bt[:], bias_d, dst=bt); k.dma("sp", nct[:], ncols_d, dst=nct)
    k.op("act", lambda e: e.activation(out=ct[:], in_=ct[:], func=AF.Silu), reads=[ct], writes=[ct])
    for j in range(NJ):
        wj = wb[j % 3]; p = ps[j % 2]
        k.dma("sp", wj[:], wada_d[j], dst=wj)
        for c in range(16):
            k.op("pe", lambda e: e.matmul(p[:], lhsT=wj[:, c, :], rhs=ct[:, c, :], start=(c == 0), stop=(c == 15)), reads=[wj, ct], writes=[p], inc=(c == 15))
        k.op("dve", lambda e: e.tensor_tensor(out=res[:, j:j + 1], in0=p[:, 0:1], in1=bt[:, j:j + 1], op=ALU.add), reads=[p, bt], writes=[res])
    for l in range(nlayers):
        b0 = l * 96
        for (dst, slot, srcap) in ((v1, 0, nct[:, l, :]), (v1, 1, res[:, b0 + 16:b0 + 32]), (v1, 2, res[:, b0:b0 + 16]),
                                   (v2, 0, res[:, b0 + 32:b0 + 48]), (v2, 1, nct[:, nlayers + l, :]), (v2, 2, res[:, b0 + 64:b0 + 80]),
                                   (v2, 3, res[:, b0 + 48:b0 + 64]), (v2, 4, res[:, b0 + 80:b0 + 96])):
            k.op("dve", lambda e: e.tensor_copy(out=dst[:, l, slot, :], in_=srcap), reads=[res, nct], writes=[dst])
    k.dma("sp", VEC1.t.rearrange("l p a c -> p l a c"), v1[:], src=v1, dst=VEC1)
    k.dma("sp", VEC2.t.rearrange("l p a c -> p l a c"), v2[:], src=v2, dst=VEC2)


def build_fused(T=T_SEQ, nlayers=4):
    nc = bass.Bass("TRN2", target_bir_lowering=False)
    k = K(nc, same_engine_sync=True)
    A = lambda n, s: nc.dram_tensor(n, s, F32, kind="ExternalInput").ap()
    xT = A("xT", [2048, T])
    cos = A("cos", [32, T]); sin = A("sin", [32, T])
    wada = A("wada", [nlayers * 96, 128, 16, 128]); cTd = A("cT", [128, 16, 2]); biasd = A("bias", [128, nlayers * 96]); ncols = A("ncols", [128, 2 * nlayers, 16])
    L = [{n: A(f"{n}_l{l}", shp) for n, shp in LAYER_INS} for l in range(nlayers)]
    out = k.dram("xo", [2048, T], F32, kind="ExternalOutput")
    VEC1 = k.dram("vec1", [nlayers, 128, 3, 16], F32); VEC2 = k.dram("vec2", [nlayers, 128, 5, 16], F32)
    XB = [k.dram("xres", [2048, T], F32) for _ in range(2)]
    pfm = k.dram("pfm", [2560, T], F32); ptm = k.dram("ptm", [T, 2576], F32); y_d = k.dram("ymix", [T, 2048], F32)
    C = emit_consts(k)
    with k.scope():
        emit_mod(k, C, wada, cTd, biasd, ncols, VEC1, VEC2, nlayers)
    for l in range(nlayers):
        W = L[l]
        xin = xT if l == 0 else XB[(l - 1) % 2].t
        xout = out if l == nlayers - 1 else XB[l % 2]
        with k.scope():
            emit_pre(k, C, T, xin, W["wfm"], W["wtm"], W["wtl"], VEC1.t[l], pfm, ptm)
        with k.scope():
            emit_sgu(k, C, T, ptm.t, 0, {"lng": W["sgu_lng"], "lnb": W["sgu_lnb"], "wT": W["sgu_wT"], "bs": W["sgu_bs"]}, y_d, 0)
        with k.scope():
            emit_attn(k, C, T, pfm.t[0:512, :], pfm.t[512:1024, :], ptm.t[:, 1024:1536], cos, sin, y_d, 512, 4)
        with k.scope():
            emit_ssd(k, C, T, pfm.t[1024:2560, :], ptm.t, 1536, 2560,
                     {"convw": W["ssd_convw"], "convb": W["ssd_convb"], "dtb": W["ssd_dtb"], "alog": W["ssd_alog"], "dskip": W["ssd_dskip"], "norm": W["ssd_norm"]}, y_d, 1024)
        with k.scope():
            emit_post(k, C, T, xin, y_d, W["wo"], VEC2.t[l], W["wr"], W["br"], W["wg"], W["wu"], W["wd"], xout, final=True, fn_d=W["fn"])
    k.finish(outs=[out]); k.close()
    return nc


def _wl(Wc, nb=128):
    n = Wc.shape[1] // nb
    return np.ascontiguousarray(Wc.reshape(16, 128, n, nb).transpose(2, 1, 0, 3))


def _col(v):
    return v.reshape(16, 128).T


def _rep(v):
    return np.ascontiguousarray(np.broadcast_to(np.asarray(v, np.float32).reshape(1, -1), (128, v.size)))


def _wgl(w):
    return np.ascontiguousarray(w.reshape(32, 16, 128, 4, 128).transpose(0, 3, 2, 1, 4).reshape(32 * 512, 2048))


def _conv_tiles(a):
    out = []
    for g in range(2):
        rows = [a[g * 512 + i * 128:g * 512 + (i + 1) * 128] for i in range(4)] + [a[1024 + g * 128:1024 + (g + 1) * 128], a[1280 + g * 128:1280 + (g + 1) * 128]]
        out.append(np.stack(rows, axis=1))
    return np.ascontiguousarray(np.stack(out, 0)).astype(np.float32)


def _rot_tables(T):
    pos = np.arange(T, dtype=np.float32)
    inv = (np.float32(500000.0) ** (-np.arange(0, 32, 2, dtype=np.float32) / np.float32(32))).astype(np.float32)
    ang = pos[:, None] * inv[None, :]
    c, s = np.cos(ang).astype(np.float32), np.sin(ang).astype(np.float32)
    return np.ascontiguousarray(np.concatenate([c, c], 1).T), np.ascontiguousarray(np.concatenate([-s, s], 1).T)


def _layer_shared(l, p, last):
    f = lambda n: np.asarray(p[n][l], np.float32)
    w_in = f("w_in")
    W_fm = np.concatenate([w_in[:, 1024:2048], w_in[:, 3584:5120]], 1)
    W_tm = np.concatenate([w_in[:, 0:1024], w_in[:, 2048:2560], w_in[:, 2560:3584], w_in[:, 5120:5136]], 1)
    Wr = np.concatenate([f("w_coarse")] + [f("w_fine")[g] for g in range(4)], axis=1)
    return {
        "wfm": _wl(W_fm), "wtm": _wl(W_tm[:, :2560], 512), "wtl": np.ascontiguousarray(W_tm[:, 2560:].reshape(16, 128, 16).transpose(1, 0, 2)),
        "sgu_lng": _rep(f("sgu_ln_g")), "sgu_lnb": _rep(f("sgu_ln_b")), "sgu_wT": np.ascontiguousarray(f("sgu_w").transpose(0, 2, 1)),
        "sgu_bs": np.ascontiguousarray(f("sgu_b").T),
        "ssd_convw": _conv_tiles(np.ascontiguousarray(f("conv_w").T)), "ssd_convb": _conv_tiles(f("conv_b")[:, None]),
        "ssd_dtb": _rep(f("dt_bias")), "ssd_alog": _rep(f("a_log")), "ssd_dskip": _rep(f("d_skip")), "ssd_norm": _rep(f("ssm_norm")),
        "wo": _wl(f("w_out")), "wr": np.ascontiguousarray(Wr.reshape(16, 128, 36).transpose(1, 0, 2)),
        "br": _rep(np.concatenate([f("b_coarse"), f("b_fine").reshape(-1)])),
        "wg": _wgl(f("w_gate")), "wu": _wgl(f("w_up")), "wd": np.ascontiguousarray(f("w_down").reshape(32 * 512, 2048)),
        "fn": np.ascontiguousarray(np.concatenate([_col(np.asarray(p["final_norm"], np.float32)), np.full((128, 1), 1.0 if last else 0.0, np.float32)], 1)),
    }


def _fused_inputs(p, T=T_SEQ, layers=(0, 1, 2, 3), nb=4, xT_list=None, total_layers=4):
    x = np.asarray(p["x"], np.float32); c = np.asarray(p["c"], np.float32)
    cosT, sinT = _rot_tables(T)
    w_ada = np.asarray(p["w_ada"], np.float32); b_ada = np.asarray(p["b_ada"], np.float32)
    nl = len(layers)
    W = np.concatenate([w_ada[l] for l in layers], axis=1)
    shared = {"cos": cosT, "sin": sinT, "wada": _wl(W),
              "bias": np.ascontiguousarray(np.concatenate([b_ada[l] for l in layers]).reshape(nl * 96, 128).T),
              "ncols": np.ascontiguousarray(np.stack([_col(np.asarray(p["norm1"][l], np.float32)) for l in layers] +
                                                     [_col(np.asarray(p["norm2"][l], np.float32)) for l in layers], 1))}
    for li, l in enumerate(layers):
        for n, a in _layer_shared(l, p, last=(l == total_layers - 1)).items():
            shared[f"{n}_l{li}"] = a
    ins = []
    for b in range(nb):
        d = dict(shared)
        d["xT"] = np.ascontiguousarray(x[b, :T].T) if xT_list is None else xT_list[b]
        cc = _col(c[b])[:, :, None]
        d["cT"] = np.ascontiguousarray(np.concatenate([cc, cc], 2))
        ins.append(d)
    return ins


LAUNCHES = [[0, 1], [2, 3]]
_NC_CACHE = {}


def kernel(**p):
    from concourse.bass_utils import run_bass_kernel_spmd
    xT = None
    for layers in LAUNCHES:
        nl = len(layers)
        if nl not in _NC_CACHE:
            _NC_CACHE[nl] = build_fused(T_SEQ, nl)
        ins = _fused_inputs(p, T_SEQ, layers, 4, xT)
        res = run_bass_kernel_spmd(_NC_CACHE[nl], ins, core_ids=[0, 1, 2, 3])
        xT = [res.results[b]["xo"] for b in range(4)]
    return np.ascontiguousarray(np.stack([xT[b].T for b in range(4)], 0)).astype(np.float32)
```

```python
import numpy as np
from contextlib import ExitStack
import concourse.bass as bass
import concourse.mybir as mybir

F32 = mybir.dt.float32
BF16 = mybir.dt.bfloat16
I32 = mybir.dt.int32
U32 = mybir.dt.uint32
AF = mybir.ActivationFunctionType
ALU = mybir.AluOpType
AX = mybir.AxisListType


class Buf:
    __slots__ = ("t", "name", "w", "rd", "dw_sem", "dw_cnt", "dr_sem", "dr_cnt", "dw_base", "dr_base")

    def __init__(self, t, name):
        self.t = t
        self.name = name
        self.w = None
        self.rd = {}
        self.dw_sem = None
        self.dw_cnt = 0
        self.dr_sem = None
        self.dr_cnt = 0
        self.dw_base = 0
        self.dr_base = 0

    def __getitem__(self, idx):
        return self.t[idx]


class DBuf:
    def __init__(self, t, name):
        self.t = t
        self.name = name
        self.pw = {}
        self.pr = {}

    def __getitem__(self, idx):
        return self.t[idx]


class K:
    ENG = ("pe", "dve", "act", "pool", "sp")

    def __init__(self, nc, same_engine_sync=True):
        self.nc = nc
        self.es = ExitStack()
        self.E = {"pe": nc.tensor, "dve": nc.vector, "act": nc.scalar, "pool": nc.gpsimd, "sp": nc.sync}
        self.sem = {e: self.es.enter_context(nc.semaphore("sem_" + e)) for e in self.ENG}
        self.cnt = {e: 0 for e in self.ENG}
        self.seen = {}
        self.same = same_engine_sync
        self.cur = self.es
        self.dma_all = {}
        self.dbufs = []
        self.sem_pool = []
        self.scope_bufs = [[]]
        self.uid = 0
        self.nsem = len(self.ENG)

    def sbuf(self, name, shape, dt):
        self.uid += 1
        name = name + "_" + str(self.uid)
        t = self.cur.enter_context(self.nc.sbuf_tensor(name, list(shape), dt))
        b = Buf(t, name)
        self.scope_bufs[-1].append(b)
        return b

    def psum(self, name, shape, dt=F32):
        self.uid += 1
        name = name + "_" + str(self.uid)
        t = self.cur.enter_context(self.nc.psum_tensor(name, list(shape), dt))
        return Buf(t, name)

    def barrier(self):
        for e in self.ENG:
            for e2 in self.ENG:
                if e2 != e and e2 != "sp" and self.cnt[e2]:
                    self._wait(e, self.sem[e2], self.cnt[e2], "E" + e2)
            for key, (sem, val) in self.dma_all.items():
                self._wait(e, sem, val, key)

    def scope(self):
        from contextlib import contextmanager

        @contextmanager
        def _s():
            prev = self.cur
            st = ExitStack()
            self.cur = st
            self.scope_bufs.append([])
            try:
                yield
            finally:
                self.barrier()
                for b in self.scope_bufs.pop():
                    if b.dw_sem is not None:
                        self.sem_pool.append((b.dw_sem, b.dw_base + 16 * b.dw_cnt))
                    if b.dr_sem is not None:
                        self.sem_pool.append((b.dr_sem, b.dr_base + 16 * b.dr_cnt))
                self.dma_all = {}
                for d in self.dbufs:
                    d.pw = {}
                    d.pr = {}
                self.seen = {kk: v for kk, v in self.seen.items() if kk[1].startswith("E")}
                self.cur = prev
                st.close()
        return _s()

    def dram(self, name, shape, dt, kind="Internal"):
        if kind == "Internal":
            self.uid += 1
            name = name + "_" + str(self.uid)
        t = self.nc.dram_tensor(name, list(shape), dt, kind=kind).ap()
        d = DBuf(t, name)
        self.dbufs.append(d)
        return d

    def view(self, buf_t, name):
        return Buf(buf_t, name)

    def _newsem(self, name):
        if self.sem_pool:
            return self.sem_pool.pop()
        self.nsem += 1
        return self.es.enter_context(self.nc.semaphore("s" + str(self.nsem))), 0

    def _wait(self, eng, sem, val, key):
        if val <= 0:
            return
        k = (eng, key)
        if self.seen.get(k, 0) >= val:
            return
        self.seen[k] = val
        self.E[eng].wait_ge(sem, val)

    def _wait_eng(self, eng, dep):
        if dep is None:
            return
        e2, c = dep
        if e2 == eng and (not self.same or eng in ("pe", "sp")):
            return
        self._wait(eng, self.sem[e2], c, "E" + e2)

    def _deps_read(self, eng, b):
        self._wait_eng(eng, b.w)
        if b.dw_cnt:
            self._wait(eng, b.dw_sem, b.dw_base + 16 * b.dw_cnt, "DW" + b.name)

    def _deps_write(self, eng, b):
        self._wait_eng(eng, b.w)
        for e2, c in b.rd.items():
            if e2 != eng:
                self._wait_eng(eng, (e2, c))
        if b.dw_cnt:
            self._wait(eng, b.dw_sem, b.dw_base + 16 * b.dw_cnt, "DW" + b.name)
        if b.dr_cnt:
            self._wait(eng, b.dr_sem, b.dr_base + 16 * b.dr_cnt, "DR" + b.name)

    def op(self, eng, fn, reads=(), writes=(), inc=True):
        for b in reads:
            self._deps_read(eng, b)
        for b in writes:
            self._deps_write(eng, b)
        ins = fn(self.E[eng])
        if inc:
            self.cnt[eng] += 1
            ins.then_inc(self.sem[eng], 1)
            c = self.cnt[eng]
        else:
            c = self.cnt[eng] + 1
        for b in reads:
            b.rd[eng] = c
        for b in writes:
            b.w = (eng, c)
            b.rd = {}
        return ins

    def dma(self, q, out_ap, in_ap, src=None, dst=None, **kw):
        s_sb = isinstance(src, Buf)
        d_sb = isinstance(dst, Buf)
        assert s_sb != d_sb, "exactly one side must be an SBUF Buf"
        if d_sb:
            self._deps_write(q, dst)
            if isinstance(src, DBuf):
                for sname, (sem, val) in src.pw.items():
                    self._wait(q, sem, val, sname)
        else:
            self._deps_read(q, src)
            if isinstance(dst, DBuf):
                for d in (dst.pw, dst.pr):
                    for sname, (sem, val) in d.items():
                        self._wait(q, sem, val, sname)
        ins = self.E[q].dma_start(out=out_ap, in_=in_ap, **kw)
        if d_sb:
            if dst.dw_sem is None:
                dst.dw_sem, dst.dw_base = self._newsem("dw_" + dst.name)
            dst.dw_cnt += 1
            ins.then_inc(dst.dw_sem, 16)
            self.dma_all["DW" + dst.name] = (dst.dw_sem, dst.dw_base + 16 * dst.dw_cnt)
            dst.rd = {}
            if isinstance(src, DBuf):
                src.pr["DW" + dst.name] = (dst.dw_sem, dst.dw_base + 16 * dst.dw_cnt)
        else:
            if src.dr_sem is None:
                src.dr_sem, src.dr_base = self._newsem("dr_" + src.name)
            src.dr_cnt += 1
            ins.then_inc(src.dr_sem, 16)
            self.dma_all["DR" + src.name] = (src.dr_sem, src.dr_base + 16 * src.dr_cnt)
            if isinstance(dst, DBuf):
                dst.pw["DR" + src.name] = (src.dr_sem, src.dr_base + 16 * src.dr_cnt)
                dst.pr = {}
        return ins

    def indirect(self, q, sb, dram, idxbuf, out_ap, in_ap, out_idx=None, in_idx=None, bound=None):
        g = self.E["pool"]
        self._deps_read("pool", idxbuf)
        if in_idx is not None:
            dst = sb
            self._deps_write("pool", dst)
            if isinstance(dram, DBuf):
                for sname, (sem, val) in dram.pw.items():
                    self._wait("pool", sem, val, sname)
            ins = g.indirect_dma_start(out=out_ap, out_offset=None, in_=in_ap, in_offset=bass.IndirectOffsetOnAxis(ap=in_idx, axis=0))
            if dst.dw_sem is None:
                dst.dw_sem, dst.dw_base = self._newsem("dw_" + dst.name)
            dst.dw_cnt += 1
            ins.then_inc(dst.dw_sem, 16)
            self.dma_all["DW" + dst.name] = (dst.dw_sem, dst.dw_base + 16 * dst.dw_cnt)
            dst.rd = {}
            if isinstance(dram, DBuf):
                dram.pr["DW" + dst.name] = (dst.dw_sem, dst.dw_base + 16 * dst.dw_cnt)
        else:
            srcb, dd = dram, sb
            self._deps_read("pool", srcb)
            for sname, (sem, val) in dd.pr.items():
                self._wait("pool", sem, val, sname)
            kw = {}
            if bound is not None:
                kw = dict(bounds_check=bound, oob_is_err=False)
            ins = g.indirect_dma_start(out=out_ap, out_offset=bass.IndirectOffsetOnAxis(ap=out_idx, axis=0), in_=in_ap, in_offset=None, **kw)
            if srcb.dr_sem is None:
                srcb.dr_sem, srcb.dr_base = self._newsem("dr_" + srcb.name)
            srcb.dr_cnt += 1
            ins.then_inc(srcb.dr_sem, 16)
            self.dma_all["DR" + srcb.name] = (srcb.dr_sem, srcb.dr_base + 16 * srcb.dr_cnt)
            dd.pw["DR" + srcb.name] = (srcb.dr_sem, srcb.dr_base + 16 * srcb.dr_cnt)
        idxbuf.rd["pool"] = self.cnt["pool"] + 1
        return ins

    def finish(self, outs=()):
        for d in outs:
            for sname, (sem, val) in d.pw.items():
                self._wait("sp", sem, val, sname)
        for e in self.ENG:
            if e != "sp" and self.cnt[e]:
                self._wait("sp", self.sem[e], self.cnt[e], "E" + e)

    def close(self):
        self.es.close()


import os
DBG = ''


FR = mybir.dt.float32r
NEG = -30000.0


def emit_consts(k):
    C = {}
    C["ones32"] = k.sbuf("c_ones32", [128, 128], F32)
    k.op("pool", lambda e: e.memset(C["ones32"][:], 1.0), writes=[C["ones32"]])
    C["ident32"] = k.sbuf("c_ident32", [128, 128], F32)
    k.op("pool", lambda e: e.memset(C["ident32"][:], 0.0), writes=[C["ident32"]])
    k.op("pool", lambda e: e.affine_select(out=C["ident32"][:], in_=C["ident32"][:], pattern=[[-1, 128]], compare_op=ALU.not_equal,
                                            fill=1.0, base=0, channel_multiplier=1), reads=[C["ident32"]], writes=[C["ident32"]])
    C["ident16"] = k.sbuf("c_ident16", [128, 128], BF16)
    k.op("dve", lambda e: e.tensor_copy(out=C["ident16"][:], in_=C["ident32"][:]), reads=[C["ident32"]], writes=[C["ident16"]])
    C["U32"] = k.sbuf("c_U32", [128, 128], F32)
    k.op("pool", lambda e: e.affine_select(out=C["U32"][:], in_=C["ones32"][:], pattern=[[1, 128]], compare_op=ALU.is_ge,
                                            fill=0.0, base=0, channel_multiplier=-1), reads=[C["ones32"]], writes=[C["U32"]])
    C["Lst32"] = k.sbuf("c_Lst32", [128, 128], F32)
    k.op("pool", lambda e: e.affine_select(out=C["Lst32"][:], in_=C["ones32"][:], pattern=[[1, 128]], compare_op=ALU.is_ge,
                                            fill=0.0, base=-1, channel_multiplier=-1), reads=[C["ones32"]], writes=[C["Lst32"]])
    z32 = k.sbuf("c_z32", [128, 128], F32)
    k.op("pool", lambda e: e.memset(z32[:], 0.0), writes=[z32])
    caus32 = k.sbuf("c_caus32", [128, 128], F32)
    k.op("pool", lambda e: e.affine_select(out=caus32[:], in_=z32[:], pattern=[[1, 128]], compare_op=ALU.is_ge,
                                            fill=NEG, base=0, channel_multiplier=-1), reads=[z32], writes=[caus32])
    C["caus16"] = k.sbuf("c_caus16", [128, 128], BF16)
    k.op("dve", lambda e: e.tensor_copy(out=C["caus16"][:], in_=caus32[:]), reads=[caus32], writes=[C["caus16"]])
    C["sel16"] = k.sbuf("c_sel16", [32, 32, 128], BF16)
    with k.scope():
      sel32 = k.sbuf("c_sel32", [32, 32, 128], F32)
      k.op("pool", lambda e: e.memset(sel32[:], 1.0), writes=[sel32])
      k.op("pool", lambda e: e.affine_select(out=sel32[:], in_=sel32[:], pattern=[[-1, 32], [0, 128]], compare_op=ALU.is_equal,
                                            fill=0.0, base=0, channel_multiplier=1), reads=[sel32], writes=[sel32])
      k.op("dve", lambda e: e.tensor_copy(out=C["sel16"][:], in_=sel32[:]), reads=[sel32], writes=[C["sel16"]])
    return C


def emit_attn(k, C, T, qT_d, kT_d, v_d, cos_d, sin_d, y_d, y_col0, nheads, src=None, dstbuf=None):
    NBLK = T // 256
    NKT = T // 128
    RC = min(T, 2048)
    scale = 128 ** -0.5
    q32 = k.sbuf("a_q32", [128, T], F32); k32 = k.sbuf("a_k32", [128, T], F32)
    q16 = k.sbuf("a_q16", [128, T], BF16); k16 = k.sbuf("a_k16", [128, T], BF16)
    swp = k.sbuf("a_swp", [32, RC], F32); cs = k.sbuf("a_cos", [32, RC], F32); sn = k.sbuf("a_sin", [32, RC], F32)
    rt = k.sbuf("a_rt", [32, RC], F32)
    V1 = k.sbuf("a_V1", [128, NKT, 129], BF16)
    kmean = k.sbuf("a_kmean", [128, NBLK], F32)
    gate = k.sbuf("a_gate", [128, 32], F32)
    mx8 = k.sbuf("a_mx8", [128, 8], F32)
    biasq = k.sbuf("a_biasq", [128, 32], F32)
    maskT = k.sbuf("a_maskT", [32, 256], BF16)
    PT = [k.sbuf(f"a_PT{i}", [128, 256], BF16) for i in range(3)]
    yo = [k.sbuf(f"a_yo{i}", [128, 128], F32) for i in range(2)]
    rec = k.sbuf("a_rec", [128, 1], F32)
    ps_s = [k.psum(f"a_ps_s{i}", [128, 256]) for i in range(2)]
    ps_o = [k.psum(f"a_ps_o{i}", [128, 129]) for i in range(4)]
    ps_g = k.psum("a_ps_g", [128, 32])
    ps_t = k.psum("a_ps_t", [32, 128])
    k.op("pool", lambda e: e.memset(gate[:], -1e30), writes=[gate])
    k.op("pool", lambda e: e.memset(V1[:, :, 128:129], 1.0), writes=[V1])
    si = 0; oi = 0; yi = 0
    for h in range(nheads):
        for (dst32, srcd) in ((q32, qT_d), (k32, kT_d)):
            k.dma("sp", dst32[:], srcd[h * 128:(h + 1) * 128, :], src=src, dst=dst32)
            for c0 in range(0, T, RC):
                k.dma("sp", swp[0:16, :], srcd[h * 128 + 16:h * 128 + 32, c0:c0 + RC], src=src, dst=swp)
                k.dma("sp", swp[16:32, :], srcd[h * 128:h * 128 + 16, c0:c0 + RC], src=src, dst=swp)
                k.dma("sp", cs[:], cos_d[:, c0:c0 + RC], dst=cs)
                k.dma("sp", sn[:], sin_d[:, c0:c0 + RC], dst=sn)
                k.op("dve", lambda e: e.tensor_tensor(out=rt[:], in0=dst32[0:32, c0:c0 + RC], in1=cs[:], op=ALU.mult), reads=[dst32, cs], writes=[rt])
                k.op("dve", lambda e: e.tensor_tensor(out=swp[:], in0=swp[:], in1=sn[:], op=ALU.mult), reads=[swp, sn], writes=[swp])
                k.op("dve", lambda e: e.tensor_tensor(out=dst32[0:32, c0:c0 + RC], in0=rt[:], in1=swp[:], op=ALU.add), reads=[rt, swp], writes=[dst32])
        k.op("act", lambda e: e.copy(out=q16[:], in_=q32[:]), reads=[q32], writes=[q16])
        k.op("act", lambda e: e.copy(out=k16[:], in_=k32[:]), reads=[k32], writes=[k16])
        k.op("dve", lambda e: e.tensor_reduce(out=kmean[:], in_=k32[:].rearrange("p (b s) -> p b s", s=256), op=ALU.add, axis=AX.X),
             reads=[k32], writes=[kmean])
        k.op("dve", lambda e: e.tensor_scalar(out=kmean[:], in0=kmean[:], scalar1=1.0 / 256, scalar2=None, op0=ALU.mult), reads=[kmean], writes=[kmean])
        k.dma("pool", V1[:, :, 0:128], v_d[:, h * 128:(h + 1) * 128].rearrange("(n p) d -> p n d", p=128), src=src, dst=V1)
        for Q in range(NBLK):
            q0 = Q * 256
            use_mask = Q > 3
            if use_mask and True:
                for half in range(2):
                    qs = slice(q0 + half * 128, q0 + half * 128 + 128)
                    k.op("pe", lambda e: e.matmul(ps_g[:, 0:NBLK], lhsT=q32[:, qs], rhs=kmean[:, 0:NBLK], start=True, stop=True), reads=[q32, kmean], writes=[ps_g])
                    k.op("dve", lambda e: e.tensor_copy(out=gate[:, 0:Q], in_=ps_g[:, 0:Q]), reads=[ps_g], writes=[gate])
                    k.op("dve", lambda e: e.max(out=mx8[:], in_=gate[:, 0:max(Q, 8)]), reads=[gate], writes=[mx8])
                    k.op("dve", lambda e: e.tensor_scalar(out=biasq[:], in0=gate[:], scalar1=mx8[:, 2:3], scalar2=1.0, op0=ALU.is_ge, op1=ALU.subtract),
                         reads=[gate, mx8], writes=[biasq])
                    k.op("dve", lambda e: e.tensor_scalar(out=biasq[:], in0=biasq[:], scalar1=-NEG, scalar2=None, op0=ALU.mult), reads=[biasq], writes=[biasq])
                    k.op("pe", lambda e: e.transpose(out=ps_t[:], in_=biasq[:], identity=C["ident32"][:]), reads=[biasq, C["ident32"]], writes=[ps_t])
                    k.op("act", lambda e: e.copy(out=maskT[:, half * 128:(half + 1) * 128], in_=ps_t[:]), reads=[ps_t], writes=[maskT])
            po = [ps_o[(oi + i) % 4] for i in range(2)]; oi += 2
            jobs = [("past", j, kt) for j in range(Q) for kt in range(2)]
            for half in range(2):
                jobs += [("own", half, kt) for kt in range(half + 1)]
            firstjob = [None, None]; lastjob = [None, None]
            for ji, (kind, a, kt) in enumerate(jobs):
                halves = (0, 1) if kind == "past" else (a,)
                for hf in halves:
                    if firstjob[hf] is None:
                        firstjob[hf] = ji
                    lastjob[hf] = ji

            def emit_S(job):
                nonlocal si
                kind, a, kt = job
                ps = ps_s[si % 2]; pt = PT[si % 3]; si += 1
                if kind == "past":
                    kti = a * 2 + kt
                    k.op("pe", lambda e: e.matmul(ps[:], lhsT=k16[:, kti * 128:(kti + 1) * 128], rhs=q16[:, q0:q0 + 256], start=True, stop=not use_mask),
                         reads=[k16, q16], writes=[ps], inc=not use_mask)
                    if use_mask:
                        k.op("pe", lambda e: e.matmul(ps[:], lhsT=C["sel16"][:, a, :], rhs=maskT[:], start=False, stop=True), reads=[C["sel16"], maskT], writes=[ps])
                    k.op("act", lambda e: e.activation(out=pt[:], in_=ps[:], func=AF.Exp, scale=scale), reads=[ps], writes=[pt])
                else:
                    half = a
                    qs = slice(q0 + half * 128, q0 + half * 128 + 128)
                    kti = Q * 2 + kt
                    diag = (kt == half)
                    k.op("pe", lambda e: e.matmul(ps[:, 0:128], lhsT=k16[:, kti * 128:(kti + 1) * 128], rhs=q16[:, qs], start=True, stop=not diag),
                         reads=[k16, q16], writes=[ps], inc=not diag)
                    if diag:
                        k.op("pe", lambda e: e.matmul(ps[:, 0:128], lhsT=C["ident16"][:], rhs=C["caus16"][:], start=False, stop=True),
                             reads=[C["ident16"], C["caus16"]], writes=[ps])
                    k.op("act", lambda e: e.activation(out=pt[:, 0:128], in_=ps[:, 0:128], func=AF.Exp, scale=scale), reads=[ps], writes=[pt])
                return pt

            def emit_PV(ji, job, pt):
                kind, a, kt = job
                if kind == "past":
                    kti = a * 2 + kt
                    for half in range(2):
                        lastf = (lastjob[half] == ji)
                        k.op("pe", lambda e: e.matmul(po[half][:], lhsT=pt[:, half * 128:(half + 1) * 128], rhs=V1[:, kti, :], start=(firstjob[half] == ji), stop=lastf),
                             reads=[pt, V1], writes=[po[half]], inc=lastf)
                else:
                    half = a
                    kti = Q * 2 + kt
                    lastf = (lastjob[half] == ji)
                    k.op("pe", lambda e: e.matmul(po[half][:], lhsT=pt[:, 0:128], rhs=V1[:, kti, :], start=(firstjob[half] == ji), stop=lastf),
                         reads=[pt, V1], writes=[po[half]], inc=lastf)

            pts = {0: emit_S(jobs[0])}
            for ji, job in enumerate(jobs):
                if ji + 1 < len(jobs):
                    pts[ji + 1] = emit_S(jobs[ji + 1])
                emit_PV(ji, job, pts.pop(ji))
            for half in range(2):
                y = yo[yi % 2]; yi += 1
                k.op("dve", lambda e: e.reciprocal(out=rec[:], in_=po[half][:, 128:129]), reads=[po[half]], writes=[rec])
                k.op("dve", lambda e: e.tensor_scalar(out=y[:], in0=po[half][:, 0:128], scalar1=rec[:, 0:1], scalar2=None, op0=ALU.mult), reads=[po[half], rec], writes=[y])
                k.dma("sp", y_d[q0 + half * 128:q0 + half * 128 + 128, y_col0 + h * 128:y_col0 + (h + 1) * 128], y[:], src=y, dst=y_d)


FR = mybir.dt.float32r


def emit_ssd(k, C, T, xbcT_d, ptm_d, z_col0, dt_col0, prm, y_d, y_col0, groups=(0, 1), src=None):
    NCH = T // 128
    cw = k.sbuf("s_cw", [128, 6, 4], F32); cb = k.sbuf("s_cb", [128, 6, 1], F32)
    dtb = k.sbuf("s_dtb", [128, 8], F32); aneg = k.sbuf("s_aneg", [128, 8], F32); dsk = k.sbuf("s_dsk", [128, 8], F32)
    dskx = k.sbuf("s_dskx", [128, 8, 64], F32)
    nrm = k.sbuf("s_nrm", [128, 512], F32)
    cin = [k.sbuf(f"s_cin{i}", [128, 6, 131], F32) for i in range(2)]
    acc = k.sbuf("s_acc", [128, 6, 128], F32); tap = k.sbuf("s_tap", [128, 6, 128], F32)
    xc = k.sbuf("s_xc", [128, 6, 128], F32)
    xcr = k.sbuf("s_xcr", [128, 2, 128], FR)
    x_tm = k.sbuf("s_xtm", [128, 8, 64], F32)
    B_tm = k.sbuf("s_Btm", [128, 128], FR)
    dtr = k.sbuf("s_dtr", [128, 8], F32); dt = k.sbuf("s_dt", [128, 8], F32); dA = k.sbuf("s_dA", [128, 8], F32)
    dArep = k.sbuf("s_dArep", [128, 8, 128], F32)
    acum = k.sbuf("s_acum", [128, 8], F32); tot = k.sbuf("s_tot", [128, 8], F32)
    dec = k.sbuf("s_dec", [128, 8, 128], F32)
    CBm = k.sbuf("s_CBm", [128, 128], F32)
    Mt = k.sbuf("s_Mt", [128, 8, 128], FR)
    xdt = k.sbuf("s_xdt", [128, 8, 64], FR)
    ea = k.sbuf("s_ea", [128, 8], F32); wend = k.sbuf("s_wend", [128, 8], F32); etot = k.sbuf("s_etot", [128, 8], F32)
    xw = k.sbuf("s_xw", [128, 8, 64], FR)
    ST32 = k.sbuf("s_ST32", [128, 8, 64], F32); STr = k.sbuf("s_STr", [128, 8, 64], FR)
    y1 = k.sbuf("s_y1", [128, 8, 64], F32); t2 = k.sbuf("s_t2", [128, 8, 64], F32)
    zt = [k.sbuf(f"s_zt{i}", [128, 512], F32) for i in range(2)]
    junk = k.sbuf("s_junk", [128, 512], F32)
    ss = k.sbuf("s_ss", [128, 1], F32)
    yo = [k.sbuf(f"s_yo{i}", [128, 512], F32) for i in range(2)]
    p_x = k.psum("s_p_x", [128, 512]); p_b = k.psum("s_p_b", [128, 128]); p_cb = k.psum("s_p_cb", [128, 128])
    p_ac = k.psum("s_p_ac", [128, 16]); p_abc = k.psum("s_p_abc", [128, 1024])
    p_y = k.psum("s_p_y", [128, 512]); p_yi = k.psum("s_p_yi", [128, 512])
    for g in groups:
        xr0 = g * 512; br0 = 1024 + g * 128; cr0 = 1280 + g * 128
        k.dma("sp", cw[:], prm["convw"][g], dst=cw)
        k.dma("sp", cb[:], prm["convb"][g], dst=cb)
        k.dma("sp", dtb[:], prm["dtb"][:, g * 8:(g + 1) * 8], dst=dtb)
        k.dma("sp", aneg[:], prm["alog"][:, g * 8:(g + 1) * 8], dst=aneg)
        k.dma("sp", dsk[:], prm["dskip"][:, g * 8:(g + 1) * 8], dst=dsk)
        k.dma("sp", nrm[:], prm["norm"][:, g * 512:(g + 1) * 512], dst=nrm)
        k.op("act", lambda e: e.activation(out=aneg[:], in_=aneg[:], func=AF.Exp), reads=[aneg], writes=[aneg])
        k.op("dve", lambda e: e.tensor_scalar(out=aneg[:], in0=aneg[:], scalar1=-1.0, scalar2=None, op0=ALU.mult), reads=[aneg], writes=[aneg])
        k.op("dve", lambda e: e.tensor_copy(out=dskx[:], in_=dsk[:].unsqueeze(2).to_broadcast([128, 8, 64])), reads=[dsk], writes=[dskx])
        k.op("pool", lambda e: e.memset(ST32[:], 0.0), writes=[ST32])
        for c in range(NCH):
            t0 = c * 128
            ci = cin[c % 2]
            lo = 3 if c == 0 else 0
            if c == 0:
                k.op("pool", lambda e: e.memset(ci[:, :, 0:3], 0.0), writes=[ci])
            k.dma("sp", ci[:, 0:4, lo:131], xbcT_d[xr0:xr0 + 512, t0 - 3 + lo:t0 + 128].rearrange("(i p) t -> p i t", p=128), src=src, dst=ci)
            k.dma("sp", ci[:, 4, lo:131], xbcT_d[br0:br0 + 128, t0 - 3 + lo:t0 + 128], src=src, dst=ci)
            k.dma("sp", ci[:, 5, lo:131], xbcT_d[cr0:cr0 + 128, t0 - 3 + lo:t0 + 128], src=src, dst=ci)
            z_ = zt[c % 2]
            k.dma("sp", z_[:], ptm_d[t0:t0 + 128, z_col0 + g * 512:z_col0 + (g + 1) * 512], src=src, dst=z_)
            k.dma("sp", dtr[:], ptm_d[t0:t0 + 128, dt_col0 + g * 8:dt_col0 + (g + 1) * 8], src=src, dst=dtr)
            k.op("dve", lambda e: e.tensor_tensor(out=acc[:], in0=ci[:, :, 3:131], in1=cw[:, :, 3:4].to_broadcast([128, 6, 128]), op=ALU.mult), reads=[ci, cw], writes=[acc])
            k.op("dve", lambda e: e.tensor_tensor(out=acc[:], in0=acc[:], in1=cb[:].to_broadcast([128, 6, 128]), op=ALU.add), reads=[acc, cb], writes=[acc])
            for j in range(3):
                k.op("pool", lambda e: e.tensor_tensor(out=tap[:], in0=ci[:, :, j:j + 128], in1=cw[:, :, j:j + 1].to_broadcast([128, 6, 128]), op=ALU.mult), reads=[ci, cw], writes=[tap])
                k.op("dve", lambda e: e.tensor_tensor(out=acc[:], in0=acc[:], in1=tap[:], op=ALU.add), reads=[acc, tap], writes=[acc])
            k.op("act", lambda e: e.activation(out=xc[:], in_=acc[:], func=AF.Silu), reads=[acc], writes=[xc])
            k.op("dve", lambda e: e.tensor_copy(out=xcr[:], in_=xc[:, 4:6, :]), reads=[xc], writes=[xcr])
            for i in range(4):
                k.op("pe", lambda e: e.transpose(out=p_x[:, i * 128:(i + 1) * 128], in_=xc[:, i, :], identity=C["ident32"][:]), reads=[xc, C["ident32"]], writes=[p_x], inc=(i == 3))
            k.op("act", lambda e: e.copy(out=x_tm[:].rearrange("p h d -> p (h d)"), in_=p_x[:]), reads=[p_x], writes=[x_tm])
            k.op("pe", lambda e: e.transpose(out=p_b[:], in_=xc[:, 4, :], identity=C["ident32"][:]), reads=[xc, C["ident32"]], writes=[p_b])
            k.op("dve", lambda e: e.tensor_copy(out=B_tm[:], in_=p_b[:]), reads=[p_b], writes=[B_tm])
            k.op("dve", lambda e: e.tensor_tensor(out=dt[:], in0=dtr[:], in1=dtb[:], op=ALU.add), reads=[dtr, dtb], writes=[dt])
            k.op("act", lambda e: e.activation(out=dt[:], in_=dt[:], func=AF.Exp), reads=[dt], writes=[dt])
            k.op("act", lambda e: e.activation(out=dt[:], in_=dt[:], func=AF.Ln, bias=1.0), reads=[dt], writes=[dt])
            k.op("dve", lambda e: e.tensor_tensor(out=dA[:], in0=dt[:], in1=aneg[:], op=ALU.mult), reads=[dt, aneg], writes=[dA])
            k.op("pe", lambda e: e.matmul(p_ac[:, 0:8], lhsT=C["U32"][:], rhs=dA[:], start=True, stop=True), reads=[C["U32"], dA], writes=[p_ac])
            k.op("pe", lambda e: e.matmul(p_ac[:, 8:16], lhsT=C["ones32"][:], rhs=dA[:], start=True, stop=True), reads=[C["ones32"], dA], writes=[p_ac])
            k.op("dve", lambda e: e.tensor_copy(out=acum[:], in_=p_ac[:, 0:8]), reads=[p_ac], writes=[acum])
            k.op("dve", lambda e: e.tensor_copy(out=tot[:], in_=p_ac[:, 8:16]), reads=[p_ac], writes=[tot])
            k.op("dve", lambda e: e.tensor_copy(out=dArep[:], in_=dA[:].unsqueeze(2).to_broadcast([128, 8, 128])), reads=[dA], writes=[dArep])
            for h in range(8):
                k.op("pe", lambda e: e.matmul(p_abc[:, h * 128:(h + 1) * 128], lhsT=dArep[:, h, :], rhs=C["U32"][:], start=True, stop=True),
                     reads=[dArep, C["U32"]], writes=[p_abc], inc=(h == 7))
            k.op("dve", lambda e: e.tensor_tensor(out=dec[:], in0=p_abc[:].rearrange("p (h l) -> p h l", h=8), in1=acum[:].unsqueeze(2).to_broadcast([128, 8, 128]), op=ALU.subtract),
                 reads=[p_abc, acum], writes=[dec])
            k.op("dve", lambda e: e.tensor_scalar(out=dec[:], in0=dec[:], scalar1=0.0, scalar2=None, op0=ALU.min), reads=[dec], writes=[dec])
            k.op("act", lambda e: e.activation(out=dec[:], in_=dec[:], func=AF.Exp), reads=[dec], writes=[dec])
            k.op("pe", lambda e: e.matmul(p_cb[:], lhsT=xcr[:, 0, :], rhs=xcr[:, 1, :], start=True, stop=True), reads=[xcr], writes=[p_cb])
            k.op("dve", lambda e: e.tensor_tensor(out=CBm[:], in0=p_cb[:], in1=C["U32"][:], op=ALU.mult), reads=[p_cb, C["U32"]], writes=[CBm])
            k.op("dve", lambda e: e.tensor_tensor(out=Mt[:], in0=dec[:], in1=CBm[:].unsqueeze(1).to_broadcast([128, 8, 128]), op=ALU.mult), reads=[dec, CBm], writes=[Mt])
            k.op("pool", lambda e: e.tensor_tensor(out=xdt[:], in0=x_tm[:], in1=dt[:].unsqueeze(2).to_broadcast([128, 8, 64]), op=ALU.mult), reads=[x_tm, dt], writes=[xdt])
            for h in range(8):
                k.op("pe", lambda e: e.matmul(p_y[:, h * 64:(h + 1) * 64], lhsT=Mt[:, h, :], rhs=xdt[:, h, :], start=True, stop=True), reads=[Mt, xdt], writes=[p_y], inc=(h == 7))
            k.op("dve", lambda e: e.tensor_copy(out=STr[:], in_=ST32[:]), reads=[ST32], writes=[STr])
            k.op("pe", lambda e: e.matmul(p_yi[:], lhsT=xcr[:, 1, :], rhs=STr[:].rearrange("p h d -> p (h d)"), start=True, stop=True), reads=[xcr, STr], writes=[p_yi])
            k.op("act", lambda e: e.activation(out=ea[:], in_=acum[:], func=AF.Exp), reads=[acum], writes=[ea])
            k.op("dve", lambda e: e.tensor_tensor(out=y1[:], in0=p_yi[:].rearrange("p (h d) -> p h d", h=8), in1=ea[:].unsqueeze(2).to_broadcast([128, 8, 64]), op=ALU.mult), reads=[p_yi, ea], writes=[y1])
            k.op("dve", lambda e: e.tensor_tensor(out=y1[:], in0=y1[:], in1=p_y[:].rearrange("p (h d) -> p h d", h=8), op=ALU.add), reads=[y1, p_y], writes=[y1])
            k.op("pool", lambda e: e.tensor_tensor(out=t2[:], in0=x_tm[:], in1=dskx[:], op=ALU.mult), reads=[x_tm, dskx], writes=[t2])
            k.op("dve", lambda e: e.tensor_tensor(out=y1[:], in0=y1[:], in1=t2[:], op=ALU.add), reads=[y1, t2], writes=[y1])
            k.op("act", lambda e: e.activation(out=z_[:], in_=z_[:], func=AF.Silu), reads=[z_], writes=[z_])
            k.op("dve", lambda e: e.tensor_tensor(out=y1[:].rearrange("p h d -> p (h d)"), in0=y1[:].rearrange("p h d -> p (h d)"), in1=z_[:], op=ALU.mult), reads=[y1, z_], writes=[y1])
            k.op("act", lambda e: e.activation(out=junk[:], in_=y1[:].rearrange("p h d -> p (h d)"), func=AF.Square, accum_out=ss[:]), reads=[y1], writes=[junk, ss])
            k.op("dve", lambda e: e.tensor_scalar(out=ss[:], in0=ss[:], scalar1=1.0 / 512, scalar2=1e-6, op0=ALU.mult, op1=ALU.add), reads=[ss], writes=[ss])
            k.op("act", lambda e: e.activation(out=ss[:], in_=ss[:], func=AF.Sqrt), reads=[ss], writes=[ss])
            k.op("dve", lambda e: e.reciprocal(out=ss[:], in_=ss[:]), reads=[ss], writes=[ss])
            y_ = yo[c % 2]
            k.op("dve", lambda e: e.scalar_tensor_tensor(out=y_[:], in0=y1[:].rearrange("p h d -> p (h d)"), scalar=ss[:, 0:1], in1=nrm[:], op0=ALU.mult, op1=ALU.mult), reads=[y1, ss, nrm], writes=[y_])
            k.dma("sp", y_d[t0:t0 + 128, y_col0 + g * 512:y_col0 + (g + 1) * 512], y_[:], src=y_, dst=y_d)
            k.op("dve", lambda e: e.tensor_tensor(out=wend[:], in0=tot[:], in1=acum[:], op=ALU.subtract), reads=[tot, acum], writes=[wend])
            k.op("act", lambda e: e.activation(out=wend[:], in_=wend[:], func=AF.Exp), reads=[wend], writes=[wend])
            k.op("dve", lambda e: e.tensor_tensor(out=wend[:], in0=wend[:], in1=dt[:], op=ALU.mult), reads=[wend, dt], writes=[wend])
            k.op("pool", lambda e: e.tensor_tensor(out=xw[:], in0=x_tm[:], in1=wend[:].unsqueeze(2).to_broadcast([128, 8, 64]), op=ALU.mult), reads=[x_tm, wend], writes=[xw])
            k.op("pe", lambda e: e.matmul(p_x[:], lhsT=B_tm[:], rhs=xw[:].rearrange("p h d -> p (h d)"), start=True, stop=True), reads=[B_tm, xw], writes=[p_x])
            k.op("act", lambda e: e.activation(out=etot[:], in_=tot[:], func=AF.Exp), reads=[tot], writes=[etot])
            k.op("dve", lambda e: e.tensor_tensor(out=ST32[:], in0=ST32[:], in1=etot[:].unsqueeze(2).to_broadcast([128, 8, 64]), op=ALU.mult), reads=[ST32, etot], writes=[ST32])
            k.op("dve", lambda e: e.tensor_tensor(out=ST32[:], in0=ST32[:], in1=p_x[:].rearrange("p (h d) -> p h d", h=8), op=ALU.add), reads=[ST32, p_x], writes=[ST32])


def emit_sgu(k, C, T, ptm_d, uv_col0, prm, y_d, y_col0, src=None):
    NCH = T // 128
    lng = k.sbuf("g_lng", [128, 512], F32); lnb = k.sbuf("g_lnb", [128, 512], F32)
    wT = k.sbuf("g_wT", [128, 4, 128], F32); bs = k.sbuf("g_bs", [128, 4], F32)
    uv = [k.sbuf(f"g_uv{i}", [128, 1024], F32) for i in range(2)]
    guv = k.sbuf("g_guv", [128, 1024], F32)
    st = k.sbuf("g_st", [128, 6], F32); mv = k.sbuf("g_mv", [128, 2], F32); rs = k.sbuf("g_rs", [128, 1], F32)
    vn = k.sbuf("g_vn", [128, 512], F32)
    yo = [k.sbuf(f"g_yo{i}", [128, 512], F32) for i in range(2)]
    ps = k.psum("g_ps", [128, 512])
    k.dma("sp", lng[:], prm["lng"], dst=lng); k.dma("sp", lnb[:], prm["lnb"], dst=lnb)
    k.dma("sp", wT[:], prm["wT"].rearrange("g s t -> s g t"), dst=wT); k.dma("sp", bs[:], prm["bs"], dst=bs)
    k.op("pool", lambda e: e.affine_select(out=wT[:], in_=wT[:], pattern=[[0, 4], [1, 128]], compare_op=ALU.is_ge, fill=0.0, base=0, channel_multiplier=-1),
         reads=[wT], writes=[wT])
    for c in range(NCH):
        t0 = c * 128
        uv_ = uv[c % 2]; y_ = yo[c % 2]
        k.dma("sp", uv_[:], ptm_d[t0:t0 + 128, uv_col0:uv_col0 + 1024], src=src, dst=uv_)
        k.op("act", lambda e: e.activation(out=guv[:], in_=uv_[:], func=AF.Gelu_apprx_tanh), reads=[uv_], writes=[guv])
        k.op("dve", lambda e: e.bn_stats(out=st[:], in_=guv[:, 512:1024]), reads=[guv], writes=[st])
        k.op("dve", lambda e: e.bn_aggr(out=mv[:], in_=st[:]), reads=[st], writes=[mv])
        k.op("dve", lambda e: e.tensor_scalar(out=rs[:], in0=mv[:, 1:2], scalar1=1e-6, scalar2=None, op0=ALU.add), reads=[mv], writes=[rs])
        k.op("act", lambda e: e.activation(out=rs[:], in_=rs[:], func=AF.Sqrt), reads=[rs], writes=[rs])
        k.op("dve", lambda e: e.reciprocal(out=rs[:], in_=rs[:]), reads=[rs], writes=[rs])
        k.op("dve", lambda e: e.tensor_scalar(out=vn[:], in0=guv[:, 512:1024], scalar1=mv[:, 0:1], scalar2=rs[:, 0:1], op0=ALU.subtract, op1=ALU.mult), reads=[guv, mv, rs], writes=[vn])
        k.op("dve", lambda e: e.tensor_tensor(out=vn[:], in0=vn[:], in1=lng[:], op=ALU.mult), reads=[vn, lng], writes=[vn])
        k.op("dve", lambda e: e.tensor_tensor(out=vn[:], in0=vn[:], in1=lnb[:], op=ALU.add), reads=[vn, lnb], writes=[vn])
        for g in range(4):
            k.op("pe", lambda e: e.matmul(ps[:, g * 128:(g + 1) * 128], lhsT=wT[:, g, :], rhs=vn[:, g * 128:(g + 1) * 128], start=True, stop=True), reads=[wT, vn], writes=[ps], inc=(g == 3))
        for g in range(4):
            k.op("dve", lambda e: e.scalar_tensor_tensor(out=y_[:, g * 128:(g + 1) * 128], in0=ps[:, g * 128:(g + 1) * 128], scalar=bs[:, g:g + 1], in1=guv[:, g * 128:(g + 1) * 128],
                                                          op0=ALU.add, op1=ALU.mult), reads=[ps, bs, guv], writes=[y_])
        k.dma("sp", y_d[t0:t0 + 128, y_col0:y_col0 + 512], y_[:], src=y_, dst=y_d)


FR = mybir.dt.float32r
U32 = mybir.dt.uint32
I32 = mybir.dt.int32
D = 2048


def emit_post(k, C, T, xT_d, y_d, wo_d, vec_d, wr_d, br_d, wg_d, wu_d, wd_d, out_d, final=False, fn_d=None, xsrc=None):
    NT = T // 128
    BS = 512
    SUB = BS // 128
    NB = 2 * T // BS + 32
    TT = 256
    NSUB = TT // 128
    xTv = xT_d.rearrange("(c p) t -> p c t", p=128)
    X1 = k.dram("p_X1", [D, T], F32); X1v = X1.t.rearrange("(c p) t -> p c t", p=128)
    H2 = k.dram("p_H2", [T, D], F32)
    Xd = k.dram("p_Xd", [NB * BS, D], F32)
    Yd = k.dram("p_Yd", [NB * BS, D], F32)
    outv = out_d.t.rearrange("(c p) t -> p c t", p=128)
    vt = k.sbuf("p_vt", [128, 5, 16], F32); a2 = k.sbuf("p_a2", [128, 16], F32)
    AB = k.sbuf("p_AB", [128, NT, 64], F32)
    CUM = k.sbuf("p_CUM", [128, NT, 32], F32)
    GT = k.sbuf("p_GT", [128, NT, 2], F32)
    carry = k.sbuf("p_carry", [128, 32], F32)
    k.dma("sp", vt[:], vec_d, dst=vt)
    k.op("dve", lambda e: e.scalar_tensor_tensor(out=a2[:], in0=vt[:, 2, :], scalar=1.0, in1=vt[:, 1, :], op0=ALU.add, op1=ALU.mult), reads=[vt], writes=[a2])
    k.op("pool", lambda e: e.memset(carry[:], 0.0), writes=[carry])
    with k.scope():
        ystage = k.sbuf("pa_ystage", [128, NSUB, D], F32)
        yT16 = k.sbuf("pa_yT16", [128, 16, TT], BF16)
        xacc = k.sbuf("pa_xacc", [128, 16, TT], F32)
        tmp = k.sbuf("pa_tmp", [128, 16, TT], F32)
        rstd = k.sbuf("pa_rstd", [128, TT], F32)
        wo = [k.sbuf(f"pa_wo{i}", [128, 16, 128], BF16) for i in range(3)]
        wr = k.sbuf("pa_wr", [128, 16, 36], F32); br = k.sbuf("pa_br", [128, 36], F32)
        hrow = [k.sbuf(f"pa_hrow{i}", [128, D], F32) for i in range(2)]
        lg = k.sbuf("pa_lg", [128, 36], F32)
        m4 = k.sbuf("pa_m4", [128, 1], F32); s4 = k.sbuf("pa_s4", [128, 1], F32); e4 = k.sbuf("pa_e4", [128, 4], F32)
        oh4 = k.sbuf("pa_oh4", [128, 4], F32)
        fs = k.sbuf("pa_fs", [128, 8], F32); mx8 = k.sbuf("pa_mx8", [128, 8], F32); e8 = k.sbuf("pa_e8", [128, 8], F32)
        selA = k.sbuf("pa_selA", [128, 8], F32); selB = k.sbuf("pa_selB", [128, 8], F32)
        nl1 = k.sbuf("pa_nl1", [128, 1], F32); den = k.sbuf("pa_den", [128, 1], F32); e2v = k.sbuf("pa_e2v", [128, 1], F32)
        Msum = k.sbuf("pa_Msum", [128, 32], F32)
        p_t = [k.psum(f"pa_p_t{i}", [128, 512]) for i in range(2)]
        p_m = [k.psum(f"pa_p_m{i}", [128, TT]) for i in range(2)]
        p_s = k.psum("pa_p_s", [128, TT])
        p_r = k.psum("pa_p_r", [128, 36])
        p_c = k.psum("pa_p_c", [128, 64])
        k.dma("sp", wr[:], wr_d, dst=wr); k.dma("sp", br[:], br_d, dst=br)
        ti = 0; mi = 0; hi = 0
        for t in range(T // TT):
            t0 = t * TT
            k.dma("sp", xacc[:], xTv[:, :, t0:t0 + TT], src=xsrc, dst=xacc)
            k.dma("sp", ystage[:], y_d.t[t0:t0 + TT, :].rearrange("(s p) d -> p s d", p=128), src=y_d, dst=ystage)
            for c in range(16):
                pt = p_t[ti % 2]; ti += 1
                for s_ in range(NSUB):
                    k.op("pe", lambda e: e.transpose(out=pt[:, s_ * 128:(s_ + 1) * 128], in_=ystage[:, s_, c * 128:(c + 1) * 128], identity=C["ident32"][:]),
                         reads=[ystage, C["ident32"]], writes=[pt], inc=(s_ == NSUB - 1))
                if c % 2 == 0:
                    k.op("act", lambda e: e.copy(out=yT16[:, c, :], in_=pt[:, 0:TT]), reads=[pt], writes=[yT16])
                else:
                    k.op("dve", lambda e: e.tensor_copy(out=yT16[:, c, :], in_=pt[:, 0:TT]), reads=[pt], writes=[yT16])
            for d in range(16):
                w_ = wo[mi % 3]; pm = p_m[mi % 2]; mi += 1
                k.dma("pool", w_[:], wo_d[d], dst=w_)
                for c in range(16):
                    k.op("pe", lambda e: e.matmul(pm[:], lhsT=w_[:, c, :], rhs=yT16[:, c, :], start=(c == 0), stop=(c == 15)), reads=[w_, yT16], writes=[pm], inc=(c == 15))
                k.op("dve", lambda e: e.scalar_tensor_tensor(out=xacc[:, d, :], in0=pm[:], scalar=vt[:, 0, d:d + 1], in1=xacc[:, d, :], op0=ALU.mult, op1=ALU.add),
                     reads=[pm, vt, xacc], writes=[xacc])
            k.dma("sp", X1v[:, :, t0:t0 + TT], xacc[:], src=xacc, dst=X1)
            k.op("act", lambda e: e.activation(out=tmp[:], in_=xacc[:], func=AF.Square), reads=[xacc], writes=[tmp])
            for c in range(16):
                k.op("pe", lambda e: e.matmul(p_s[:], lhsT=C["ones32"][:], rhs=tmp[:, c, :], start=(c == 0), stop=(c == 15)), reads=[C["ones32"], tmp], writes=[p_s], inc=(c == 15))
            k.op("dve", lambda e: e.tensor_scalar(out=rstd[:], in0=p_s[:], scalar1=1.0 / D, scalar2=1e-6, op0=ALU.mult, op1=ALU.add), reads=[p_s], writes=[rstd])
            k.op("act", lambda e: e.activation(out=rstd[:], in_=rstd[:], func=AF.Sqrt), reads=[rstd], writes=[rstd])
            k.op("dve", lambda e: e.reciprocal(out=rstd[:], in_=rstd[:]), reads=[rstd], writes=[rstd])
            for c in range(16):
                k.op("dve", lambda e: e.scalar_tensor_tensor(out=tmp[:, c, :], in0=xacc[:, c, :], scalar=a2[:, c:c + 1], in1=rstd[:], op0=ALU.mult, op1=ALU.mult),
                     reads=[xacc, a2, rstd], writes=[tmp])
                k.op("act", lambda e: e.activation(out=tmp[:, c, :], in_=tmp[:, c, :], func=AF.Identity, bias=vt[:, 3, c:c + 1]), reads=[tmp, vt], writes=[tmp])
            for s_ in range(NSUB):
                n = t * NSUB + s_
                ts_ = slice(s_ * 128, (s_ + 1) * 128)
                for c in range(16):
                    k.op("pe", lambda e: e.matmul(p_r[:], lhsT=tmp[:, c, ts_], rhs=wr[:, c, :], start=(c == 0), stop=(c == 15)), reads=[tmp, wr], writes=[p_r], inc=(c == 15))
                k.op("dve", lambda e: e.tensor_tensor(out=lg[:], in0=p_r[:], in1=br[:], op=ALU.add), reads=[p_r, br], writes=[lg])
                k.op("dve", lambda e: e.tensor_reduce(out=m4[:], in_=lg[:, 0:4], op=ALU.max, axis=AX.X), reads=[lg], writes=[m4])
                k.op("dve", lambda e: e.tensor_scalar(out=oh4[:], in0=lg[:, 0:4], scalar1=m4[:, 0:1], scalar2=None, op0=ALU.is_ge), reads=[lg, m4], writes=[oh4])
                k.op("dve", lambda e: e.tensor_scalar(out=e4[:], in0=lg[:, 0:4], scalar1=m4[:, 0:1], scalar2=None, op0=ALU.subtract), reads=[lg, m4], writes=[e4])
                k.op("act", lambda e: e.activation(out=e4[:], in_=e4[:], func=AF.Exp), reads=[e4], writes=[e4])
                k.op("dve", lambda e: e.tensor_reduce(out=s4[:], in_=e4[:], op=ALU.add, axis=AX.X), reads=[e4], writes=[s4])
                k.op("dve", lambda e: e.tensor_scalar(out=fs[:], in0=lg[:, 4:12], scalar1=oh4[:, 0:1], scalar2=None, op0=ALU.mult), reads=[lg, oh4], writes=[fs])
                for g in range(1, 4):
                    k.op("dve", lambda e: e.scalar_tensor_tensor(out=fs[:], in0=lg[:, 4 + 8 * g:12 + 8 * g], scalar=oh4[:, g:g + 1], in1=fs[:], op0=ALU.mult, op1=ALU.add),
                         reads=[lg, oh4, fs], writes=[fs])
                k.op("dve", lambda e: e.max(out=mx8[:], in_=fs[:]), reads=[fs], writes=[mx8])
                k.op("dve", lambda e: e.tensor_scalar(out=selA[:], in0=fs[:], scalar1=mx8[:, 0:1], scalar2=None, op0=ALU.is_ge), reads=[fs, mx8], writes=[selA])
                k.op("dve", lambda e: e.tensor_scalar(out=selB[:], in0=fs[:], scalar1=mx8[:, 1:2], scalar2=None, op0=ALU.is_ge), reads=[fs, mx8], writes=[selB])
                k.op("dve", lambda e: e.tensor_tensor(out=selB[:], in0=selB[:], in1=selA[:], op=ALU.subtract), reads=[selB, selA], writes=[selB])
                k.op("dve", lambda e: e.tensor_tensor(out=e2v[:], in0=mx8[:, 1:2], in1=mx8[:, 0:1], op=ALU.subtract), reads=[mx8], writes=[e2v])
                k.op("act", lambda e: e.activation(out=e2v[:], in_=e2v[:], func=AF.Exp), reads=[e2v], writes=[e2v])
                k.op("dve", lambda e: e.scalar_tensor_tensor(out=den[:], in0=e2v[:], scalar=1.0, in1=s4[:], op0=ALU.add, op1=ALU.mult), reads=[e2v, s4], writes=[den])
                k.op("dve", lambda e: e.reciprocal(out=GT[:, n, 0:1], in_=den[:]), reads=[den], writes=[GT])
                k.op("dve", lambda e: e.tensor_tensor(out=GT[:, n, 1:2], in0=GT[:, n, 0:1], in1=e2v[:], op=ALU.mult), reads=[GT, e2v], writes=[GT])
                for g in range(4):
                    k.op("dve", lambda e: e.tensor_scalar(out=AB[:, n, 8 * g:8 * g + 8], in0=selA[:], scalar1=oh4[:, g:g + 1], scalar2=None, op0=ALU.mult), reads=[selA, oh4], writes=[AB])
                    k.op("dve", lambda e: e.tensor_scalar(out=AB[:, n, 32 + 8 * g:40 + 8 * g], in0=selB[:], scalar1=oh4[:, g:g + 1], scalar2=None, op0=ALU.mult), reads=[selB, oh4], writes=[AB])
                k.op("dve", lambda e: e.tensor_tensor(out=Msum[:], in0=AB[:, n, 0:32], in1=AB[:, n, 32:64], op=ALU.add), reads=[AB], writes=[Msum])
                k.op("pe", lambda e: e.matmul(p_c[:, 0:32], lhsT=C["Lst32"][:], rhs=Msum[:], start=True, stop=True), reads=[C["Lst32"], Msum], writes=[p_c])
                k.op("pe", lambda e: e.matmul(p_c[:, 32:64], lhsT=C["ones32"][:], rhs=Msum[:], start=True, stop=True), reads=[C["ones32"], Msum], writes=[p_c])
                k.op("dve", lambda e: e.tensor_tensor(out=CUM[:, n, :], in0=p_c[:, 0:32], in1=carry[:], op=ALU.add), reads=[p_c, carry], writes=[CUM])
                k.op("dve", lambda e: e.tensor_tensor(out=carry[:], in0=carry[:], in1=p_c[:, 32:64], op=ALU.add), reads=[carry, p_c], writes=[carry])
                hr = hrow[hi % 2]; hi += 1
                for q4 in range(4):
                    pt = p_t[ti % 2]; ti += 1
                    for cc in range(4):
                        c = q4 * 4 + cc
                        k.op("pe", lambda e: e.transpose(out=pt[:, cc * 128:(cc + 1) * 128], in_=tmp[:, c, ts_], identity=C["ident32"][:]), reads=[tmp, C["ident32"]], writes=[pt], inc=(cc == 3))
                    if q4 % 2 == 0:
                        k.op("act", lambda e: e.copy(out=hr[:, q4 * 512:(q4 + 1) * 512], in_=pt[:]), reads=[pt], writes=[hr])
                    else:
                        k.op("dve", lambda e: e.tensor_copy(out=hr[:, q4 * 512:(q4 + 1) * 512], in_=pt[:]), reads=[pt], writes=[hr])
                k.dma("sp", H2.t[t0 + s_ * 128:t0 + (s_ + 1) * 128, :], hr[:], src=hr, dst=H2)
    pstart = k.sbuf("p_pstart", [128, 32], F32)
    IDXW = k.sbuf("p_IDXW", [128, NB, 4], I32)
    DEST = k.sbuf("p_DEST", [128, NT, 2], I32)
    with k.scope():
        pc = k.sbuf("pb_pc", [128, 32], F32); pend = k.sbuf("pb_pend", [128, 32], F32)
        onesr = k.sbuf("pb_onesr", [128, 32], F32)
        I128 = k.sbuf("pb_I128", [128, NB], F32); BE = k.sbuf("pb_BE", [128, NB], F32)
        fcp = k.sbuf("pb_fcp", [128, 4], F32); idxf = k.sbuf("pb_idxf", [128, NB, 4], F32)
        dsum = k.sbuf("pb_dsum", [128, 32], F32); dj = k.sbuf("pb_dj", [128, 32], F32); dtmp = k.sbuf("pb_dtmp", [128, NT, 2], F32)
        k.op("pool", lambda e: e.iota(I128[:], pattern=[[BS, NB]], base=0, channel_multiplier=0, allow_small_or_imprecise_dtypes=True), writes=[I128])
        for ex in range(32):
            k.op("dve", lambda e: e.tensor_scalar(out=BE[:], in0=I128[:], scalar1=carry[:, ex:ex + 1], scalar2=0.0, op0=ALU.is_lt, op1=ALU.add, accum_out=pc[:, ex:ex + 1]),
                 reads=[I128, carry], writes=[BE, pc])
        k.op("dve", lambda e: e.tensor_scalar(out=pc[:], in0=pc[:], scalar1=float(BS), scalar2=None, op0=ALU.mult), reads=[pc], writes=[pc])
        k.op("pool", lambda e: e.memset(onesr[:], 1.0), writes=[onesr])
        k.op("dve", lambda e: e.tensor_tensor_scan(out=pend[:], data0=onesr[:], data1=pc[:], initial=0.0, op0=ALU.mult, op1=ALU.add), reads=[onesr, pc], writes=[pend])
        k.op("dve", lambda e: e.tensor_tensor(out=pstart[:], in0=pend[:], in1=pc[:], op=ALU.subtract), reads=[pend, pc], writes=[pstart])
        k.op("pool", lambda e: e.memset(BE[:], 0.0), writes=[BE])
        for ex in range(32):
            k.op("dve", lambda e: e.scalar_tensor_tensor(out=BE[:], in0=I128[:], scalar=pend[:, ex:ex + 1], in1=BE[:], op0=ALU.is_ge, op1=ALU.add), reads=[I128, pend, BE], writes=[BE])
        k.op("dve", lambda e: e.tensor_scalar(out=BE[:], in0=BE[:], scalar1=31.0, scalar2=512.0, op0=ALU.min, op1=ALU.mult), reads=[BE], writes=[BE])
        k.op("pool", lambda e: e.iota(fcp[:], pattern=[[128, 4]], base=0, channel_multiplier=1, allow_small_or_imprecise_dtypes=True), writes=[fcp])
        k.op("dve", lambda e: e.tensor_tensor(out=idxf[:], in0=BE[:].unsqueeze(2).to_broadcast([128, NB, 4]), in1=fcp[:].unsqueeze(1).to_broadcast([128, NB, 4]), op=ALU.add), reads=[BE, fcp], writes=[idxf])
        k.op("dve", lambda e: e.tensor_copy(out=IDXW[:], in_=idxf[:]), reads=[idxf], writes=[IDXW])
        for n in range(NT):
            k.op("dve", lambda e: e.tensor_tensor(out=dsum[:], in0=CUM[:, n, :], in1=pstart[:], op=ALU.add), reads=[CUM, pstart], writes=[dsum])
            for j in range(2):
                k.op("dve", lambda e: e.tensor_tensor(out=dj[:], in0=dsum[:], in1=AB[:, n, 32 * j:32 * j + 32], op=ALU.mult), reads=[dsum, AB], writes=[dj])
                k.op("dve", lambda e: e.tensor_reduce(out=dtmp[:, n, j:j + 1], in_=dj[:], op=ALU.add, axis=AX.X), reads=[dj], writes=[dtmp])
        k.op("dve", lambda e: e.tensor_copy(out=DEST[:], in_=dtmp[:]), reads=[dtmp], writes=[DEST])
    with k.scope():
        zr = k.sbuf("pc_zero", [128, D], F32)
        k.op("pool", lambda e: e.memset(zr[:], 0.0), writes=[zr])
        for r in range(NB * SUB):
            k.dma("sp", Xd.t[r * 128:(r + 1) * 128, :], zr[:], src=zr, dst=Xd)
    with k.scope():
        hr = [k.sbuf(f"pc_hr{i}", [128, D], F32) for i in range(3)]
        for n in range(NT):
            h_ = hr[n % 3]
            k.dma("pool", h_[:], H2.t[n * 128:(n + 1) * 128, :], src=H2, dst=h_)
            for j in range(2):
                k.indirect("pool", Xd, h_, DEST, out_ap=Xd.t[:, :], out_idx=DEST[:, n, j:j + 1], in_ap=h_[:])
    with k.scope():
        xb = [k.sbuf(f"pd_xb{i}", [128, D], F32) for i in range(2)]
        xbT = [k.sbuf(f"pd_xbT{i}", [128, 16, BS], BF16) for i in range(2)]
        wg = [k.sbuf(f"pd_wg{fc}", [128, 2048], BF16) for fc in range(4)]
        wu = [k.sbuf(f"pd_wu{fc}", [128, 2048], BF16) for fc in range(4)]
        wd = [[k.sbuf(f"pd_wd{i}_{fc}", [128, 2048], BF16) for fc in range(4)] for i in range(2)]
        sg = k.sbuf("pd_sg", [128, BS], F32)
        hid = [k.sbuf(f"pd_hid{i}", [128, 4, BS], BF16) for i in range(2)]
        yb = [k.sbuf(f"pd_yb{i}", [128, D], F32) for i in range(2)]
        p_t = [k.psum(f"pd_p_t{i}", [128, 512]) for i in range(2)]
        p_g = [k.psum(f"pd_p_g{i}", [128, BS]) for i in range(2)]
        p_u = [k.psum(f"pd_p_u{i}", [128, BS]) for i in range(2)]
        p_d = [k.psum(f"pd_p_d{i}", [128, 512]) for i in range(2)]
        ti = 0; gi = 0; di = 0; xi = 0; yi = 0
        for i in range(NB):
            xT_ = xbT[i % 2]; wd_ = wd[i % 2]; hid_ = hid[i % 2]
            for fc in range(4):
                k.indirect("pool", wg[fc], None, IDXW, out_ap=wg[fc][:], in_ap=wg_d[:, :], in_idx=IDXW[:, i, fc:fc + 1])
                k.indirect("pool", wu[fc], None, IDXW, out_ap=wu[fc][:], in_ap=wu_d[:, :], in_idx=IDXW[:, i, fc:fc + 1])
            for fc in range(4):
                k.indirect("pool", wd_[fc], None, IDXW, out_ap=wd_[fc][:], in_ap=wd_d[:, :], in_idx=IDXW[:, i, fc:fc + 1])
            for s_ in range(SUB):
                x_ = xb[xi % 2]; xi += 1
                k.dma("sp", x_[:], Xd.t[i * BS + s_ * 128:i * BS + (s_ + 1) * 128, :], src=Xd, dst=x_)
                for q4 in range(4):
                    pt = p_t[ti % 2]; ti += 1
                    for cc in range(4):
                        c = q4 * 4 + cc
                        k.op("pe", lambda e: e.transpose(out=pt[:, cc * 128:(cc + 1) * 128], in_=x_[:, c * 128:(c + 1) * 128], identity=C["ident32"][:]), reads=[x_, C["ident32"]], writes=[pt], inc=(cc == 3))
                    if q4 % 2 == 0:
                        k.op("act", lambda e: e.copy(out=xT_[:, q4 * 4:(q4 + 1) * 4, s_ * 128:(s_ + 1) * 128], in_=pt[:].rearrange("p (c t) -> p c t", c=4)), reads=[pt], writes=[xT_])
                    else:
                        k.op("dve", lambda e: e.tensor_copy(out=xT_[:, q4 * 4:(q4 + 1) * 4, s_ * 128:(s_ + 1) * 128], in_=pt[:].rearrange("p (c t) -> p c t", c=4)), reads=[pt], writes=[xT_])
            for fc in range(4):
                pg = p_g[gi % 2]; pu = p_u[gi % 2]; gi += 1
                for c in range(16):
                    k.op("pe", lambda e: e.matmul(pg[:], lhsT=wg[fc][:, c * 128:(c + 1) * 128], rhs=xT_[:, c, :], start=(c == 0), stop=(c == 15)), reads=[wg[fc], xT_], writes=[pg], inc=(c == 15))
                for c in range(16):
                    k.op("pe", lambda e: e.matmul(pu[:], lhsT=wu[fc][:, c * 128:(c + 1) * 128], rhs=xT_[:, c, :], start=(c == 0), stop=(c == 15)), reads=[wu[fc], xT_], writes=[pu], inc=(c == 15))
                k.op("act", lambda e: e.activation(out=sg[:], in_=pg[:], func=AF.Silu), reads=[pg], writes=[sg])
                k.op("dve", lambda e: e.tensor_tensor(out=hid_[:, fc, :], in0=sg[:], in1=pu[:], op=ALU.mult), reads=[sg, pu], writes=[hid_])
            for s_ in range(SUB):
                y_ = yb[yi % 2]; yi += 1
                for dq in range(4):
                    pd = p_d[di % 2]; di += 1
                    for fc in range(4):
                        k.op("pe", lambda e: e.matmul(pd[:], lhsT=hid_[:, fc, s_ * 128:(s_ + 1) * 128], rhs=wd_[fc][:, dq * 512:(dq + 1) * 512], start=(fc == 0), stop=(fc == 3)), reads=[hid_, wd_[fc]], writes=[pd], inc=(fc == 3))
                    if dq % 2 == 0:
                        k.op("act", lambda e: e.copy(out=y_[:, dq * 512:(dq + 1) * 512], in_=pd[:]), reads=[pd], writes=[y_])
                    else:
                        k.op("dve", lambda e: e.tensor_copy(out=y_[:, dq * 512:(dq + 1) * 512], in_=pd[:]), reads=[pd], writes=[y_])
                k.dma("sp", Yd.t[i * BS + s_ * 128:i * BS + (s_ + 1) * 128, :], y_[:], src=y_, dst=Yd)
    with k.scope():
        y1 = [k.sbuf(f"pe_y1{i}", [128, D], F32) for i in range(2)]
        y2 = [k.sbuf(f"pe_y2{i}", [128, D], F32) for i in range(2)]
        x1 = [k.sbuf(f"pe_x1{i}", [128, 16, 128], F32) for i in range(2)]
        sq = k.sbuf("pe_sq", [128, 16, 128], F32); rs = k.sbuf("pe_rs", [128, 128], F32)
        fn17 = k.sbuf("pe_fn17", [128, 17], F32); fnv = k.sbuf("pe_fn", [128, 16], F32)
        p_t = [k.psum(f"pe_p_t{i}", [128, 512]) for i in range(2)]
        p_s = k.psum("pe_p_s", [128, 128])
        alp = k.sbuf("pe_alp", [128, 1], F32); oma = k.sbuf("pe_oma", [128, 1], F32); scl = k.sbuf("pe_scl", [128, 128], F32)
        k.dma("sp", fn17[:], fn_d, dst=fn17)
        k.op("dve", lambda e: e.tensor_copy(out=alp[:], in_=fn17[:, 16:17]), reads=[fn17], writes=[alp])
        k.op("dve", lambda e: e.tensor_scalar(out=fnv[:], in0=fn17[:, 0:16], scalar1=alp[:, 0:1], scalar2=None, op0=ALU.mult), reads=[fn17, alp], writes=[fnv])
        k.op("dve", lambda e: e.tensor_scalar(out=oma[:], in0=alp[:], scalar1=-1.0, scalar2=1.0, op0=ALU.mult, op1=ALU.add), reads=[alp], writes=[oma])
        ti = 0
        for n in range(NT):
            a_ = y1[n % 2]; b_ = y2[n % 2]; x_ = x1[n % 2]
            k.indirect("pool", a_, Yd, DEST, out_ap=a_[:], in_ap=Yd.t[:, :], in_idx=DEST[:, n, 0:1])
            k.indirect("pool", b_, Yd, DEST, out_ap=b_[:], in_ap=Yd.t[:, :], in_idx=DEST[:, n, 1:2])
            k.dma("sp", x_[:], X1v[:, :, n * 128:(n + 1) * 128], src=X1, dst=x_)
            k.op("dve", lambda e: e.tensor_scalar(out=a_[:], in0=a_[:], scalar1=GT[:, n, 0:1], scalar2=None, op0=ALU.mult), reads=[a_, GT], writes=[a_])
            k.op("dve", lambda e: e.scalar_tensor_tensor(out=a_[:], in0=b_[:], scalar=GT[:, n, 1:2], in1=a_[:], op0=ALU.mult, op1=ALU.add), reads=[b_, GT, a_], writes=[a_])
            for q4 in range(4):
                pt = p_t[ti % 2]; ti += 1
                for cc in range(4):
                    c = q4 * 4 + cc
                    k.op("pe", lambda e: e.transpose(out=pt[:, cc * 128:(cc + 1) * 128], in_=a_[:, c * 128:(c + 1) * 128], identity=C["ident32"][:]), reads=[a_, C["ident32"]], writes=[pt], inc=(cc == 3))
                for cc in range(4):
                    c = q4 * 4 + cc
                    k.op("dve", lambda e: e.scalar_tensor_tensor(out=x_[:, c, :], in0=pt[:, cc * 128:(cc + 1) * 128], scalar=vt[:, 4, c:c + 1], in1=x_[:, c, :], op0=ALU.mult, op1=ALU.add),
                         reads=[pt, vt, x_], writes=[x_])
            if True:
                k.op("act", lambda e: e.activation(out=sq[:], in_=x_[:], func=AF.Square), reads=[x_], writes=[sq])
                for c in range(16):
                    k.op("pe", lambda e: e.matmul(p_s[:], lhsT=C["ones32"][:], rhs=sq[:, c, :], start=(c == 0), stop=(c == 15)), reads=[C["ones32"], sq], writes=[p_s], inc=(c == 15))
                k.op("dve", lambda e: e.tensor_scalar(out=rs[:], in0=p_s[:], scalar1=1.0 / D, scalar2=1e-6, op0=ALU.mult, op1=ALU.add), reads=[p_s], writes=[rs])
                k.op("act", lambda e: e.activation(out=rs[:], in_=rs[:], func=AF.Sqrt), reads=[rs], writes=[rs])
                k.op("dve", lambda e: e.reciprocal(out=rs[:], in_=rs[:]), reads=[rs], writes=[rs])
                for c in range(16):
                    k.op("dve", lambda e: e.tensor_scalar(out=scl[:], in0=rs[:], scalar1=fnv[:, c:c + 1], scalar2=oma[:, 0:1], op0=ALU.mult, op1=ALU.add), reads=[rs, fnv, oma], writes=[scl])
                    k.op("dve", lambda e: e.tensor_tensor(out=x_[:, c, :], in0=x_[:, c, :], in1=scl[:], op=ALU.mult), reads=[x_, scl], writes=[x_])
            k.dma("sp", outv[:, :, n * 128:(n + 1) * 128], x_[:], src=x_, dst=out_d)


D = 2048
TM_BLOCKS = [(0, 512), (512, 512), (1024, 512), (1536, 512), (2048, 512), (2560, 16)]


def emit_pre(k, C, T, xT_d, wfm_d, wtm_d, wtm_last_d, vec_d, pfm, ptm, xsrc=None):
    TT = 512
    xTv = xT_d.rearrange("(c p) t -> p c t", p=128)
    vt = k.sbuf("r_vt", [128, 3, 16], F32); acol = k.sbuf("r_acol", [128, 16], F32)
    xt = k.sbuf("r_xt", [128, 16, TT], F32); tmp = k.sbuf("r_tmp", [128, 16, TT], F32)
    hT = k.sbuf("r_hT", [128, 16, TT], BF16); rstd = k.sbuf("r_rstd", [128, TT], F32)
    wb = [k.sbuf(f"r_wb{i}", [128, 16, 128], BF16) for i in range(3)]
    wt = [k.sbuf(f"r_wt{i}", [128, 16, 512], BF16) for i in range(2)]
    ob = [k.sbuf(f"r_ob{i}", [128, TT], F32) for i in range(3)]
    pss = k.psum("r_pss", [128, TT]); psm = [k.psum(f"r_psm{i}", [128, TT]) for i in range(4)]
    k.dma("sp", vt[:], vec_d, dst=vt)
    k.op("dve", lambda e: e.scalar_tensor_tensor(out=acol[:], in0=vt[:, 1, :], scalar=1.0, in1=vt[:, 0, :], op0=ALU.add, op1=ALU.mult), reads=[vt], writes=[acol])
    wi = 0; ti = 0
    for t in range(T // TT):
        t0 = t * TT
        k.dma("sp", xt[:], xTv[:, :, t0:t0 + TT], src=xsrc, dst=xt)
        k.op("act", lambda e: e.activation(out=tmp[:], in_=xt[:], func=AF.Square), reads=[xt], writes=[tmp])
        for c in range(16):
            k.op("pe", lambda e: e.matmul(pss[:], lhsT=C["ones32"][:], rhs=tmp[:, c, :], start=(c == 0), stop=(c == 15)), reads=[C["ones32"], tmp], writes=[pss], inc=(c == 15))
        k.op("dve", lambda e: e.tensor_scalar(out=rstd[:], in0=pss[:], scalar1=1.0 / D, scalar2=1e-6, op0=ALU.mult, op1=ALU.add), reads=[pss], writes=[rstd])
        k.op("act", lambda e: e.activation(out=rstd[:], in_=rstd[:], func=AF.Sqrt), reads=[rstd], writes=[rstd])
        k.op("dve", lambda e: e.reciprocal(out=rstd[:], in_=rstd[:]), reads=[rstd], writes=[rstd])
        for c in range(16):
            k.op("dve", lambda e: e.scalar_tensor_tensor(out=tmp[:, c, :], in0=xt[:, c, :], scalar=acol[:, c:c + 1], in1=rstd[:], op0=ALU.mult, op1=ALU.mult), reads=[xt, acol, rstd], writes=[tmp])
            k.op("act", lambda e: e.activation(out=hT[:, c, :], in_=tmp[:, c, :], func=AF.Identity, bias=vt[:, 2, c:c + 1]), reads=[tmp, vt], writes=[hT])
        for m in range(20):
            wj = wb[wi % 3]; p = psm[wi % 4]; o = ob[wi % 3]; wi += 1
            k.dma("pool", wj[:], wfm_d[m], dst=wj)
            for c in range(16):
                k.op("pe", lambda e: e.matmul(p[:], lhsT=wj[:, c, :], rhs=hT[:, c, :], start=(c == 0), stop=(c == 15)), reads=[wj, hT], writes=[p], inc=(c == 15))
            if m % 2 == 0:
                k.op("dve", lambda e: e.tensor_copy(out=o[:], in_=p[:]), reads=[p], writes=[o])
            else:
                k.op("act", lambda e: e.copy(out=o[:], in_=p[:]), reads=[p], writes=[o])
            k.dma("sp", pfm.t[m * 128:(m + 1) * 128, t0:t0 + TT], o[:], src=o, dst=pfm)
        for bi, (c0, n) in enumerate(TM_BLOCKS):
            wj = wt[ti % 2]; ti += 1
            if n == 512:
                k.dma("pool", wj[:], wtm_d[bi], dst=wj)
            else:
                k.dma("pool", wj[:, :, 0:n], wtm_last_d, dst=wj)
            for s_ in range(TT // 128):
                p = psm[wi % 4]; o = ob[wi % 3]; wi += 1
                for c in range(16):
                    k.op("pe", lambda e: e.matmul(p[:, 0:n], lhsT=hT[:, c, s_ * 128:(s_ + 1) * 128], rhs=wj[:, c, 0:n], start=(c == 0), stop=(c == 15)), reads=[hT, wj], writes=[p], inc=(c == 15))
                if s_ % 2 == 0:
                    k.op("dve", lambda e: e.tensor_copy(out=o[:, 0:n], in_=p[:, 0:n]), reads=[p], writes=[o])
                else:
                    k.op("act", lambda e: e.copy(out=o[:, 0:n], in_=p[:, 0:n]), reads=[p], writes=[o])
                k.dma("sp", ptm.t[t0 + s_ * 128:t0 + (s_ + 1) * 128, c0:c0 + n], o[:, 0:n], src=o, dst=ptm)


NCH = 16


T_SEQ = 8192
LAYER_INS = [("wfm", [20, 128, 16, 128]), ("wtm", [5, 128, 16, 512]), ("wtl", [128, 16, 16]),
             ("sgu_lng", [128, 512]), ("sgu_lnb", [128, 512]), ("sgu_wT", [4, 128, 128]), ("sgu_bs", [128, 4]),
             ("ssd_convw", [2, 128, 6, 4]), ("ssd_convb", [2, 128, 6, 1]), ("ssd_dtb", [128, 16]), ("ssd_alog", [128, 16]),
             ("ssd_dskip", [128, 16]), ("ssd_norm", [128, 1024]),
             ("wo", [16, 128, 16, 128]), ("wr", [128, 16, 36]), ("br", [128, 36]),
             ("wg", [32 * 512, 2048]), ("wu", [32 * 512, 2048]), ("wd", [32 * 512, 2048]), ("fn", [128, 17])]


def emit_mod(k, C, wada_d, cT_d, bias_d, ncols_d, VEC1, VEC2, nlayers):
    NJ = nlayers * 96
    ct = k.sbuf("m_ct", [128, 16, 2], F32); bt = k.sbuf("m_bt", [128, NJ], F32); res = k.sbuf("m_res", [128, NJ], F32)
    nct = k.sbuf("m_nct", [128, 2 * nlayers, 16], F32)
    wb = [k.sbuf(f"m_wb{i}", [128, 16, 128], F32) for i in range(3)]
    ps = [k.psum(f"m_ps{i}", [128, 2]) for i in range(2)]
    v1 = k.sbuf("m_v1", [128, nlayers, 3, 16], F32); v2 = k.sbuf("m_v2", [128, nlayers, 5, 16], F32)
    k.dma("sp", ct[:], cT_d, dst=ct); k.dma("sp", bt[:], bias_d, dst=bt); k.dma("sp", nct[:], ncols_d, dst=nct)
    k.op("act", lambda e: e.activation(out=ct[:], in_=ct[:], func=AF.Silu), reads=[ct], writes=[ct])
    for j in range(NJ):
        wj = wb[j % 3]; p = ps[j % 2]
        k.dma("sp", wj[:], wada_d[j], dst=wj)
        for c in range(16):
            k.op("pe", lambda e: e.matmul(p[:], lhsT=wj[:, c, :], rhs=ct[:, c, :], start=(c == 0), stop=(c == 15)), reads=[wj, ct], writes=[p], inc=(c == 15))
        k.op("dve", lambda e: e.tensor_tensor(out=res[:, j:j + 1], in0=p[:, 0:1], in1=bt[:, j:j + 1], op=ALU.add), reads=[p, bt], writes=[res])
    for l in range(nlayers):
        b0 = l * 96
        for (dst, slot, srcap) in ((v1, 0, nct[:, l, :]), (v1, 1, res[:, b0 + 16:b0 + 32]), (v1, 2, res[:, b0:b0 + 16]),
                                   (v2, 0, res[:, b0 + 32:b0 + 48]), (v2, 1, nct[:, nlayers + l, :]), (v2, 2, res[:, b0 + 64:b0 + 80]),
                                   (v2, 3, res[:, b0 + 48:b0 + 64]), (v2, 4, res[:, b0 + 80:b0 + 96])):
            k.op("dve", lambda e: e.tensor_copy(out=dst[:, l, slot, :], in_=srcap), reads=[res, nct], writes=[dst])
    k.dma("sp", VEC1.t.rearrange("l p a c -> p l a c"), v1[:], src=v1, dst=VEC1)
    k.dma("sp", VEC2.t.rearrange("l p a c -> p l a c"), v2[:], src=v2, dst=VEC2)


def build_fused(T=T_SEQ, nlayers=4):
    nc = bass.Bass("TRN2", target_bir_lowering=False)
    k = K(nc, same_engine_sync=True)
    A = lambda n, s: nc.dram_tensor(n, s, F32, kind="ExternalInput").ap()
    xT = A("xT", [2048, T])
    cos = A("cos", [32, T]); sin = A("sin", [32, T])
    wada = A("wada", [nlayers * 96, 128, 16, 128]); cTd = A("cT", [128, 16, 2]); biasd = A("bias", [128, nlayers * 96]); ncols = A("ncols", [128, 2 * nlayers, 16])
    L = [{n: A(f"{n}_l{l}", shp) for n, shp in LAYER_INS} for l in range(nlayers)]
    out = k.dram("xo", [2048, T], F32, kind="ExternalOutput")
    VEC1 = k.dram("vec1", [nlayers, 128, 3, 16], F32); VEC2 = k.dram("vec2", [nlayers, 128, 5, 16], F32)
    XB = [k.dram("xres", [2048, T], F32) for _ in range(2)]
    pfm = k.dram("pfm", [2560, T], F32); ptm = k.dram("ptm", [T, 2576], F32); y_d = k.dram("ymix", [T, 2048], F32)
    C = emit_consts(k)
    with k.scope():
        emit_mod(k, C, wada, cTd, biasd, ncols, VEC1, VEC2, nlayers)
    for l in range(nlayers):
        W = L[l]
        xin = xT if l == 0 else XB[(l - 1) % 2].t
        xout = out if l == nlayers - 1 else XB[l % 2]
        with k.scope():
            emit_pre(k, C, T, xin, W["wfm"], W["wtm"], W["wtl"], VEC1.t[l], pfm, ptm)
        with k.scope():
            emit_sgu(k, C, T, ptm.t, 0, {"lng": W["sgu_lng"], "lnb": W["sgu_lnb"], "wT": W["sgu_wT"], "bs": W["sgu_bs"]}, y_d, 0)
        with k.scope():
            emit_attn(k, C, T, pfm.t[0:512, :], pfm.t[512:1024, :], ptm.t[:, 1024:1536], cos, sin, y_d, 512, 4)
        with k.scope():
            emit_ssd(k, C, T, pfm.t[1024:2560, :], ptm.t, 1536, 2560,
                     {"convw": W["ssd_convw"], "convb": W["ssd_convb"], "dtb": W["ssd_dtb"], "alog": W["ssd_alog"], "dskip": W["ssd_dskip"], "norm": W["ssd_norm"]}, y_d, 1024)
        with k.scope():
            emit_post(k, C, T, xin, y_d, W["wo"], VEC2.t[l], W["wr"], W["br"], W["wg"], W["wu"], W["wd"], xout, final=True, fn_d=W["fn"])
    k.finish(outs=[out]); k.close()
    return nc


def _wl(Wc, nb=128):
    n = Wc.shape[1] // nb
    return np.ascontiguousarray(Wc.reshape(16, 128, n, nb).transpose(2, 1, 0, 3))


def _col(v):
    return v.reshape(16, 128).T


def _rep(v):
    return np.ascontiguousarray(np.broadcast_to(np.asarray(v, np.float32).reshape(1, -1), (128, v.size)))


def _wgl(w):
    return np.ascontiguousarray(w.reshape(32, 16, 128, 4, 128).transpose(0, 3, 2, 1, 4).reshape(32 * 512, 2048))


def _conv_tiles(a):
    out = []
    for g in range(2):
        rows = [a[g * 512 + i * 128:g * 512 + (i + 1) * 128] for i in range(4)] + [a[1024 + g * 128:1024 + (g + 1) * 128], a[1280 + g * 128:1280 + (g + 1) * 128]]
        out.append(np.stack(rows, axis=1))
    return np.ascontiguousarray(np.stack(out, 0)).astype(np.float32)


def _rot_tables(T):
    pos = np.arange(T, dtype=np.float32)
    inv = (np.float32(500000.0) ** (-np.arange(0, 32, 2, dtype=np.float32) / np.float32(32))).astype(np.float32)
    ang = pos[:, None] * inv[None, :]
    c, s = np.cos(ang).astype(np.float32), np.sin(ang).astype(np.float32)
    return np.ascontiguousarray(np.concatenate([c, c], 1).T), np.ascontiguousarray(np.concatenate([-s, s], 1).T)


def _layer_shared(l, p, last):
    f = lambda n: np.asarray(p[n][l], np.float32)
    w_in = f("w_in")
    W_fm = np.concatenate([w_in[:, 1024:2048], w_in[:, 3584:5120]], 1)
    W_tm = np.concatenate([w_in[:, 0:1024], w_in[:, 2048:2560], w_in[:, 2560:3584], w_in[:, 5120:5136]], 1)
    Wr = np.concatenate([f("w_coarse")] + [f("w_fine")[g] for g in range(4)], axis=1)
    return {
        "wfm": _wl(W_fm), "wtm": _wl(W_tm[:, :2560], 512), "wtl": np.ascontiguousarray(W_tm[:, 2560:].reshape(16, 128, 16).transpose(1, 0, 2)),
        "sgu_lng": _rep(f("sgu_ln_g")), "sgu_lnb": _rep(f("sgu_ln_b")), "sgu_wT": np.ascontiguousarray(f("sgu_w").transpose(0, 2, 1)),
        "sgu_bs": np.ascontiguousarray(f("sgu_b").T),
        "ssd_convw": _conv_tiles(np.ascontiguousarray(f("conv_w").T)), "ssd_convb": _conv_tiles(f("conv_b")[:, None]),
        "ssd_dtb": _rep(f("dt_bias")), "ssd_alog": _rep(f("a_log")), "ssd_dskip": _rep(f("d_skip")), "ssd_norm": _rep(f("ssm_norm")),
        "wo": _wl(f("w_out")), "wr": np.ascontiguousarray(Wr.reshape(16, 128, 36).transpose(1, 0, 2)),
        "br": _rep(np.concatenate([f("b_coarse"), f("b_fine").reshape(-1)])),
        "wg": _wgl(f("w_gate")), "wu": _wgl(f("w_up")), "wd": np.ascontiguousarray(f("w_down").reshape(32 * 512, 2048)),
        "fn": np.ascontiguousarray(np.concatenate([_col(np.asarray(p["final_norm"], np.float32)), np.full((128, 1), 1.0 if last else 0.0, np.float32)], 1)),
    }


def _fused_inputs(p, T=T_SEQ, layers=(0, 1, 2, 3), nb=4, xT_list=None, total_layers=4):
    x = np.asarray(p["x"], np.float32); c = np.asarray(p["c"], np.float32)
    cosT, sinT = _rot_tables(T)
    w_ada = np.asarray(p["w_ada"], np.float32); b_ada = np.asarray(p["b_ada"], np.float32)
    nl = len(layers)
    W = np.concatenate([w_ada[l] for l in layers], axis=1)
    shared = {"cos": cosT, "sin": sinT, "wada": _wl(W),
              "bias": np.ascontiguousarray(np.concatenate([b_ada[l] for l in layers]).reshape(nl * 96, 128).T),
              "ncols": np.ascontiguousarray(np.stack([_col(np.asarray(p["norm1"][l], np.float32)) for l in layers] +
                                                     [_col(np.asarray(p["norm2"][l], np.float32)) for l in layers], 1))}
    for li, l in enumerate(layers):
        for n, a in _layer_shared(l, p, last=(l == total_layers - 1)).items():
            shared[f"{n}_l{li}"] = a
    ins = []
    for b in range(nb):
        d = dict(shared)
        d["xT"] = np.ascontiguousarray(x[b, :T].T) if xT_list is None else xT_list[b]
        cc = _col(c[b])[:, :, None]
        d["cT"] = np.ascontiguousarray(np.concatenate([cc, cc], 2))
        ins.append(d)
    return ins


LAUNCHES = [[0, 1, 2, 3]]
_NC_CACHE = {}


def kernel(**p):
    from concourse.bass_utils import run_bass_kernel_spmd
    xT = None
    for layers in LAUNCHES:
        nl = len(layers)
        if nl not in _NC_CACHE:
            _NC_CACHE[nl] = build_fused(T_SEQ, nl)
        ins = _fused_inputs(p, T_SEQ, layers, 4, xT)
        res = run_bass_kernel_spmd(_NC_CACHE[nl], ins, core_ids=[0, 1, 2, 3])
        xT = [res.results[b]["xo"] for b in range(4)]
    return np.ascontiguousarray(np.stack([xT[b].T for b in range(4)], 0)).astype(np.float32)
```

```python
import numpy as np
from contextlib import ExitStack
import concourse.bass as bass
import concourse.mybir as mybir

F32 = mybir.dt.float32
BF16 = mybir.dt.bfloat16
I32 = mybir.dt.int32
U32 = mybir.dt.uint32
AF = mybir.ActivationFunctionType
ALU = mybir.AluOpType
AX = mybir.AxisListType


class Buf:
    __slots__ = ("t", "name", "w", "rd", "dw_sem", "dw_cnt", "dr_sem", "dr_cnt", "dw_base", "dr_base")

    def __init__(self, t, name):
        self.t = t
        self.name = name
        self.w = None
        self.rd = {}
        self.dw_sem = None
        self.dw_cnt = 0
        self.dr_sem = None
        self.dr_cnt = 0
        self.dw_base = 0
        self.dr_base = 0

    def __getitem__(self, idx):
        return self.t[idx]


class DBuf:
    def __init__(self, t, name):
        self.t = t
        self.name = name
        self.pw = {}
        self.pr = {}

    def __getitem__(self, idx):
        return self.t[idx]


class K:
    ENG = ("pe", "dve", "act", "pool", "sp")

    def __init__(self, nc, same_engine_sync=True):
        self.nc = nc
        self.es = ExitStack()
        self.E = {"pe": nc.tensor, "dve": nc.vector, "act": nc.scalar, "pool": nc.gpsimd, "sp": nc.sync}
        self.sem = {e: self.es.enter_context(nc.semaphore("sem_" + e)) for e in self.ENG}
        self.cnt = {e: 0 for e in self.ENG}
        self.seen = {}
        self.same = same_engine_sync
        self.cur = self.es
        self.dma_all = {}
        self.dbufs = []
        self.sem_pool = []
        self.scope_bufs = [[]]
        self.uid = 0
        self.nsem = len(self.ENG)

    def sbuf(self, name, shape, dt):
        self.uid += 1
        name = name + "_" + str(self.uid)
        t = self.cur.enter_context(self.nc.sbuf_tensor(name, list(shape), dt))
        b = Buf(t, name)
        self.scope_bufs[-1].append(b)
        return b

    def psum(self, name, shape, dt=F32):
        self.uid += 1
        name = name + "_" + str(self.uid)
        t = self.cur.enter_context(self.nc.psum_tensor(name, list(shape), dt))
        return Buf(t, name)

    def barrier(self):
        for e in self.ENG:
            for e2 in self.ENG:
                if e2 != e and e2 != "sp" and self.cnt[e2]:
                    self._wait(e, self.sem[e2], self.cnt[e2], "E" + e2)
            for key, (sem, val) in self.dma_all.items():
                self._wait(e, sem, val, key)

    def scope(self):
        from contextlib import contextmanager

        @contextmanager
        def _s():
            prev = self.cur
            st = ExitStack()
            self.cur = st
            self.scope_bufs.append([])
            try:
                yield
            finally:
                self.barrier()
                for b in self.scope_bufs.pop():
                    if b.dw_sem is not None:
                        self.sem_pool.append((b.dw_sem, b.dw_base + 16 * b.dw_cnt))
                    if b.dr_sem is not None:
                        self.sem_pool.append((b.dr_sem, b.dr_base + 16 * b.dr_cnt))
                self.dma_all = {}
                for d in self.dbufs:
                    d.pw = {}
                    d.pr = {}
                self.seen = {kk: v for kk, v in self.seen.items() if kk[1].startswith("E")}
                self.cur = prev
                st.close()
        return _s()

    def dram(self, name, shape, dt, kind="Internal"):
        if kind == "Internal":
            self.uid += 1
            name = name + "_" + str(self.uid)
        t = self.nc.dram_tensor(name, list(shape), dt, kind=kind).ap()
        d = DBuf(t, name)
        self.dbufs.append(d)
        return d

    def view(self, buf_t, name):
        return Buf(buf_t, name)

    def _newsem(self, name):
        if self.sem_pool:
            return self.sem_pool.pop()
        self.nsem += 1
        return self.es.enter_context(self.nc.semaphore("s" + str(self.nsem))), 0

    def _wait(self, eng, sem, val, key):
        if val <= 0:
            return
        k = (eng, key)
        if self.seen.get(k, 0) >= val:
            return
        self.seen[k] = val
        self.E[eng].wait_ge(sem, val)

    def _wait_eng(self, eng, dep):
        if dep is None:
            return
        e2, c = dep
        if e2 == eng and (not self.same or eng in ("pe", "sp")):
            return
        self._wait(eng, self.sem[e2], c, "E" + e2)

    def _deps_read(self, eng, b):
        self._wait_eng(eng, b.w)
        if b.dw_cnt:
            self._wait(eng, b.dw_sem, b.dw_base + 16 * b.dw_cnt, "DW" + b.name)

    def _deps_write(self, eng, b):
        self._wait_eng(eng, b.w)
        for e2, c in b.rd.items():
            if e2 != eng:
                self._wait_eng(eng, (e2, c))
        if b.dw_cnt:
            self._wait(eng, b.dw_sem, b.dw_base + 16 * b.dw_cnt, "DW" + b.name)
        if b.dr_cnt:
            self._wait(eng, b.dr_sem, b.dr_base + 16 * b.dr_cnt, "DR" + b.name)

    def op(self, eng, fn, reads=(), writes=(), inc=True):
        for b in reads:
            self._deps_read(eng, b)
        for b in writes:
            self._deps_write(eng, b)
        ins = fn(self.E[eng])
        if inc:
            self.cnt[eng] += 1
            ins.then_inc(self.sem[eng], 1)
            c = self.cnt[eng]
        else:
            c = self.cnt[eng] + 1
        for b in reads:
            b.rd[eng] = c
        for b in writes:
            b.w = (eng, c)
            b.rd = {}
        return ins

    def dma(self, q, out_ap, in_ap, src=None, dst=None, **kw):
        s_sb = isinstance(src, Buf)
        d_sb = isinstance(dst, Buf)
        assert s_sb != d_sb, "exactly one side must be an SBUF Buf"
        if d_sb:
            self._deps_write(q, dst)
            if isinstance(src, DBuf):
                for sname, (sem, val) in src.pw.items():
                    self._wait(q, sem, val, sname)
        else:
            self._deps_read(q, src)
            if isinstance(dst, DBuf):
                for d in (dst.pw, dst.pr):
                    for sname, (sem, val) in d.items():
                        self._wait(q, sem, val, sname)
        ins = self.E[q].dma_start(out=out_ap, in_=in_ap, **kw)
        if d_sb:
            if dst.dw_sem is None:
                dst.dw_sem, dst.dw_base = self._newsem("dw_" + dst.name)
            dst.dw_cnt += 1
            ins.then_inc(dst.dw_sem, 16)
            self.dma_all["DW" + dst.name] = (dst.dw_sem, dst.dw_base + 16 * dst.dw_cnt)
            dst.rd = {}
            if isinstance(src, DBuf):
                src.pr["DW" + dst.name] = (dst.dw_sem, dst.dw_base + 16 * dst.dw_cnt)
        else:
            if src.dr_sem is None:
                src.dr_sem, src.dr_base = self._newsem("dr_" + src.name)
            src.dr_cnt += 1
            ins.then_inc(src.dr_sem, 16)
            self.dma_all["DR" + src.name] = (src.dr_sem, src.dr_base + 16 * src.dr_cnt)
            if isinstance(dst, DBuf):
                dst.pw["DR" + src.name] = (src.dr_sem, src.dr_base + 16 * src.dr_cnt)
                dst.pr = {}
        return ins

    def indirect(self, q, sb, dram, idxbuf, out_ap, in_ap, out_idx=None, in_idx=None, bound=None):
        g = self.E["pool"]
        self._deps_read("pool", idxbuf)
        if in_idx is not None:
            dst = sb
            self._deps_write("pool", dst)
            if isinstance(dram, DBuf):
                for sname, (sem, val) in dram.pw.items():
                    self._wait("pool", sem, val, sname)
            ins = g.indirect_dma_start(out=out_ap, out_offset=None, in_=in_ap, in_offset=bass.IndirectOffsetOnAxis(ap=in_idx, axis=0))
            if dst.dw_sem is None:
                dst.dw_sem, dst.dw_base = self._newsem("dw_" + dst.name)
            dst.dw_cnt += 1
            ins.then_inc(dst.dw_sem, 16)
            self.dma_all["DW" + dst.name] = (dst.dw_sem, dst.dw_base + 16 * dst.dw_cnt)
            dst.rd = {}
            if isinstance(dram, DBuf):
                dram.pr["DW" + dst.name] = (dst.dw_sem, dst.dw_base + 16 * dst.dw_cnt)
        else:
            srcb, dd = dram, sb
            self._deps_read("pool", srcb)
            for sname, (sem, val) in dd.pr.items():
                self._wait("pool", sem, val, sname)
            kw = {}
            if bound is not None:
                kw = dict(bounds_check=bound, oob_is_err=False)
            ins = g.indirect_dma_start(out=out_ap, out_offset=bass.IndirectOffsetOnAxis(ap=out_idx, axis=0), in_=in_ap, in_offset=None, **kw)
            if srcb.dr_sem is None:
                srcb.dr_sem, srcb.dr_base = self._newsem("dr_" + srcb.name)
            srcb.dr_cnt += 1
            ins.then_inc(srcb.dr_sem, 16)
            self.dma_all["DR" + srcb.name] = (srcb.dr_sem, srcb.dr_base + 16 * srcb.dr_cnt)
            dd.pw["DR" + srcb.name] = (srcb.dr_sem, srcb.dr_base + 16 * srcb.dr_cnt)
        idxbuf.rd["pool"] = self.cnt["pool"] + 1
        return ins

    def finish(self, outs=()):
        for d in outs:
            for sname, (sem, val) in d.pw.items():
                self._wait("sp", sem, val, sname)
        for e in self.ENG:
            if e != "sp" and self.cnt[e]:
                self._wait("sp", self.sem[e], self.cnt[e], "E" + e)

    def close(self):
        self.es.close()


import os
DBG = ''


FR = mybir.dt.float32r
NEG = -30000.0


def emit_consts(k):
    C = {}
    C["ones32"] = k.sbuf("c_ones32", [128, 128], F32)
    k.op("pool", lambda e: e.memset(C["ones32"][:], 1.0), writes=[C["ones32"]])
    C["ident32"] = k.sbuf("c_ident32", [128, 128], F32)
    k.op("pool", lambda e: e.memset(C["ident32"][:], 0.0), writes=[C["ident32"]])
    k.op("pool", lambda e: e.affine_select(out=C["ident32"][:], in_=C["ident32"][:], pattern=[[-1, 128]], compare_op=ALU.not_equal,
                                            fill=1.0, base=0, channel_multiplier=1), reads=[C["ident32"]], writes=[C["ident32"]])
    C["ident16"] = k.sbuf("c_ident16", [128, 128], BF16)
    k.op("dve", lambda e: e.tensor_copy(out=C["ident16"][:], in_=C["ident32"][:]), reads=[C["ident32"]], writes=[C["ident16"]])
    C["U32"] = k.sbuf("c_U32", [128, 128], F32)
    k.op("pool", lambda e: e.affine_select(out=C["U32"][:], in_=C["ones32"][:], pattern=[[1, 128]], compare_op=ALU.is_ge,
                                            fill=0.0, base=0, channel_multiplier=-1), reads=[C["ones32"]], writes=[C["U32"]])
    C["Lst32"] = k.sbuf("c_Lst32", [128, 128], F32)
    k.op("pool", lambda e: e.affine_select(out=C["Lst32"][:], in_=C["ones32"][:], pattern=[[1, 128]], compare_op=ALU.is_ge,
                                            fill=0.0, base=-1, channel_multiplier=-1), reads=[C["ones32"]], writes=[C["Lst32"]])
    z32 = k.sbuf("c_z32", [128, 128], F32)
    k.op("pool", lambda e: e.memset(z32[:], 0.0), writes=[z32])
    caus32 = k.sbuf("c_caus32", [128, 128], F32)
    k.op("pool", lambda e: e.affine_select(out=caus32[:], in_=z32[:], pattern=[[1, 128]], compare_op=ALU.is_ge,
                                            fill=NEG, base=0, channel_multiplier=-1), reads=[z32], writes=[caus32])
    C["caus16"] = k.sbuf("c_caus16", [128, 128], BF16)
    k.op("dve", lambda e: e.tensor_copy(out=C["caus16"][:], in_=caus32[:]), reads=[caus32], writes=[C["caus16"]])
    C["sel16"] = k.sbuf("c_sel16", [32, 32, 128], BF16)
    with k.scope():
      sel32 = k.sbuf("c_sel32", [32, 32, 128], F32)
      k.op("pool", lambda e: e.memset(sel32[:], 1.0), writes=[sel32])
      k.op("pool", lambda e: e.affine_select(out=sel32[:], in_=sel32[:], pattern=[[-1, 32], [0, 128]], compare_op=ALU.is_equal,
                                            fill=0.0, base=0, channel_multiplier=1), reads=[sel32], writes=[sel32])
      k.op("dve", lambda e: e.tensor_copy(out=C["sel16"][:], in_=sel32[:]), reads=[sel32], writes=[C["sel16"]])
    return C


def emit_attn(k, C, T, qT_d, kT_d, v_d, cos_d, sin_d, y_d, y_col0, nheads, src=None, dstbuf=None):
    NBLK = T // 256
    NKT = T // 128
    RC = min(T, 2048)
    scale = 128 ** -0.5
    q32 = k.sbuf("a_q32", [128, T], F32); k32 = k.sbuf("a_k32", [128, T], F32)
    q16 = k.sbuf("a_q16", [128, T], BF16); k16 = k.sbuf("a_k16", [128, T], BF16)
    swp = k.sbuf("a_swp", [32, RC], F32); cs = k.sbuf("a_cos", [32, RC], F32); sn = k.sbuf("a_sin", [32, RC], F32)
    rt = k.sbuf("a_rt", [32, RC], F32)
    V1 = k.sbuf("a_V1", [128, NKT, 129], BF16)
    kmean = k.sbuf("a_kmean", [128, NBLK], F32)
    gate = k.sbuf("a_gate", [128, 32], F32)
    mx8 = k.sbuf("a_mx8", [128, 8], F32)
    biasq = k.sbuf("a_biasq", [128, 32], F32)
    maskT = k.sbuf("a_maskT", [32, 256], BF16)
    PT = [k.sbuf(f"a_PT{i}", [128, 256], BF16) for i in range(3)]
    yo = [k.sbuf(f"a_yo{i}", [128, 128], F32) for i in range(2)]
    rec = k.sbuf("a_rec", [128, 1], F32)
    ps_s = [k.psum(f"a_ps_s{i}", [128, 256]) for i in range(2)]
    ps_o = [k.psum(f"a_ps_o{i}", [128, 129]) for i in range(4)]
    ps_g = k.psum("a_ps_g", [128, 32])
    ps_t = k.psum("a_ps_t", [32, 128])
    k.op("pool", lambda e: e.memset(gate[:], -1e30), writes=[gate])
    k.op("pool", lambda e: e.memset(V1[:, :, 128:129], 1.0), writes=[V1])
    si = 0; oi = 0; yi = 0
    for h in range(nheads):
        for (dst32, srcd) in ((q32, qT_d), (k32, kT_d)):
            k.dma("sp", dst32[:], srcd[h * 128:(h + 1) * 128, :], src=src, dst=dst32)
            for c0 in range(0, T, RC):
                k.dma("sp", swp[0:16, :], srcd[h * 128 + 16:h * 128 + 32, c0:c0 + RC], src=src, dst=swp)
                k.dma("sp", swp[16:32, :], srcd[h * 128:h * 128 + 16, c0:c0 + RC], src=src, dst=swp)
                k.dma("sp", cs[:], cos_d[:, c0:c0 + RC], dst=cs)
                k.dma("sp", sn[:], sin_d[:, c0:c0 + RC], dst=sn)
                k.op("dve", lambda e: e.tensor_tensor(out=rt[:], in0=dst32[0:32, c0:c0 + RC], in1=cs[:], op=ALU.mult), reads=[dst32, cs], writes=[rt])
                k.op("dve", lambda e: e.tensor_tensor(out=swp[:], in0=swp[:], in1=sn[:], op=ALU.mult), reads=[swp, sn], writes=[swp])
                k.op("dve", lambda e: e.tensor_tensor(out=dst32[0:32, c0:c0 + RC], in0=rt[:], in1=swp[:], op=ALU.add), reads=[rt, swp], writes=[dst32])
        k.op("act", lambda e: e.copy(out=q16[:], in_=q32[:]), reads=[q32], writes=[q16])
        k.op("act", lambda e: e.copy(out=k16[:], in_=k32[:]), reads=[k32], writes=[k16])
        k.op("dve", lambda e: e.tensor_reduce(out=kmean[:], in_=k32[:].rearrange("p (b s) -> p b s", s=256), op=ALU.add, axis=AX.X),
             reads=[k32], writes=[kmean])
        k.op("dve", lambda e: e.tensor_scalar(out=kmean[:], in0=kmean[:], scalar1=1.0 / 256, scalar2=None, op0=ALU.mult), reads=[kmean], writes=[kmean])
        k.dma("pool", V1[:, :, 0:128], v_d[:, h * 128:(h + 1) * 128].rearrange("(n p) d -> p n d", p=128), src=src, dst=V1)
        for Q in range(NBLK):
            q0 = Q * 256
            use_mask = Q > 3
            if use_mask and True:
                for half in range(2):
                    qs = slice(q0 + half * 128, q0 + half * 128 + 128)
                    k.op("pe", lambda e: e.matmul(ps_g[:, 0:NBLK], lhsT=q32[:, qs], rhs=kmean[:, 0:NBLK], start=True, stop=True), reads=[q32, kmean], writes=[ps_g])
                    k.op("dve", lambda e: e.tensor_copy(out=gate[:, 0:Q], in_=ps_g[:, 0:Q]), reads=[ps_g], writes=[gate])
                    k.op("dve", lambda e: e.max(out=mx8[:], in_=gate[:, 0:max(Q, 8)]), reads=[gate], writes=[mx8])
                    k.op("dve", lambda e: e.tensor_scalar(out=biasq[:], in0=gate[:], scalar1=mx8[:, 2:3], scalar2=1.0, op0=ALU.is_ge, op1=ALU.subtract),
                         reads=[gate, mx8], writes=[biasq])
                    k.op("dve", lambda e: e.tensor_scalar(out=biasq[:], in0=biasq[:], scalar1=-NEG, scalar2=None, op0=ALU.mult), reads=[biasq], writes=[biasq])
                    k.op("pe", lambda e: e.transpose(out=ps_t[:], in_=biasq[:], identity=C["ident32"][:]), reads=[biasq, C["ident32"]], writes=[ps_t])
                    k.op("act", lambda e: e.copy(out=maskT[:, half * 128:(half + 1) * 128], in_=ps_t[:]), reads=[ps_t], writes=[maskT])
            po = [ps_o[(oi + i) % 4] for i in range(2)]; oi += 2
            jobs = [("past", j, kt) for j in range(Q) for kt in range(2)]
            for half in range(2):
                jobs += [("own", half, kt) for kt in range(half + 1)]
            firstjob = [None, None]; lastjob = [None, None]
            for ji, (kind, a, kt) in enumerate(jobs):
                halves = (0, 1) if kind == "past" else (a,)
                for hf in halves:
                    if firstjob[hf] is None:
                        firstjob[hf] = ji
                    lastjob[hf] = ji

            def emit_S(job):
                nonlocal si
                kind, a, kt = job
                ps = ps_s[si % 2]; pt = PT[si % 3]; si += 1
                if kind == "past":
                    kti = a * 2 + kt
                    k.op("pe", lambda e: e.matmul(ps[:], lhsT=k16[:, kti * 128:(kti + 1) * 128], rhs=q16[:, q0:q0 + 256], start=True, stop=not use_mask),
                         reads=[k16, q16], writes=[ps], inc=not use_mask)
                    if use_mask:
                        k.op("pe", lambda e: e.matmul(ps[:], lhsT=C["sel16"][:, a, :], rhs=maskT[:], start=False, stop=True), reads=[C["sel16"], maskT], writes=[ps])
                    k.op("act", lambda e: e.activation(out=pt[:], in_=ps[:], func=AF.Exp, scale=scale), reads=[ps], writes=[pt])
                else:
                    half = a
                    qs = slice(q0 + half * 128, q0 + half * 128 + 128)
                    kti = Q * 2 + kt
                    diag = (kt == half)
                    k.op("pe", lambda e: e.matmul(ps[:, 0:128], lhsT=k16[:, kti * 128:(kti + 1) * 128], rhs=q16[:, qs], start=True, stop=not diag),
                         reads=[k16, q16], writes=[ps], inc=not diag)
                    if diag:
                        k.op("pe", lambda e: e.matmul(ps[:, 0:128], lhsT=C["ident16"][:], rhs=C["caus16"][:], start=False, stop=True),
                             reads=[C["ident16"], C["caus16"]], writes=[ps])
                    k.op("act", lambda e: e.activation(out=pt[:, 0:128], in_=ps[:, 0:128], func=AF.Exp, scale=scale), reads=[ps], writes=[pt])
                return pt

            def emit_PV(ji, job, pt):
                kind, a, kt = job
                if kind == "past":
                    kti = a * 2 + kt
                    for half in range(2):
                        lastf = (lastjob[half] == ji)
                        k.op("pe", lambda e: e.matmul(po[half][:], lhsT=pt[:, half * 128:(half + 1) * 128], rhs=V1[:, kti, :], start=(firstjob[half] == ji), stop=lastf),
                             reads=[pt, V1], writes=[po[half]], inc=lastf)
                else:
                    half = a
                    kti = Q * 2 + kt
                    lastf = (lastjob[half] == ji)
                    k.op("pe", lambda e: e.matmul(po[half][:], lhsT=pt[:, 0:128], rhs=V1[:, kti, :], start=(firstjob[half] == ji), stop=lastf),
                         reads=[pt, V1], writes=[po[half]], inc=lastf)

            pts = {0: emit_S(jobs[0])}
            for ji, job in enumerate(jobs):
                if ji + 1 < len(jobs):
                    pts[ji + 1] = emit_S(jobs[ji + 1])
                emit_PV(ji, job, pts.pop(ji))
            for half in range(2):
                y = yo[yi % 2]; yi += 1
                k.op("dve", lambda e: e.reciprocal(out=rec[:], in_=po[half][:, 128:129]), reads=[po[half]], writes=[rec])
                k.op("dve", lambda e: e.tensor_scalar(out=y[:], in0=po[half][:, 0:128], scalar1=rec[:, 0:1], scalar2=None, op0=ALU.mult), reads=[po[half], rec], writes=[y])
                k.dma("sp", y_d[q0 + half * 128:q0 + half * 128 + 128, y_col0 + h * 128:y_col0 + (h + 1) * 128], y[:], src=y, dst=y_d)


FR = mybir.dt.float32r


def emit_ssd(k, C, T, xbcT_d, ptm_d, z_col0, dt_col0, prm, y_d, y_col0, groups=(0, 1), src=None):
    NCH = T // 128
    cw = k.sbuf("s_cw", [128, 6, 4], F32); cb = k.sbuf("s_cb", [128, 6, 1], F32)
    dtb = k.sbuf("s_dtb", [128, 8], F32); aneg = k.sbuf("s_aneg", [128, 8], F32); dsk = k.sbuf("s_dsk", [128, 8], F32)
    dskx = k.sbuf("s_dskx", [128, 8, 64], F32)
    nrm = k.sbuf("s_nrm", [128, 512], F32)
    cin = [k.sbuf(f"s_cin{i}", [128, 6, 131], F32) for i in range(2)]
    acc = k.sbuf("s_acc", [128, 6, 128], F32); tap = k.sbuf("s_tap", [128, 6, 128], F32)
    xc = k.sbuf("s_xc", [128, 6, 128], F32)
    xcr = k.sbuf("s_xcr", [128, 2, 128], FR)
    x_tm = k.sbuf("s_xtm", [128, 8, 64], F32)
    B_tm = k.sbuf("s_Btm", [128, 128], FR)
    dtr = k.sbuf("s_dtr", [128, 8], F32); dt = k.sbuf("s_dt", [128, 8], F32); dA = k.sbuf("s_dA", [128, 8], F32)
    dArep = k.sbuf("s_dArep", [128, 8, 128], F32)
    acum = k.sbuf("s_acum", [128, 8], F32); tot = k.sbuf("s_tot", [128, 8], F32)
    dec = k.sbuf("s_dec", [128, 8, 128], F32)
    CBm = k.sbuf("s_CBm", [128, 128], F32)
    Mt = k.sbuf("s_Mt", [128, 8, 128], FR)
    xdt = k.sbuf("s_xdt", [128, 8, 64], FR)
    ea = k.sbuf("s_ea", [128, 8], F32); wend = k.sbuf("s_wend", [128, 8], F32); etot = k.sbuf("s_etot", [128, 8], F32)
    xw = k.sbuf("s_xw", [128, 8, 64], FR)
    ST32 = k.sbuf("s_ST32", [128, 8, 64], F32); STr = k.sbuf("s_STr", [128, 8, 64], FR)
    y1 = k.sbuf("s_y1", [128, 8, 64], F32); t2 = k.sbuf("s_t2", [128, 8, 64], F32)
    zt = [k.sbuf(f"s_zt{i}", [128, 512], F32) for i in range(2)]
    junk = k.sbuf("s_junk", [128, 512], F32)
    ss = k.sbuf("s_ss", [128, 1], F32)
    yo = [k.sbuf(f"s_yo{i}", [128, 512], F32) for i in range(2)]
    p_x = k.psum("s_p_x", [128, 512]); p_b = k.psum("s_p_b", [128, 128]); p_cb = k.psum("s_p_cb", [128, 128])
    p_ac = k.psum("s_p_ac", [128, 16]); p_abc = k.psum("s_p_abc", [128, 1024])
    p_y = k.psum("s_p_y", [128, 512]); p_yi = k.psum("s_p_yi", [128, 512])
    for g in groups:
        xr0 = g * 512; br0 = 1024 + g * 128; cr0 = 1280 + g * 128
        k.dma("sp", cw[:], prm["convw"][g], dst=cw)
        k.dma("sp", cb[:], prm["convb"][g], dst=cb)
        k.dma("sp", dtb[:], prm["dtb"][:, g * 8:(g + 1) * 8], dst=dtb)
        k.dma("sp", aneg[:], prm["alog"][:, g * 8:(g + 1) * 8], dst=aneg)
        k.dma("sp", dsk[:], prm["dskip"][:, g * 8:(g + 1) * 8], dst=dsk)
        k.dma("sp", nrm[:], prm["norm"][:, g * 512:(g + 1) * 512], dst=nrm)
        k.op("act", lambda e: e.activation(out=aneg[:], in_=aneg[:], func=AF.Exp), reads=[aneg], writes=[aneg])
        k.op("dve", lambda e: e.tensor_scalar(out=aneg[:], in0=aneg[:], scalar1=-1.0, scalar2=None, op0=ALU.mult), reads=[aneg], writes=[aneg])
        k.op("dve", lambda e: e.tensor_copy(out=dskx[:], in_=dsk[:].unsqueeze(2).to_broadcast([128, 8, 64])), reads=[dsk], writes=[dskx])
        k.op("pool", lambda e: e.memset(ST32[:], 0.0), writes=[ST32])
        for c in range(NCH):
            t0 = c * 128
            ci = cin[c % 2]
            lo = 3 if c == 0 else 0
            if c == 0:
                k.op("pool", lambda e: e.memset(ci[:, :, 0:3], 0.0), writes=[ci])
            k.dma("sp", ci[:, 0:4, lo:131], xbcT_d[xr0:xr0 + 512, t0 - 3 + lo:t0 + 128].rearrange("(i p) t -> p i t", p=128), src=src, dst=ci)
            k.dma("sp", ci[:, 4, lo:131], xbcT_d[br0:br0 + 128, t0 - 3 + lo:t0 + 128], src=src, dst=ci)
            k.dma("sp", ci[:, 5, lo:131], xbcT_d[cr0:cr0 + 128, t0 - 3 + lo:t0 + 128], src=src, dst=ci)
            z_ = zt[c % 2]
            k.dma("sp", z_[:], ptm_d[t0:t0 + 128, z_col0 + g * 512:z_col0 + (g + 1) * 512], src=src, dst=z_)
            k.dma("sp", dtr[:], ptm_d[t0:t0 + 128, dt_col0 + g * 8:dt_col0 + (g + 1) * 8], src=src, dst=dtr)
            k.op("dve", lambda e: e.tensor_tensor(out=acc[:], in0=ci[:, :, 3:131], in1=cw[:, :, 3:4].to_broadcast([128, 6, 128]), op=ALU.mult), reads=[ci, cw], writes=[acc])
            k.op("dve", lambda e: e.tensor_tensor(out=acc[:], in0=acc[:], in1=cb[:].to_broadcast([128, 6, 128]), op=ALU.add), reads=[acc, cb], writes=[acc])
            for j in range(3):
                k.op("pool", lambda e: e.tensor_tensor(out=tap[:], in0=ci[:, :, j:j + 128], in1=cw[:, :, j:j + 1].to_broadcast([128, 6, 128]), op=ALU.mult), reads=[ci, cw], writes=[tap])
                k.op("dve", lambda e: e.tensor_tensor(out=acc[:], in0=acc[:], in1=tap[:], op=ALU.add), reads=[acc, tap], writes=[acc])
            k.op("act", lambda e: e.activation(out=xc[:], in_=acc[:], func=AF.Silu), reads=[acc], writes=[xc])
            k.op("dve", lambda e: e.tensor_copy(out=xcr[:], in_=xc[:, 4:6, :]), reads=[xc], writes=[xcr])
            for i in range(4):
                k.op("pe", lambda e: e.transpose(out=p_x[:, i * 128:(i + 1) * 128], in_=xc[:, i, :], identity=C["ident32"][:]), reads=[xc, C["ident32"]], writes=[p_x], inc=(i == 3))
            k.op("act", lambda e: e.copy(out=x_tm[:].rearrange("p h d -> p (h d)"), in_=p_x[:]), reads=[p_x], writes=[x_tm])
            k.op("pe", lambda e: e.transpose(out=p_b[:], in_=xc[:, 4, :], identity=C["ident32"][:]), reads=[xc, C["ident32"]], writes=[p_b])
            k.op("dve", lambda e: e.tensor_copy(out=B_tm[:], in_=p_b[:]), reads=[p_b], writes=[B_tm])
            k.op("dve", lambda e: e.tensor_tensor(out=dt[:], in0=dtr[:], in1=dtb[:], op=ALU.add), reads=[dtr, dtb], writes=[dt])
            k.op("act", lambda e: e.activation(out=dt[:], in_=dt[:], func=AF.Exp), reads=[dt], writes=[dt])
            k.op("act", lambda e: e.activation(out=dt[:], in_=dt[:], func=AF.Ln, bias=1.0), reads=[dt], writes=[dt])
            k.op("dve", lambda e: e.tensor_tensor(out=dA[:], in0=dt[:], in1=aneg[:], op=ALU.mult), reads=[dt, aneg], writes=[dA])
            k.op("pe", lambda e: e.matmul(p_ac[:, 0:8], lhsT=C["U32"][:], rhs=dA[:], start=True, stop=True), reads=[C["U32"], dA], writes=[p_ac])
            k.op("pe", lambda e: e.matmul(p_ac[:, 8:16], lhsT=C["ones32"][:], rhs=dA[:], start=True, stop=True), reads=[C["ones32"], dA], writes=[p_ac])
            k.op("dve", lambda e: e.tensor_copy(out=acum[:], in_=p_ac[:, 0:8]), reads=[p_ac], writes=[acum])
            k.op("dve", lambda e: e.tensor_copy(out=tot[:], in_=p_ac[:, 8:16]), reads=[p_ac], writes=[tot])
            k.op("dve", lambda e: e.tensor_copy(out=dArep[:], in_=dA[:].unsqueeze(2).to_broadcast([128, 8, 128])), reads=[dA], writes=[dArep])
            for h in range(8):
                k.op("pe", lambda e: e.matmul(p_abc[:, h * 128:(h + 1) * 128], lhsT=dArep[:, h, :], rhs=C["U32"][:], start=True, stop=True),
                     reads=[dArep, C["U32"]], writes=[p_abc], inc=(h == 7))
            k.op("dve", lambda e: e.tensor_tensor(out=dec[:], in0=p_abc[:].rearrange("p (h l) -> p h l", h=8), in1=acum[:].unsqueeze(2).to_broadcast([128, 8, 128]), op=ALU.subtract),
                 reads=[p_abc, acum], writes=[dec])
            k.op("dve", lambda e: e.tensor_scalar(out=dec[:], in0=dec[:], scalar1=0.0, scalar2=None, op0=ALU.min), reads=[dec], writes=[dec])
            k.op("act", lambda e: e.activation(out=dec[:], in_=dec[:], func=AF.Exp), reads=[dec], writes=[dec])
            k.op("pe", lambda e: e.matmul(p_cb[:], lhsT=xcr[:, 0, :], rhs=xcr[:, 1, :], start=True, stop=True), reads=[xcr], writes=[p_cb])
            k.op("dve", lambda e: e.tensor_tensor(out=CBm[:], in0=p_cb[:], in1=C["U32"][:], op=ALU.mult), reads=[p_cb, C["U32"]], writes=[CBm])
            k.op("dve", lambda e: e.tensor_tensor(out=Mt[:], in0=dec[:], in1=CBm[:].unsqueeze(1).to_broadcast([128, 8, 128]), op=ALU.mult), reads=[dec, CBm], writes=[Mt])
            k.op("pool", lambda e: e.tensor_tensor(out=xdt[:], in0=x_tm[:], in1=dt[:].unsqueeze(2).to_broadcast([128, 8, 64]), op=ALU.mult), reads=[x_tm, dt], writes=[xdt])
            for h in range(8):
                k.op("pe", lambda e: e.matmul(p_y[:, h * 64:(h + 1) * 64], lhsT=Mt[:, h, :], rhs=xdt[:, h, :], start=True, stop=True), reads=[Mt, xdt], writes=[p_y], inc=(h == 7))
            k.op("dve", lambda e: e.tensor_copy(out=STr[:], in_=ST32[:]), reads=[ST32], writes=[STr])
            k.op("pe", lambda e: e.matmul(p_yi[:], lhsT=xcr[:, 1, :], rhs=STr[:].rearrange("p h d -> p (h d)"), start=True, stop=True), reads=[xcr, STr], writes=[p_yi])
            k.op("act", lambda e: e.activation(out=ea[:], in_=acum[:], func=AF.Exp), reads=[acum], writes=[ea])
            k.op("dve", lambda e: e.tensor_tensor(out=y1[:], in0=p_yi[:].rearrange("p (h d) -> p h d", h=8), in1=ea[:].unsqueeze(2).to_broadcast([128, 8, 64]), op=ALU.mult), reads=[p_yi, ea], writes=[y1])
            k.op("dve", lambda e: e.tensor_tensor(out=y1[:], in0=y1[:], in1=p_y[:].rearrange("p (h d) -> p h d", h=8), op=ALU.add), reads=[y1, p_y], writes=[y1])
            k.op("pool", lambda e: e.tensor_tensor(out=t2[:], in0=x_tm[:], in1=dskx[:], op=ALU.mult), reads=[x_tm, dskx], writes=[t2])
            k.op("dve", lambda e: e.tensor_tensor(out=y1[:], in0=y1[:], in1=t2[:], op=ALU.add), reads=[y1, t2], writes=[y1])
            k.op("act", lambda e: e.activation(out=z_[:], in_=z_[:], func=AF.Silu), reads=[z_], writes=[z_])
            k.op("dve", lambda e: e.tensor_tensor(out=y1[:].rearrange("p h d -> p (h d)"), in0=y1[:].rearrange("p h d -> p (h d)"), in1=z_[:], op=ALU.mult), reads=[y1, z_], writes=[y1])
            k.op("act", lambda e: e.activation(out=junk[:], in_=y1[:].rearrange("p h d -> p (h d)"), func=AF.Square, accum_out=ss[:]), reads=[y1], writes=[junk, ss])
            k.op("dve", lambda e: e.tensor_scalar(out=ss[:], in0=ss[:], scalar1=1.0 / 512, scalar2=1e-6, op0=ALU.mult, op1=ALU.add), reads=[ss], writes=[ss])
            k.op("act", lambda e: e.activation(out=ss[:], in_=ss[:], func=AF.Sqrt), reads=[ss], writes=[ss])
            k.op("dve", lambda e: e.reciprocal(out=ss[:], in_=ss[:]), reads=[ss], writes=[ss])
            y_ = yo[c % 2]
            k.op("dve", lambda e: e.scalar_tensor_tensor(out=y_[:], in0=y1[:].rearrange("p h d -> p (h d)"), scalar=ss[:, 0:1], in1=nrm[:], op0=ALU.mult, op1=ALU.mult), reads=[y1, ss, nrm], writes=[y_])
            k.dma("sp", y_d[t0:t0 + 128, y_col0 + g * 512:y_col0 + (g + 1) * 512], y_[:], src=y_, dst=y_d)
            k.op("dve", lambda e: e.tensor_tensor(out=wend[:], in0=tot[:], in1=acum[:], op=ALU.subtract), reads=[tot, acum], writes=[wend])
            k.op("act", lambda e: e.activation(out=wend[:], in_=wend[:], func=AF.Exp), reads=[wend], writes=[wend])
            k.op("dve", lambda e: e.tensor_tensor(out=wend[:], in0=wend[:], in1=dt[:], op=ALU.mult), reads=[wend, dt], writes=[wend])
            k.op("pool", lambda e: e.tensor_tensor(out=xw[:], in0=x_tm[:], in1=wend[:].unsqueeze(2).to_broadcast([128, 8, 64]), op=ALU.mult), reads=[x_tm, wend], writes=[xw])
            k.op("pe", lambda e: e.matmul(p_x[:], lhsT=B_tm[:], rhs=xw[:].rearrange("p h d -> p (h d)"), start=True, stop=True), reads=[B_tm, xw], writes=[p_x])
            k.op("act", lambda e: e.activation(out=etot[:], in_=tot[:], func=AF.Exp), reads=[tot], writes=[etot])
            k.op("dve", lambda e: e.tensor_tensor(out=ST32[:], in0=ST32[:], in1=etot[:].unsqueeze(2).to_broadcast([128, 8, 64]), op=ALU.mult), reads=[ST32, etot], writes=[ST32])
            k.op("dve", lambda e: e.tensor_tensor(out=ST32[:], in0=ST32[:], in1=p_x[:].rearrange("p (h d) -> p h d", h=8), op=ALU.add), reads=[ST32, p_x], writes=[ST32])


def emit_sgu(k, C, T, ptm_d, uv_col0, prm, y_d, y_col0, src=None):
    NCH = T // 128
    lng = k.sbuf("g_lng", [128, 512], F32); lnb = k.sbuf("g_lnb", [128, 512], F32)
    wT = k.sbuf("g_wT", [128, 4, 128], F32); bs = k.sbuf("g_bs", [128, 4], F32)
    uv = [k.sbuf(f"g_uv{i}", [128, 1024], F32) for i in range(2)]
    guv = k.sbuf("g_guv", [128, 1024], F32)
    st = k.sbuf("g_st", [128, 6], F32); mv = k.sbuf("g_mv", [128, 2], F32); rs = k.sbuf("g_rs", [128, 1], F32)
    vn = k.sbuf("g_vn", [128, 512], F32)
    yo = [k.sbuf(f"g_yo{i}", [128, 512], F32) for i in range(2)]
    ps = k.psum("g_ps", [128, 512])
    k.dma("sp", lng[:], prm["lng"], dst=lng); k.dma("sp", lnb[:], prm["lnb"], dst=lnb)
    k.dma("sp", wT[:], prm["wT"].rearrange("g s t -> s g t"), dst=wT); k.dma("sp", bs[:], prm["bs"], dst=bs)
    k.op("pool", lambda e: e.affine_select(out=wT[:], in_=wT[:], pattern=[[0, 4], [1, 128]], compare_op=ALU.is_ge, fill=0.0, base=0, channel_multiplier=-1),
         reads=[wT], writes=[wT])
    for c in range(NCH):
        t0 = c * 128
        uv_ = uv[c % 2]; y_ = yo[c % 2]
        k.dma("sp", uv_[:], ptm_d[t0:t0 + 128, uv_col0:uv_col0 + 1024], src=src, dst=uv_)
        k.op("act", lambda e: e.activation(out=guv[:], in_=uv_[:], func=AF.Gelu_apprx_tanh), reads=[uv_], writes=[guv])
        k.op("dve", lambda e: e.bn_stats(out=st[:], in_=guv[:, 512:1024]), reads=[guv], writes=[st])
        k.op("dve", lambda e: e.bn_aggr(out=mv[:], in_=st[:]), reads=[st], writes=[mv])
        k.op("dve", lambda e: e.tensor_scalar(out=rs[:], in0=mv[:, 1:2], scalar1=1e-6, scalar2=None, op0=ALU.add), reads=[mv], writes=[rs])
        k.op("act", lambda e: e.activation(out=rs[:], in_=rs[:], func=AF.Sqrt), reads=[rs], writes=[rs])
        k.op("dve", lambda e: e.reciprocal(out=rs[:], in_=rs[:]), reads=[rs], writes=[rs])
        k.op("dve", lambda e: e.tensor_scalar(out=vn[:], in0=guv[:, 512:1024], scalar1=mv[:, 0:1], scalar2=rs[:, 0:1], op0=ALU.subtract, op1=ALU.mult), reads=[guv, mv, rs], writes=[vn])
        k.op("dve", lambda e: e.tensor_tensor(out=vn[:], in0=vn[:], in1=lng[:], op=ALU.mult), reads=[vn, lng], writes=[vn])
        k.op("dve", lambda e: e.tensor_tensor(out=vn[:], in0=vn[:], in1=lnb[:], op=ALU.add), reads=[vn, lnb], writes=[vn])
        for g in range(4):
            k.op("pe", lambda e: e.matmul(ps[:, g * 128:(g + 1) * 128], lhsT=wT[:, g, :], rhs=vn[:, g * 128:(g + 1) * 128], start=True, stop=True), reads=[wT, vn], writes=[ps], inc=(g == 3))
        for g in range(4):
            k.op("dve", lambda e: e.scalar_tensor_tensor(out=y_[:, g * 128:(g + 1) * 128], in0=ps[:, g * 128:(g + 1) * 128], scalar=bs[:, g:g + 1], in1=guv[:, g * 128:(g + 1) * 128],
                                                          op0=ALU.add, op1=ALU.mult), reads=[ps, bs, guv], writes=[y_])
        k.dma("sp", y_d[t0:t0 + 128, y_col0:y_col0 + 512], y_[:], src=y_, dst=y_d)


FR = mybir.dt.float32r
U32 = mybir.dt.uint32
I32 = mybir.dt.int32
D = 2048


def emit_post(k, C, T, xT_d, y_d, wo_d, vec_d, wr_d, br_d, wg_d, wu_d, wd_d, out_d, final=False, fn_d=None, xsrc=None):
    NT = T // 128
    BS = 512
    SUB = BS // 128
    NB = 2 * T // BS + 32
    TT = 512
    NSUB = TT // 128
    xTv = xT_d.rearrange("(c p) t -> p c t", p=128)
    X1 = k.dram("p_X1", [D, T], F32); X1v = X1.t.rearrange("(c p) t -> p c t", p=128)
    H2 = k.dram("p_H2", [T, D], F32)
    Xd = k.dram("p_Xd", [NB * BS, D], F32)
    Yd = k.dram("p_Yd", [NB * BS, D], F32)
    outv = out_d.t.rearrange("(c p) t -> p c t", p=128)
    vt = k.sbuf("p_vt", [128, 5, 16], F32); a2 = k.sbuf("p_a2", [128, 16], F32)
    AB = k.sbuf("p_AB", [128, NT, 64], F32)
    CUM = k.sbuf("p_CUM", [128, NT, 32], F32)
    GT = k.sbuf("p_GT", [128, NT, 2], F32)
    carry = k.sbuf("p_carry", [128, 32], F32)
    k.dma("sp", vt[:], vec_d, dst=vt)
    k.op("dve", lambda e: e.scalar_tensor_tensor(out=a2[:], in0=vt[:, 2, :], scalar=1.0, in1=vt[:, 1, :], op0=ALU.add, op1=ALU.mult), reads=[vt], writes=[a2])
    k.op("pool", lambda e: e.memset(carry[:], 0.0), writes=[carry])
    with k.scope():
        ystage = k.sbuf("pa_ystage", [128, NSUB, D], F32)
        yT16 = k.sbuf("pa_yT16", [128, 16, TT], BF16)
        xacc = k.sbuf("pa_xacc", [128, 16, TT], F32)
        tmp = k.sbuf("pa_tmp", [128, 16, TT], F32)
        rstd = k.sbuf("pa_rstd", [128, TT], F32)
        wo = [k.sbuf(f"pa_wo{i}", [128, 16, 128], BF16) for i in range(3)]
        wr = k.sbuf("pa_wr", [128, 16, 36], F32); br = k.sbuf("pa_br", [128, 36], F32)
        hrow = [k.sbuf(f"pa_hrow{i}", [128, D], F32) for i in range(2)]
        lg = k.sbuf("pa_lg", [128, 36], F32)
        m4 = k.sbuf("pa_m4", [128, 1], F32); s4 = k.sbuf("pa_s4", [128, 1], F32); e4 = k.sbuf("pa_e4", [128, 4], F32)
        oh4 = k.sbuf("pa_oh4", [128, 4], F32)
        fs = k.sbuf("pa_fs", [128, 8], F32); mx8 = k.sbuf("pa_mx8", [128, 8], F32); e8 = k.sbuf("pa_e8", [128, 8], F32)
        selA = k.sbuf("pa_selA", [128, 8], F32); selB = k.sbuf("pa_selB", [128, 8], F32)
        nl1 = k.sbuf("pa_nl1", [128, 1], F32); den = k.sbuf("pa_den", [128, 1], F32); e2v = k.sbuf("pa_e2v", [128, 1], F32)
        Msum = k.sbuf("pa_Msum", [128, 32], F32)
        p_t = [k.psum(f"pa_p_t{i}", [128, 512]) for i in range(2)]
        p_m = [k.psum(f"pa_p_m{i}", [128, TT]) for i in range(2)]
        p_s = k.psum("pa_p_s", [128, TT])
        p_r = k.psum("pa_p_r", [128, 36])
        p_c = k.psum("pa_p_c", [128, 64])
        k.dma("sp", wr[:], wr_d, dst=wr); k.dma("sp", br[:], br_d, dst=br)
        ti = 0; mi = 0; hi = 0
        for t in range(T // TT):
            t0 = t * TT
            k.dma("sp", xacc[:], xTv[:, :, t0:t0 + TT], src=xsrc, dst=xacc)
            k.dma("sp", ystage[:], y_d.t[t0:t0 + TT, :].rearrange("(s p) d -> p s d", p=128), src=y_d, dst=ystage)
            for c in range(16):
                pt = p_t[ti % 2]; ti += 1
                for s_ in range(NSUB):
                    k.op("pe", lambda e: e.transpose(out=pt[:, s_ * 128:(s_ + 1) * 128], in_=ystage[:, s_, c * 128:(c + 1) * 128], identity=C["ident32"][:]),
                         reads=[ystage, C["ident32"]], writes=[pt], inc=(s_ == NSUB - 1))
                if c % 2 == 0:
                    k.op("act", lambda e: e.copy(out=yT16[:, c, :], in_=pt[:, 0:TT]), reads=[pt], writes=[yT16])
                else:
                    k.op("dve", lambda e: e.tensor_copy(out=yT16[:, c, :], in_=pt[:, 0:TT]), reads=[pt], writes=[yT16])
            for d in range(16):
                w_ = wo[mi % 3]; pm = p_m[mi % 2]; mi += 1
                k.dma("pool", w_[:], wo_d[d], dst=w_)
                for c in range(16):
                    k.op("pe", lambda e: e.matmul(pm[:], lhsT=w_[:, c, :], rhs=yT16[:, c, :], start=(c == 0), stop=(c == 15)), reads=[w_, yT16], writes=[pm], inc=(c == 15))
                k.op("dve", lambda e: e.scalar_tensor_tensor(out=xacc[:, d, :], in0=pm[:], scalar=vt[:, 0, d:d + 1], in1=xacc[:, d, :], op0=ALU.mult, op1=ALU.add),
                     reads=[pm, vt, xacc], writes=[xacc])
            k.dma("sp", X1v[:, :, t0:t0 + TT], xacc[:], src=xacc, dst=X1)
            k.op("act", lambda e: e.activation(out=tmp[:], in_=xacc[:], func=AF.Square), reads=[xacc], writes=[tmp])
            for c in range(16):
                k.op("pe", lambda e: e.matmul(p_s[:], lhsT=C["ones32"][:], rhs=tmp[:, c, :], start=(c == 0), stop=(c == 15)), reads=[C["ones32"], tmp], writes=[p_s], inc=(c == 15))
            k.op("dve", lambda e: e.tensor_scalar(out=rstd[:], in0=p_s[:], scalar1=1.0 / D, scalar2=1e-6, op0=ALU.mult, op1=ALU.add), reads=[p_s], writes=[rstd])
            k.op("act", lambda e: e.activation(out=rstd[:], in_=rstd[:], func=AF.Sqrt), reads=[rstd], writes=[rstd])
            k.op("dve", lambda e: e.reciprocal(out=rstd[:], in_=rstd[:]), reads=[rstd], writes=[rstd])
            for c in range(16):
                k.op("dve", lambda e: e.scalar_tensor_tensor(out=tmp[:, c, :], in0=xacc[:, c, :], scalar=a2[:, c:c + 1], in1=rstd[:], op0=ALU.mult, op1=ALU.mult),
                     reads=[xacc, a2, rstd], writes=[tmp])
                k.op("act", lambda e: e.activation(out=tmp[:, c, :], in_=tmp[:, c, :], func=AF.Identity, bias=vt[:, 3, c:c + 1]), reads=[tmp, vt], writes=[tmp])
            for s_ in range(NSUB):
                n = t * NSUB + s_
                ts_ = slice(s_ * 128, (s_ + 1) * 128)
                for c in range(16):
                    k.op("pe", lambda e: e.matmul(p_r[:], lhsT=tmp[:, c, ts_], rhs=wr[:, c, :], start=(c == 0), stop=(c == 15)), reads=[tmp, wr], writes=[p_r], inc=(c == 15))
                k.op("dve", lambda e: e.tensor_tensor(out=lg[:], in0=p_r[:], in1=br[:], op=ALU.add), reads=[p_r, br], writes=[lg])
                k.op("dve", lambda e: e.tensor_reduce(out=m4[:], in_=lg[:, 0:4], op=ALU.max, axis=AX.X), reads=[lg], writes=[m4])
                k.op("dve", lambda e: e.tensor_scalar(out=oh4[:], in0=lg[:, 0:4], scalar1=m4[:, 0:1], scalar2=None, op0=ALU.is_ge), reads=[lg, m4], writes=[oh4])
                k.op("dve", lambda e: e.tensor_scalar(out=e4[:], in0=lg[:, 0:4], scalar1=m4[:, 0:1], scalar2=None, op0=ALU.subtract), reads=[lg, m4], writes=[e4])
                k.op("act", lambda e: e.activation(out=e4[:], in_=e4[:], func=AF.Exp), reads=[e4], writes=[e4])
                k.op("dve", lambda e: e.tensor_reduce(out=s4[:], in_=e4[:], op=ALU.add, axis=AX.X), reads=[e4], writes=[s4])
                k.op("dve", lambda e: e.tensor_scalar(out=fs[:], in0=lg[:, 4:12], scalar1=oh4[:, 0:1], scalar2=None, op0=ALU.mult), reads=[lg, oh4], writes=[fs])
                for g in range(1, 4):
                    k.op("dve", lambda e: e.scalar_tensor_tensor(out=fs[:], in0=lg[:, 4 + 8 * g:12 + 8 * g], scalar=oh4[:, g:g + 1], in1=fs[:], op0=ALU.mult, op1=ALU.add),
                         reads=[lg, oh4, fs], writes=[fs])
                k.op("dve", lambda e: e.max(out=mx8[:], in_=fs[:]), reads=[fs], writes=[mx8])
                k.op("dve", lambda e: e.tensor_scalar(out=selA[:], in0=fs[:], scalar1=mx8[:, 0:1], scalar2=None, op0=ALU.is_ge), reads=[fs, mx8], writes=[selA])
                k.op("dve", lambda e: e.tensor_scalar(out=selB[:], in0=fs[:], scalar1=mx8[:, 1:2], scalar2=None, op0=ALU.is_ge), reads=[fs, mx8], writes=[selB])
                k.op("dve", lambda e: e.tensor_tensor(out=selB[:], in0=selB[:], in1=selA[:], op=ALU.subtract), reads=[selB, selA], writes=[selB])
                k.op("dve", lambda e: e.tensor_tensor(out=e2v[:], in0=mx8[:, 1:2], in1=mx8[:, 0:1], op=ALU.subtract), reads=[mx8], writes=[e2v])
                k.op("act", lambda e: e.activation(out=e2v[:], in_=e2v[:], func=AF.Exp), reads=[e2v], writes=[e2v])
                k.op("dve", lambda e: e.scalar_tensor_tensor(out=den[:], in0=e2v[:], scalar=1.0, in1=s4[:], op0=ALU.add, op1=ALU.mult), reads=[e2v, s4], writes=[den])
                k.op("dve", lambda e: e.reciprocal(out=GT[:, n, 0:1], in_=den[:]), reads=[den], writes=[GT])
                k.op("dve", lambda e: e.tensor_tensor(out=GT[:, n, 1:2], in0=GT[:, n, 0:1], in1=e2v[:], op=ALU.mult), reads=[GT, e2v], writes=[GT])
                for g in range(4):
                    k.op("dve", lambda e: e.tensor_scalar(out=AB[:, n, 8 * g:8 * g + 8], in0=selA[:], scalar1=oh4[:, g:g + 1], scalar2=None, op0=ALU.mult), reads=[selA, oh4], writes=[AB])
                    k.op("dve", lambda e: e.tensor_scalar(out=AB[:, n, 32 + 8 * g:40 + 8 * g], in0=selB[:], scalar1=oh4[:, g:g + 1], scalar2=None, op0=ALU.mult), reads=[selB, oh4], writes=[AB])
                k.op("dve", lambda e: e.tensor_tensor(out=Msum[:], in0=AB[:, n, 0:32], in1=AB[:, n, 32:64], op=ALU.add), reads=[AB], writes=[Msum])
                k.op("pe", lambda e: e.matmul(p_c[:, 0:32], lhsT=C["Lst32"][:], rhs=Msum[:], start=True, stop=True), reads=[C["Lst32"], Msum], writes=[p_c])
                k.op("pe", lambda e: e.matmul(p_c[:, 32:64], lhsT=C["ones32"][:], rhs=Msum[:], start=True, stop=True), reads=[C["ones32"], Msum], writes=[p_c])
                k.op("dve", lambda e: e.tensor_tensor(out=CUM[:, n, :], in0=p_c[:, 0:32], in1=carry[:], op=ALU.add), reads=[p_c, carry], writes=[CUM])
                k.op("dve", lambda e: e.tensor_tensor(out=carry[:], in0=carry[:], in1=p_c[:, 32:64], op=ALU.add), reads=[carry, p_c], writes=[carry])
                hr = hrow[hi % 2]; hi += 1
                for q4 in range(4):
                    pt = p_t[ti % 2]; ti += 1
                    for cc in range(4):
                        c = q4 * 4 + cc
                        k.op("pe", lambda e: e.transpose(out=pt[:, cc * 128:(cc + 1) * 128], in_=tmp[:, c, ts_], identity=C["ident32"][:]), reads=[tmp, C["ident32"]], writes=[pt], inc=(cc == 3))
                    if q4 % 2 == 0:
                        k.op("act", lambda e: e.copy(out=hr[:, q4 * 512:(q4 + 1) * 512], in_=pt[:]), reads=[pt], writes=[hr])
                    else:
                        k.op("dve", lambda e: e.tensor_copy(out=hr[:, q4 * 512:(q4 + 1) * 512], in_=pt[:]), reads=[pt], writes=[hr])
                k.dma("sp", H2.t[t0 + s_ * 128:t0 + (s_ + 1) * 128, :], hr[:], src=hr, dst=H2)
    pstart = k.sbuf("p_pstart", [128, 32], F32)
    IDXW = k.sbuf("p_IDXW", [128, NB, 4], I32)
    DEST = k.sbuf("p_DEST", [128, NT, 2], I32)
    with k.scope():
        pc = k.sbuf("pb_pc", [128, 32], F32); pend = k.sbuf("pb_pend", [128, 32], F32)
        onesr = k.sbuf("pb_onesr", [128, 32], F32)
        I128 = k.sbuf("pb_I128", [128, NB], F32); BE = k.sbuf("pb_BE", [128, NB], F32)
        fcp = k.sbuf("pb_fcp", [128, 4], F32); idxf = k.sbuf("pb_idxf", [128, NB, 4], F32)
        dsum = k.sbuf("pb_dsum", [128, 32], F32); dj = k.sbuf("pb_dj", [128, 32], F32); dtmp = k.sbuf("pb_dtmp", [128, NT, 2], F32)
        k.op("pool", lambda e: e.iota(I128[:], pattern=[[BS, NB]], base=0, channel_multiplier=0, allow_small_or_imprecise_dtypes=True), writes=[I128])
        for ex in range(32):
            k.op("dve", lambda e: e.tensor_scalar(out=BE[:], in0=I128[:], scalar1=carry[:, ex:ex + 1], scalar2=0.0, op0=ALU.is_lt, op1=ALU.add, accum_out=pc[:, ex:ex + 1]),
                 reads=[I128, carry], writes=[BE, pc])
        k.op("dve", lambda e: e.tensor_scalar(out=pc[:], in0=pc[:], scalar1=float(BS), scalar2=None, op0=ALU.mult), reads=[pc], writes=[pc])
        k.op("pool", lambda e: e.memset(onesr[:], 1.0), writes=[onesr])
        k.op("dve", lambda e: e.tensor_tensor_scan(out=pend[:], data0=onesr[:], data1=pc[:], initial=0.0, op0=ALU.mult, op1=ALU.add), reads=[onesr, pc], writes=[pend])
        k.op("dve", lambda e: e.tensor_tensor(out=pstart[:], in0=pend[:], in1=pc[:], op=ALU.subtract), reads=[pend, pc], writes=[pstart])
        k.op("pool", lambda e: e.memset(BE[:], 0.0), writes=[BE])
        for ex in range(32):
            k.op("dve", lambda e: e.scalar_tensor_tensor(out=BE[:], in0=I128[:], scalar=pend[:, ex:ex + 1], in1=BE[:], op0=ALU.is_ge, op1=ALU.add), reads=[I128, pend, BE], writes=[BE])
        k.op("dve", lambda e: e.tensor_scalar(out=BE[:], in0=BE[:], scalar1=31.0, scalar2=512.0, op0=ALU.min, op1=ALU.mult), reads=[BE], writes=[BE])
        k.op("pool", lambda e: e.iota(fcp[:], pattern=[[128, 4]], base=0, channel_multiplier=1, allow_small_or_imprecise_dtypes=True), writes=[fcp])
        k.op("dve", lambda e: e.tensor_tensor(out=idxf[:], in0=BE[:].unsqueeze(2).to_broadcast([128, NB, 4]), in1=fcp[:].unsqueeze(1).to_broadcast([128, NB, 4]), op=ALU.add), reads=[BE, fcp], writes=[idxf])
        k.op("dve", lambda e: e.tensor_copy(out=IDXW[:], in_=idxf[:]), reads=[idxf], writes=[IDXW])
        for n in range(NT):
            k.op("dve", lambda e: e.tensor_tensor(out=dsum[:], in0=CUM[:, n, :], in1=pstart[:], op=ALU.add), reads=[CUM, pstart], writes=[dsum])
            for j in range(2):
                k.op("dve", lambda e: e.tensor_tensor(out=dj[:], in0=dsum[:], in1=AB[:, n, 32 * j:32 * j + 32], op=ALU.mult), reads=[dsum, AB], writes=[dj])
                k.op("dve", lambda e: e.tensor_reduce(out=dtmp[:, n, j:j + 1], in_=dj[:], op=ALU.add, axis=AX.X), reads=[dj], writes=[dtmp])
        k.op("dve", lambda e: e.tensor_copy(out=DEST[:], in_=dtmp[:]), reads=[dtmp], writes=[DEST])
    with k.scope():
        zr = k.sbuf("pc_zero", [128, D], F32)
        k.op("pool", lambda e: e.memset(zr[:], 0.0), writes=[zr])
        for r in range(NB * SUB):
            k.dma("sp", Xd.t[r * 128:(r + 1) * 128, :], zr[:], src=zr, dst=Xd)
    with k.scope():
        hr = [k.sbuf(f"pc_hr{i}", [128, D], F32) for i in range(3)]
        for n in range(NT):
            h_ = hr[n % 3]
            k.dma("pool", h_[:], H2.t[n * 128:(n + 1) * 128, :], src=H2, dst=h_)
            for j in range(2):
                k.indirect("pool", Xd, h_, DEST, out_ap=Xd.t[:, :], out_idx=DEST[:, n, j:j + 1], in_ap=h_[:])
    with k.scope():
        xb = [k.sbuf(f"pd_xb{i}", [128, D], F32) for i in range(2)]
        xbT = [k.sbuf(f"pd_xbT{i}", [128, 16, BS], BF16) for i in range(2)]
        wg = [k.sbuf(f"pd_wg{fc}", [128, 2048], BF16) for fc in range(4)]
        wu = [k.sbuf(f"pd_wu{fc}", [128, 2048], BF16) for fc in range(4)]
        wd = [[k.sbuf(f"pd_wd{i}_{fc}", [128, 2048], BF16) for fc in range(4)] for i in range(2)]
        sg = k.sbuf("pd_sg", [128, BS], F32)
        hid = [k.sbuf(f"pd_hid{i}", [128, 4, BS], BF16) for i in range(2)]
        yb = [k.sbuf(f"pd_yb{i}", [128, D], F32) for i in range(2)]
        p_t = [k.psum(f"pd_p_t{i}", [128, 512]) for i in range(2)]
        p_g = [k.psum(f"pd_p_g{i}", [128, BS]) for i in range(2)]
        p_u = [k.psum(f"pd_p_u{i}", [128, BS]) for i in range(2)]
        p_d = [k.psum(f"pd_p_d{i}", [128, 512]) for i in range(2)]
        ti = 0; gi = 0; di = 0; xi = 0; yi = 0
        for i in range(NB):
            xT_ = xbT[i % 2]; wd_ = wd[i % 2]; hid_ = hid[i % 2]
            for fc in range(4):
                k.indirect("pool", wg[fc], None, IDXW, out_ap=wg[fc][:], in_ap=wg_d[:, :], in_idx=IDXW[:, i, fc:fc + 1])
                k.indirect("pool", wu[fc], None, IDXW, out_ap=wu[fc][:], in_ap=wu_d[:, :], in_idx=IDXW[:, i, fc:fc + 1])
            for fc in range(4):
                k.indirect("pool", wd_[fc], None, IDXW, out_ap=wd_[fc][:], in_ap=wd_d[:, :], in_idx=IDXW[:, i, fc:fc + 1])
            for s_ in range(SUB):
                x_ = xb[xi % 2]; xi += 1
                k.dma("sp", x_[:], Xd.t[i * BS + s_ * 128:i * BS + (s_ + 1) * 128, :], src=Xd, dst=x_)
                for q4 in range(4):
                    pt = p_t[ti % 2]; ti += 1
                    for cc in range(4):
                        c = q4 * 4 + cc
                        k.op("pe", lambda e: e.transpose(out=pt[:, cc * 128:(cc + 1) * 128], in_=x_[:, c * 128:(c + 1) * 128], identity=C["ident32"][:]), reads=[x_, C["ident32"]], writes=[pt], inc=(cc == 3))
                    if q4 % 2 == 0:
                        k.op("act", lambda e: e.copy(out=xT_[:, q4 * 4:(q4 + 1) * 4, s_ * 128:(s_ + 1) * 128], in_=pt[:].rearrange("p (c t) -> p c t", c=4)), reads=[pt], writes=[xT_])
                    else:
                        k.op("dve", lambda e: e.tensor_copy(out=xT_[:, q4 * 4:(q4 + 1) * 4, s_ * 128:(s_ + 1) * 128], in_=pt[:].rearrange("p (c t) -> p c t", c=4)), reads=[pt], writes=[xT_])
            for fc in range(4):
                pg = p_g[gi % 2]; pu = p_u[gi % 2]; gi += 1
                for c in range(16):
                    k.op("pe", lambda e: e.matmul(pg[:], lhsT=wg[fc][:, c * 128:(c + 1) * 128], rhs=xT_[:, c, :], start=(c == 0), stop=(c == 15)), reads=[wg[fc], xT_], writes=[pg], inc=(c == 15))
                for c in range(16):
                    k.op("pe", lambda e: e.matmul(pu[:], lhsT=wu[fc][:, c * 128:(c + 1) * 128], rhs=xT_[:, c, :], start=(c == 0), stop=(c == 15)), reads=[wu[fc], xT_], writes=[pu], inc=(c == 15))
                k.op("act", lambda e: e.activation(out=sg[:], in_=pg[:], func=AF.Silu), reads=[pg], writes=[sg])
                k.op("dve", lambda e: e.tensor_tensor(out=hid_[:, fc, :], in0=sg[:], in1=pu[:], op=ALU.mult), reads=[sg, pu], writes=[hid_])
            for s_ in range(SUB):
                y_ = yb[yi % 2]; yi += 1
                for dq in range(4):
                    pd = p_d[di % 2]; di += 1
                    for fc in range(4):
                        k.op("pe", lambda e: e.matmul(pd[:], lhsT=hid_[:, fc, s_ * 128:(s_ + 1) * 128], rhs=wd_[fc][:, dq * 512:(dq + 1) * 512], start=(fc == 0), stop=(fc == 3)), reads=[hid_, wd_[fc]], writes=[pd], inc=(fc == 3))
                    if dq % 2 == 0:
                        k.op("act", lambda e: e.copy(out=y_[:, dq * 512:(dq + 1) * 512], in_=pd[:]), reads=[pd], writes=[y_])
                    else:
                        k.op("dve", lambda e: e.tensor_copy(out=y_[:, dq * 512:(dq + 1) * 512], in_=pd[:]), reads=[pd], writes=[y_])
                k.dma("sp", Yd.t[i * BS + s_ * 128:i * BS + (s_ + 1) * 128, :], y_[:], src=y_, dst=Yd)
    with k.scope():
        y1 = [k.sbuf(f"pe_y1{i}", [128, D], F32) for i in range(2)]
        y2 = [k.sbuf(f"pe_y2{i}", [128, D], F32) for i in range(2)]
        x1 = [k.sbuf(f"pe_x1{i}", [128, 16, 128], F32) for i in range(2)]
        sq = k.sbuf("pe_sq", [128, 16, 128], F32); rs = k.sbuf("pe_rs", [128, 128], F32)
        fn17 = k.sbuf("pe_fn17", [128, 17], F32); fnv = k.sbuf("pe_fn", [128, 16], F32)
        p_t = [k.psum(f"pe_p_t{i}", [128, 512]) for i in range(2)]
        p_s = k.psum("pe_p_s", [128, 128])
        alp = k.sbuf("pe_alp", [128, 1], F32); oma = k.sbuf("pe_oma", [128, 1], F32); scl = k.sbuf("pe_scl", [128, 128], F32)
        k.dma("sp", fn17[:], fn_d, dst=fn17)
        k.op("dve", lambda e: e.tensor_copy(out=alp[:], in_=fn17[:, 16:17]), reads=[fn17], writes=[alp])
        k.op("dve", lambda e: e.tensor_scalar(out=fnv[:], in0=fn17[:, 0:16], scalar1=alp[:, 0:1], scalar2=None, op0=ALU.mult), reads=[fn17, alp], writes=[fnv])
        k.op("dve", lambda e: e.tensor_scalar(out=oma[:], in0=alp[:], scalar1=-1.0, scalar2=1.0, op0=ALU.mult, op1=ALU.add), reads=[alp], writes=[oma])
        ti = 0
        for n in range(NT):
            a_ = y1[n % 2]; b_ = y2[n % 2]; x_ = x1[n % 2]
            k.indirect("pool", a_, Yd, DEST, out_ap=a_[:], in_ap=Yd.t[:, :], in_idx=DEST[:, n, 0:1])
            k.indirect("pool", b_, Yd, DEST, out_ap=b_[:], in_ap=Yd.t[:, :], in_idx=DEST[:, n, 1:2])
            k.dma("sp", x_[:], X1v[:, :, n * 128:(n + 1) * 128], src=X1, dst=x_)
            k.op("dve", lambda e: e.tensor_scalar(out=a_[:], in0=a_[:], scalar1=GT[:, n, 0:1], scalar2=None, op0=ALU.mult), reads=[a_, GT], writes=[a_])
            k.op("dve", lambda e: e.scalar_tensor_tensor(out=a_[:], in0=b_[:], scalar=GT[:, n, 1:2], in1=a_[:], op0=ALU.mult, op1=ALU.add), reads=[b_, GT, a_], writes=[a_])
            for q4 in range(4):
                pt = p_t[ti % 2]; ti += 1
                for cc in range(4):
                    c = q4 * 4 + cc
                    k.op("pe", lambda e: e.transpose(out=pt[:, cc * 128:(cc + 1) * 128], in_=a_[:, c * 128:(c + 1) * 128], identity=C["ident32"][:]), reads=[a_, C["ident32"]], writes=[pt], inc=(cc == 3))
                for cc in range(4):
                    c = q4 * 4 + cc
                    k.op("dve", lambda e: e.scalar_tensor_tensor(out=x_[:, c, :], in0=pt[:, cc * 128:(cc + 1) * 128], scalar=vt[:, 4, c:c + 1], in1=x_[:, c, :], op0=ALU.mult, op1=ALU.add),
                         reads=[pt, vt, x_], writes=[x_])
            if True:
                k.op("act", lambda e: e.activation(out=sq[:], in_=x_[:], func=AF.Square), reads=[x_], writes=[sq])
                for c in range(16):
                    k.op("pe", lambda e: e.matmul(p_s[:], lhsT=C["ones32"][:], rhs=sq[:, c, :], start=(c == 0), stop=(c == 15)), reads=[C["ones32"], sq], writes=[p_s], inc=(c == 15))
                k.op("dve", lambda e: e.tensor_scalar(out=rs[:], in0=p_s[:], scalar1=1.0 / D, scalar2=1e-6, op0=ALU.mult, op1=ALU.add), reads=[p_s], writes=[rs])
                k.op("act", lambda e: e.activation(out=rs[:], in_=rs[:], func=AF.Sqrt), reads=[rs], writes=[rs])
                k.op("dve", lambda e: e.reciprocal(out=rs[:], in_=rs[:]), reads=[rs], writes=[rs])
                for c in range(16):
                    k.op("dve", lambda e: e.tensor_scalar(out=scl[:], in0=rs[:], scalar1=fnv[:, c:c + 1], scalar2=oma[:, 0:1], op0=ALU.mult, op1=ALU.add), reads=[rs, fnv, oma], writes=[scl])
                    k.op("dve", lambda e: e.tensor_tensor(out=x_[:, c, :], in0=x_[:, c, :], in1=scl[:], op=ALU.mult), reads=[x_, scl], writes=[x_])
            k.dma("sp", outv[:, :, n * 128:(n + 1) * 128], x_[:], src=x_, dst=out_d)


D = 2048
TM_BLOCKS = [(0, 512), (512, 512), (1024, 512), (1536, 512), (2048, 512), (2560, 16)]


def emit_pre(k, C, T, xT_d, wfm_d, wtm_d, wtm_last_d, vec_d, pfm, ptm, xsrc=None):
    TT = 512
    xTv = xT_d.rearrange("(c p) t -> p c t", p=128)
    vt = k.sbuf("r_vt", [128, 3, 16], F32); acol = k.sbuf("r_acol", [128, 16], F32)
    xt = k.sbuf("r_xt", [128, 16, TT], F32); tmp = k.sbuf("r_tmp", [128, 16, TT], F32)
    hT = k.sbuf("r_hT", [128, 16, TT], BF16); rstd = k.sbuf("r_rstd", [128, TT], F32)
    wb = [k.sbuf(f"r_wb{i}", [128, 16, 128], BF16) for i in range(3)]
    wt = [k.sbuf(f"r_wt{i}", [128, 16, 512], BF16) for i in range(2)]
    ob = [k.sbuf(f"r_ob{i}", [128, TT], F32) for i in range(3)]
    pss = k.psum("r_pss", [128, TT]); psm = [k.psum(f"r_psm{i}", [128, TT]) for i in range(4)]
    k.dma("sp", vt[:], vec_d, dst=vt)
    k.op("dve", lambda e: e.scalar_tensor_tensor(out=acol[:], in0=vt[:, 1, :], scalar=1.0, in1=vt[:, 0, :], op0=ALU.add, op1=ALU.mult), reads=[vt], writes=[acol])
    wi = 0; ti = 0
    for t in range(T // TT):
        t0 = t * TT
        k.dma("sp", xt[:], xTv[:, :, t0:t0 + TT], src=xsrc, dst=xt)
        k.op("act", lambda e: e.activation(out=tmp[:], in_=xt[:], func=AF.Square), reads=[xt], writes=[tmp])
        for c in range(16):
            k.op("pe", lambda e: e.matmul(pss[:], lhsT=C["ones32"][:], rhs=tmp[:, c, :], start=(c == 0), stop=(c == 15)), reads=[C["ones32"], tmp], writes=[pss], inc=(c == 15))
        k.op("dve", lambda e: e.tensor_scalar(out=rstd[:], in0=pss[:], scalar1=1.0 / D, scalar2=1e-6, op0=ALU.mult, op1=ALU.add), reads=[pss], writes=[rstd])
        k.op("act", lambda e: e.activation(out=rstd[:], in_=rstd[:], func=AF.Sqrt), reads=[rstd], writes=[rstd])
        k.op("dve", lambda e: e.reciprocal(out=rstd[:], in_=rstd[:]), reads=[rstd], writes=[rstd])
        for c in range(16):
            k.op("dve", lambda e: e.scalar_tensor_tensor(out=tmp[:, c, :], in0=xt[:, c, :], scalar=acol[:, c:c + 1], in1=rstd[:], op0=ALU.mult, op1=ALU.mult), reads=[xt, acol, rstd], writes=[tmp])
            k.op("act", lambda e: e.activation(out=hT[:, c, :], in_=tmp[:, c, :], func=AF.Identity, bias=vt[:, 2, c:c + 1]), reads=[tmp, vt], writes=[hT])
        for m in range(20):
            wj = wb[wi % 3]; p = psm[wi % 4]; o = ob[wi % 3]; wi += 1
            k.dma("pool", wj[:], wfm_d[m], dst=wj)
            for c in range(16):
                k.op("pe", lambda e: e.matmul(p[:], lhsT=wj[:, c, :], rhs=hT[:, c, :], start=(c == 0), stop=(c == 15)), reads=[wj, hT], writes=[p], inc=(c == 15))
            if m % 2 == 0:
                k.op("dve", lambda e: e.tensor_copy(out=o[:], in_=p[:]), reads=[p], writes=[o])
            else:
                k.op("act", lambda e: e.copy(out=o[:], in_=p[:]), reads=[p], writes=[o])
            k.dma("sp", pfm.t[m * 128:(m + 1) * 128, t0:t0 + TT], o[:], src=o, dst=pfm)
        for bi, (c0, n) in enumerate(TM_BLOCKS):
            wj = wt[ti % 2]; ti += 1
            if n == 512:
                k.dma("pool", wj[:], wtm_d[bi], dst=wj)
            else:
                k.dma("pool", wj[:, :, 0:n], wtm_last_d, dst=wj)
            for s_ in range(TT // 128):
                p = psm[wi % 4]; o = ob[wi % 3]; wi += 1
                for c in range(16):
                    k.op("pe", lambda e: e.matmul(p[:, 0:n], lhsT=hT[:, c, s_ * 128:(s_ + 1) * 128], rhs=wj[:, c, 0:n], start=(c == 0), stop=(c == 15)), reads=[hT, wj], writes=[p], inc=(c == 15))
                if s_ % 2 == 0:
                    k.op("dve", lambda e: e.tensor_copy(out=o[:, 0:n], in_=p[:, 0:n]), reads=[p], writes=[o])
                else:
                    k.op("act", lambda e: e.copy(out=o[:, 0:n], in_=p[:, 0:n]), reads=[p], writes=[o])
                k.dma("sp", ptm.t[t0 + s_ * 128:t0 + (s_ + 1) * 128, c0:c0 + n], o[:, 0:n], src=o, dst=ptm)


NCH = 16


T_SEQ = 8192
LAYER_INS = [("wfm", [20, 128, 16, 128]), ("wtm", [5, 128, 16, 512]), ("wtl", [128, 16, 16]),
             ("sgu_lng", [128, 512]), ("sgu_lnb", [128, 512]), ("sgu_wT", [4, 128, 128]), ("sgu_bs", [128, 4]),
             ("ssd_convw", [2, 128, 6, 4]), ("ssd_convb", [2, 128, 6, 1]), ("ssd_dtb", [128, 16]), ("ssd_alog", [128, 16]),
             ("ssd_dskip", [128, 16]), ("ssd_norm", [128, 1024]),
             ("wo", [16, 128, 16, 128]), ("wr", [128, 16, 36]), ("br", [128, 36]),
             ("wg", [32 * 512, 2048]), ("wu", [32 * 512, 2048]), ("wd", [32 * 512, 2048]), ("fn", [128, 17])]


def emit_mod(k, C, wada_d, cT_d, bias_d, ncols_d, VEC1, VEC2, nlayers):
    NJ = nlayers * 96
    ct = k.sbuf("m_ct", [128, 16, 2], F32); bt = k.sbuf("m_bt", [128, NJ], F32); res = k.sbuf("m_res", [128, NJ], F32)
    nct = k.sbuf("m_nct", [128, 2 * nlayers, 16], F32)
    wb = [k.sbuf(f"m_wb{i}", [128, 16, 128], F32) for i in range(3)]
    ps = [k.psum(f"m_ps{i}", [128, 2]) for i in range(2)]
    v1 = k.sbuf("m_v1", [128, nlayers, 3, 16], F32); v2 = k.sbuf("m_v2", [128, nlayers, 5, 16], F32)
    k.dma("sp", ct[:], cT_d, dst=ct); k.dma("sp", bt[:], bias_d, dst=bt); k.dma("sp", nct[:], ncols_d, dst=nct)
    k.op("act", lambda e: e.activation(out=ct[:], in_=ct[:], func=AF.Silu), reads=[ct], writes=[ct])
    for j in range(NJ):
        wj = wb[j % 3]; p = ps[j % 2]
        k.dma("sp", wj[:], wada_d[j], dst=wj)
        for c in range(16):
            k.op("pe", lambda e: e.matmul(p[:], lhsT=wj[:, c, :], rhs=ct[:, c, :], start=(c == 0), stop=(c == 15)), reads=[wj, ct], writes=[p], inc=(c == 15))
        k.op("dve", lambda e: e.tensor_tensor(out=res[:, j:j + 1], in0=p[:, 0:1], in1=bt[:, j:j + 1], op=ALU.add), reads=[p, bt], writes=[res])
    for l in range(nlayers):
        b0 = l * 96
        for (dst, slot, srcap) in ((v1, 0, nct[:, l, :]), (v1, 1, res[:, b0 + 16:b0 + 32]), (v1, 2, res[:, b0:b0 + 16]),
                                   (v2, 0, res[:, b0 + 32:b0 + 48]), (v2, 1, nct[:, nlayers + l, :]), (v2, 2, res[:, b0 + 64:b0 + 80]),
                                   (v2, 3, res[:, b0 + 48:b0 + 64]), (v2, 4, res[:, b0 + 80:b0 + 96])):
            k.op("dve", lambda e: e.tensor_copy(out=dst[:, l, slot, :], in_=srcap), reads=[res, nct], writes=[dst])
    k.dma("sp", VEC1.t.rearrange("l p a c -> p l a c"), v1[:], src=v1, dst=VEC1)
    k.dma("sp", VEC2.t.rearrange("l p a c -> p l a c"), v2[:], src=v2, dst=VEC2)


def build_fused(T=T_SEQ, nlayers=4):
    nc = bass.Bass("TRN2", target_bir_lowering=False)
    k = K(nc, same_engine_sync=True)
    A = lambda n, s: nc.dram_tensor(n, s, F32, kind="ExternalInput").ap()
    xT = A("xT", [2048, T])
    cos = A("cos", [32, T]); sin = A("sin", [32, T])
    wada = A("wada", [nlayers * 96, 128, 16, 128]); cTd = A("cT", [128, 16, 2]); biasd = A("bias", [128, nlayers * 96]); ncols = A("ncols", [128, 2 * nlayers, 16])
    L = [{n: A(f"{n}_l{l}", shp) for n, shp in LAYER_INS} for l in range(nlayers)]
    out = k.dram("xo", [2048, T], F32, kind="ExternalOutput")
    VEC1 = k.dram("vec1", [nlayers, 128, 3, 16], F32); VEC2 = k.dram("vec2", [nlayers, 128, 5, 16], F32)
    XB = [k.dram("xres", [2048, T], F32) for _ in range(2)]
    pfm = k.dram("pfm", [2560, T], F32); ptm = k.dram("ptm", [T, 2576], F32); y_d = k.dram("ymix", [T, 2048], F32)
    C = emit_consts(k)
    with k.scope():
        emit_mod(k, C, wada, cTd, biasd, ncols, VEC1, VEC2, nlayers)
    for l in range(nlayers):
        W = L[l]
        xin = xT if l == 0 else XB[(l - 1) % 2].t
        xout = out if l == nlayers - 1 else XB[l % 2]
        with k.scope():
            emit_pre(k, C, T, xin, W["wfm"], W["wtm"], W["wtl"], VEC1.t[l], pfm, ptm)
        with k.scope():
            emit_sgu(k, C, T, ptm.t, 0, {"lng": W["sgu_lng"], "lnb": W["sgu_lnb"], "wT": W["sgu_wT"], "bs": W["sgu_bs"]}, y_d, 0)
        with k.scope():
            emit_attn(k, C, T, pfm.t[0:512, :], pfm.t[512:1024, :], ptm.t[:, 1024:1536], cos, sin, y_d, 512, 4)
        with k.scope():
            emit_ssd(k, C, T, pfm.t[1024:2560, :], ptm.t, 1536, 2560,
                     {"convw": W["ssd_convw"], "convb": W["ssd_convb"], "dtb": W["ssd_dtb"], "alog": W["ssd_alog"], "dskip": W["ssd_dskip"], "norm": W["ssd_norm"]}, y_d, 1024)
        with k.scope():
            emit_post(k, C, T, xin, y_d, W["wo"], VEC2.t[l], W["wr"], W["br"], W["wg"], W["wu"], W["wd"], xout, final=True, fn_d=W["fn"])
    k.finish(outs=[out]); k.close()
    return nc


def _wl(Wc, nb=128):
    n = Wc.shape[1] // nb
    return np.ascontiguousarray(Wc.reshape(16, 128, n, nb).transpose(2, 1, 0, 3))


def _col(v):
    return v.reshape(16, 128).T


def _rep(v):
    return np.ascontiguousarray(np.broadcast_to(np.asarray(v, np.float32).reshape(1, -1), (128, v.size)))


def _wgl(w):
    return np.ascontiguousarray(w.reshape(32, 16, 128, 4, 128).transpose(0, 3, 2, 1, 4).reshape(32 * 512, 2048))


def _conv_tiles(a):
    out = []
    for g in range(2):
        rows = [a[g * 512 + i * 128:g * 512 + (i + 1) * 128] for i in range(4)] + [a[1024 + g * 128:1024 + (g + 1) * 128], a[1280 + g * 128:1280 + (g + 1) * 128]]
        out.append(np.stack(rows, axis=1))
    return np.ascontiguousarray(np.stack(out, 0)).astype(np.float32)


def _rot_tables(T):
    pos = np.arange(T, dtype=np.float32)
    inv = (np.float32(500000.0) ** (-np.arange(0, 32, 2, dtype=np.float32) / np.float32(32))).astype(np.float32)
    ang = pos[:, None] * inv[None, :]
    c, s = np.cos(ang).astype(np.float32), np.sin(ang).astype(np.float32)
    return np.ascontiguousarray(np.concatenate([c, c], 1).T), np.ascontiguousarray(np.concatenate([-s, s], 1).T)


def _layer_shared(l, p, last):
    f = lambda n: np.asarray(p[n][l], np.float32)
    w_in = f("w_in")
    W_fm = np.concatenate([w_in[:, 1024:2048], w_in[:, 3584:5120]], 1)
    W_tm = np.concatenate([w_in[:, 0:1024], w_in[:, 2048:2560], w_in[:, 2560:3584], w_in[:, 5120:5136]], 1)
    Wr = np.concatenate([f("w_coarse")] + [f("w_fine")[g] for g in range(4)], axis=1)
    return {
        "wfm": _wl(W_fm), "wtm": _wl(W_tm[:, :2560], 512), "wtl": np.ascontiguousarray(W_tm[:, 2560:].reshape(16, 128, 16).transpose(1, 0, 2)),
        "sgu_lng": _rep(f("sgu_ln_g")), "sgu_lnb": _rep(f("sgu_ln_b")), "sgu_wT": np.ascontiguousarray(f("sgu_w").transpose(0, 2, 1)),
        "sgu_bs": np.ascontiguousarray(f("sgu_b").T),
        "ssd_convw": _conv_tiles(np.ascontiguousarray(f("conv_w").T)), "ssd_convb": _conv_tiles(f("conv_b")[:, None]),
        "ssd_dtb": _rep(f("dt_bias")), "ssd_alog": _rep(f("a_log")), "ssd_dskip": _rep(f("d_skip")), "ssd_norm": _rep(f("ssm_norm")),
        "wo": _wl(f("w_out")), "wr": np.ascontiguousarray(Wr.reshape(16, 128, 36).transpose(1, 0, 2)),
        "br": _rep(np.concatenate([f("b_coarse"), f("b_fine").reshape(-1)])),
        "wg": _wgl(f("w_gate")), "wu": _wgl(f("w_up")), "wd": np.ascontiguousarray(f("w_down").reshape(32 * 512, 2048)),
        "fn": np.ascontiguousarray(np.concatenate([_col(np.asarray(p["final_norm"], np.float32)), np.full((128, 1), 1.0 if last else 0.0, np.float32)], 1)),
    }


def _fused_inputs(p, T=T_SEQ, layers=(0, 1, 2, 3), nb=4, xT_list=None, total_layers=4):
    x = np.asarray(p["x"], np.float32); c = np.asarray(p["c"], np.float32)
    cosT, sinT = _rot_tables(T)
    w_ada = np.asarray(p["w_ada"], np.float32); b_ada = np.asarray(p["b_ada"], np.float32)
    nl = len(layers)
    W = np.concatenate([w_ada[l] for l in layers], axis=1)
    shared = {"cos": cosT, "sin": sinT, "wada": _wl(W),
              "bias": np.ascontiguousarray(np.concatenate([b_ada[l] for l in layers]).reshape(nl * 96, 128).T),
              "ncols": np.ascontiguousarray(np.stack([_col(np.asarray(p["norm1"][l], np.float32)) for l in layers] +
                                                     [_col(np.asarray(p["norm2"][l], np.float32)) for l in layers], 1))}
    for li, l in enumerate(layers):
        for n, a in _layer_shared(l, p, last=(l == total_layers - 1)).items():
            shared[f"{n}_l{li}"] = a
    ins = []
    for b in range(nb):
        d = dict(shared)
        d["xT"] = np.ascontiguousarray(x[b, :T].T) if xT_list is None else xT_list[b]
        cc = _col(c[b])[:, :, None]
        d["cT"] = np.ascontiguousarray(np.concatenate([cc, cc], 2))
        ins.append(d)
    return ins


LAUNCHES = [[0, 1, 2, 3]]
_NC_CACHE = {}


def kernel(**p):
    from concourse.bass_utils import run_bass_kernel_spmd
    xT = None
    for layers in LAUNCHES:
        nl = len(layers)
        if nl not in _NC_CACHE:
            _NC_CACHE[nl] = build_fused(T_SEQ, nl)
        ins = _fused_inputs(p, T_SEQ, layers, 4, xT)
        res = run_bass_kernel_spmd(_NC_CACHE[nl], ins, core_ids=[0, 1, 2, 3])
        xT = [res.results[b]["xo"] for b in range(4)]
    return np.ascontiguousarray(np.stack([xT[b].T for b in range(4)], 0)).astype(np.float32)
```
